# Optimizing a Trainium2 kernel written in Bass

```python
import math
import jax, jax.numpy as jnp
from jax import lax
import numpy as np

D_MODEL = 1024
BATCH = 8
SEQ = 2048
DEPTH = 1

MLA_HEADS = D_MODEL // 128
QK_NOPE_DIM = 64
QK_ROPE_DIM = 32
V_HEAD_DIM = 64
Q_LORA_RANK = 3 * D_MODEL // 8
KV_LORA_RANK = D_MODEL // 4
MLA_WIDTH = MLA_HEADS * V_HEAD_DIM
CONV_CHANNELS = D_MODEL - MLA_WIDTH
CONV_WIDTH = 31
ROPE_THETA = 10000.0
Q_BLOCK = 128
N_EXPERTS = 16
CAPACITY_FACTOR = 2
D_FF_EXPERT = 2 * D_MODEL
IN_COLS = Q_LORA_RANK + KV_LORA_RANK + QK_ROPE_DIM + 2 * CONV_CHANNELS
DEEPNORM_ALPHA = (2.0 * DEPTH) ** 0.25
DEEPNORM_BETA = (8.0 * DEPTH) ** -0.25
LN_EPS = 1e-5
RMS_EPS = 1e-6

kernel_name = "hybrid_mla_conformer_ecmoe_deepnorm"


def layer_norm(x, g, b):
    xf = x.astype(jnp.float32)
    mu = jnp.mean(xf, axis=-1, keepdims=True)
    xc = xf - mu
    var = jnp.mean(xc * xc, axis=-1, keepdims=True)
    y = xc * lax.rsqrt(var + LN_EPS) * g.astype(jnp.float32) + b.astype(jnp.float32)
    return y.astype(x.dtype)


def rms_norm(x, g):
    xf = x.astype(jnp.float32)
    y = xf * lax.rsqrt(jnp.mean(xf * xf, axis=-1, keepdims=True) + RMS_EPS)
    return (y * g.astype(jnp.float32)).astype(x.dtype)


def rope(x, positions):
    half = x.shape[-1] // 2
    inv_freq = ROPE_THETA ** (-jnp.arange(half, dtype=jnp.float32) / half)
    ang = positions.astype(jnp.float32)[..., None] * inv_freq
    cos = jnp.cos(ang)[:, :, None, :]
    sin = jnp.sin(ang)[:, :, None, :]
    xf = x.astype(jnp.float32)
    x1, x2 = xf[..., :half], xf[..., half:]
    out = jnp.concatenate([x1 * cos - x2 * sin, x2 * cos + x1 * sin], axis=-1)
    return out.astype(x.dtype)


def mla_attention(cq, ckv, kr, positions, q_norm_g, w_qb, kv_norm_g, w_kvb):
    B, S, _ = cq.shape
    H = MLA_HEADS
    q = jnp.einsum('bsr,rf->bsf', rms_norm(cq, q_norm_g), w_qb).reshape(B, S, H, QK_NOPE_DIM + QK_ROPE_DIM)
    kv = jnp.einsum('bsr,rf->bsf', rms_norm(ckv, kv_norm_g), w_kvb).reshape(B, S, H, QK_NOPE_DIM + V_HEAD_DIM)
    q = jnp.concatenate([q[..., :QK_NOPE_DIM], rope(q[..., QK_NOPE_DIM:], positions)], axis=-1)
    k_pe = jnp.broadcast_to(rope(kr[:, :, None, :], positions), (B, S, H, QK_ROPE_DIM))
    k = jnp.concatenate([kv[..., :QK_NOPE_DIM], k_pe], axis=-1)
    v = kv[..., QK_NOPE_DIM:]
    scale = (QK_NOPE_DIM + QK_ROPE_DIM) ** -0.5
    n_blocks = S // Q_BLOCK
    q_blocks = q.reshape(B, n_blocks, Q_BLOCK, H, -1).transpose(1, 0, 2, 3, 4)

    def attend(qb):
        s = jnp.einsum('bqhd,bkhd->bhqk', qb, k).astype(jnp.float32) * scale
        p = jax.nn.softmax(s, axis=-1).astype(v.dtype)
        return jnp.einsum('bhqk,bkhd->bqhd', p, v)

    o = lax.map(attend, q_blocks)
    return o.transpose(1, 0, 2, 3, 4).reshape(B, S, H * V_HEAD_DIM)


def conformer_conv(u, conv_w, conv_b, conv_ln_g, conv_ln_b):
    a, g = u[..., :CONV_CHANNELS], u[..., CONV_CHANNELS:]
    h = a * jax.nn.sigmoid(g)
    h = lax.conv_general_dilated(
        h, conv_w[:, None, :].astype(h.dtype), window_strides=(1,), padding='SAME',
        dimension_numbers=('NWC', 'WIO', 'NWC'), feature_group_count=CONV_CHANNELS)
    h = h + conv_b
    h = layer_norm(h, conv_ln_g, conv_ln_b)
    return jax.nn.silu(h)


def expert_choice_moe(h, w_router, w_gate, w_up, w_down):
    B, S, D = h.shape
    capacity = CAPACITY_FACTOR * S // N_EXPERTS
    logits = jnp.einsum('bsd,de->bse', h, w_router).astype(jnp.float32)
    affinity = jax.nn.softmax(logits, axis=-1)
    gates, idx = lax.top_k(affinity.transpose(0, 2, 1), capacity)
    xg = jax.vmap(lambda hb, ib: hb[ib])(h, idx)
    a = jnp.einsum('becd,edf->becf', xg, w_gate)
    u = jnp.einsum('becd,edf->becf', xg, w_up)
    y = jnp.einsum('becf,efd->becd', jax.nn.silu(a) * u, w_down)
    y = y * gates[..., None].astype(y.dtype)

    def scatter(yb, ib):
        return jnp.zeros((S, D), yb.dtype).at[ib.reshape(-1)].add(yb.reshape(-1, D))

    return jax.vmap(scatter)(y, idx)


def setup_inputs(seed: int = 0) -> dict:
    key = jax.random.key(seed)
    ks = jax.random.split(key, 24)
    f32 = jnp.float32
    L = DEPTH

    def nrm(k, shape, scale):
        return jax.random.normal(k, shape, f32) * scale

    def gain(k, shape):
        return 1.0 + 0.02 * jax.random.normal(k, shape, f32)

    x = jax.random.normal(ks[0], (BATCH, SEQ, D_MODEL), f32)
    offsets = jax.random.randint(ks[1], (BATCH, 1), 0, 64, dtype=jnp.int32)
    positions = jnp.arange(SEQ, dtype=jnp.int32)[None, :] + offsets
    return {
        "x": x,
        "positions": positions,
        "emb_ln_g": gain(ks[2], (D_MODEL,)),
        "emb_ln_b": nrm(ks[3], (D_MODEL,), 0.02),
        "w_in": nrm(ks[4], (L, D_MODEL, IN_COLS), D_MODEL ** -0.5),
        "q_norm_g": gain(ks[5], (L, Q_LORA_RANK)),
        "w_qb": nrm(ks[6], (L, Q_LORA_RANK, MLA_HEADS * (QK_NOPE_DIM + QK_ROPE_DIM)), Q_LORA_RANK ** -0.5),
        "kv_norm_g": gain(ks[7], (L, KV_LORA_RANK)),
        "w_kvb": nrm(ks[8], (L, KV_LORA_RANK, MLA_HEADS * (QK_NOPE_DIM + V_HEAD_DIM)), KV_LORA_RANK ** -0.5),
        "conv_w": nrm(ks[9], (L, CONV_WIDTH, CONV_CHANNELS), CONV_WIDTH ** -0.5),
        "conv_b": nrm(ks[10], (L, CONV_CHANNELS), 0.02),
        "conv_ln_g": gain(ks[11], (L, CONV_CHANNELS)),
        "conv_ln_b": nrm(ks[12], (L, CONV_CHANNELS), 0.02),
        "w_o": nrm(ks[13], (L, MLA_WIDTH + CONV_CHANNELS, D_MODEL), D_MODEL ** -0.5 * DEEPNORM_BETA),
        "ln1_g": gain(ks[14], (L, D_MODEL)),
        "ln1_b": nrm(ks[15], (L, D_MODEL), 0.02),
        "w_router": nrm(ks[16], (L, D_MODEL, N_EXPERTS), D_MODEL ** -0.5),
        "w_gate": nrm(ks[17], (L, N_EXPERTS, D_MODEL, D_FF_EXPERT), D_MODEL ** -0.5),
        "w_up": nrm(ks[18], (L, N_EXPERTS, D_MODEL, D_FF_EXPERT), D_MODEL ** -0.5),
        "w_down": nrm(ks[19], (L, N_EXPERTS, D_FF_EXPERT, D_MODEL), D_FF_EXPERT ** -0.5 * DEEPNORM_BETA),
        "ln2_g": gain(ks[20], (L, D_MODEL)),
        "ln2_b": nrm(ks[21], (L, D_MODEL), 0.02),
    }


def reference(x, positions, emb_ln_g, emb_ln_b, w_in, q_norm_g, w_qb, kv_norm_g, w_kvb,
              conv_w, conv_b, conv_ln_g, conv_ln_b, w_o, ln1_g, ln1_b,
              w_router, w_gate, w_up, w_down, ln2_g, ln2_b):
    h = layer_norm(x, emb_ln_g, emb_ln_b)
    c1 = Q_LORA_RANK
    c2 = c1 + KV_LORA_RANK
    c3 = c2 + QK_ROPE_DIM
    for l in range(DEPTH):
        proj = jnp.einsum('bsd,df->bsf', h, w_in[l])
        attn_out = mla_attention(proj[..., :c1], proj[..., c1:c2], proj[..., c2:c3], positions,
                                 q_norm_g[l], w_qb[l], kv_norm_g[l], w_kvb[l])
        conv_out = conformer_conv(proj[..., c3:], conv_w[l], conv_b[l], conv_ln_g[l], conv_ln_b[l])
        groups = jnp.concatenate([attn_out, conv_out], axis=-1)
        mix = jnp.einsum('bsf,fd->bsd', groups, w_o[l])
        h = layer_norm(DEEPNORM_ALPHA * h + mix, ln1_g[l], ln1_b[l])
        moe = expert_choice_moe(h, w_router[l], w_gate[l], w_up[l], w_down[l])
        h = layer_norm(DEEPNORM_ALPHA * h + moe, ln2_g[l], ln2_b[l])
    return h
```

```python
import numpy as np
import ml_dtypes
import concourse.bass as bass
import concourse.mybir as mybir
from concourse.bass_utils import run_bass_kernel_spmd

F32 = mybir.dt.float32
BF16 = mybir.dt.bfloat16
I32 = mybir.dt.int32
AF = mybir.ActivationFunctionType
ALU = mybir.AluOpType
AX = mybir.AxisListType

S = 2048
D = 1024
NT = 16
H = 8
E = 16
CAP = 256
FF = 2048
ALPHA = float(2.0 ** 0.25)
LN_EPS = 1e-5
RMS_EPS = 1e-6
SCALE = float(96.0 ** -0.5)
PI = float(np.pi)
TWO_PI = float(2.0 * np.pi)
NBIS = 28
FG = 512
NWB = 5


class _Op:
    __slots__ = ("eng", "fn", "deps", "sig", "sigval", "is_dma", "dsem", "dval")


class Sched:
    ENG = ("pe", "act", "dve", "pool", "sp")

    def __init__(self, nc, n_dma_sems=24):
        self.nc = nc
        self.ops = []
        self.last_w = {}
        self.readers = {}
        self.n_dma = n_dma_sems
        self.dma_rr = 0
        self.dma_rrq = {}
        self.dma_last = [None] * n_dma_sems
        self.dma_count = [0] * n_dma_sems
        self.eng_last = {e: None for e in self.ENG}
        self.out_dmas = []
        self.capture = None

    def add(self, eng, fn, r=(), w=(), dma=False):
        if self.capture is not None:
            self.capture.append((eng, fn, tuple(r), tuple(w), dma))
            return None
        op = _Op()
        op.eng = eng
        op.fn = fn
        op.deps = {}
        op.sig = False
        op.sigval = 0
        op.is_dma = dma
        for x in r:
            wr = self.last_w.get(x)
            if wr is not None:
                op.deps[wr] = "raw"
        for x in w:
            wr = self.last_w.get(x)
            if wr is not None and wr not in op.deps:
                op.deps[wr] = "waw"
            for rd in self.readers.get(x, ()):
                if rd is not op and rd not in op.deps:
                    op.deps[rd] = "war"
        for x in r:
            self.readers.setdefault(x, []).append(op)
        for x in w:
            self.last_w[x] = op
            self.readers[x] = []
        if dma:
            half = self.n_dma // 2
            base = half if eng == "pool" else 0
            k = base + self.dma_rrq.get(eng, 0)
            self.dma_rrq[eng] = (self.dma_rrq.get(eng, 0) + 1) % half
            prev = self.dma_last[k]
            if prev is not None:
                op.deps[prev] = "raw"
            self.dma_count[k] += 1
            op.dsem = k
            op.dval = 16 * self.dma_count[k]
            self.dma_last[k] = op
        else:
            self.eng_last[eng] = op
        self.ops.append(op)
        return op

    def barrier(self):
        lasts = [o for o in self.eng_last.values() if o is not None]
        lasts += [o for o in self.dma_last if o is not None]
        for e in self.ENG:
            op = _Op()
            op.eng = e
            op.fn = None
            op.deps = {o: "raw" for o in lasts}
            op.sig = False
            op.sigval = 0
            op.is_dma = False
            self.ops.append(op)


def _mk_sems(nc, names):
    import contextlib
    st = contextlib.ExitStack()
    sems = [st.enter_context(nc.semaphore(n)) for n in names]
    return st, sems


def emit_program(sch, nc):
    engobj = {"pe": nc.tensor, "act": nc.scalar, "dve": nc.vector, "pool": nc.gpsimd, "sp": nc.sync}

    def skip(op, d, kind):
        if d.is_dma or op.is_dma:
            return False
        if d.eng != op.eng:
            return False
        if op.fn is None:
            return True
        if op.eng == "pe":
            return True
        return False

    for op in sch.ops:
        for d, kind in op.deps.items():
            if d.is_dma or skip(op, d, kind):
                continue
            d.sig = True
    cnt = {e: 0 for e in Sched.ENG}
    for op in sch.ops:
        if op.fn is not None and (not op.is_dma) and op.sig:
            cnt[op.eng] += 1
            op.sigval = cnt[op.eng]
    st, sems = _mk_sems(nc, ["se_" + e for e in Sched.ENG] + ["sd_%d" % i for i in range(sch.n_dma)])
    esem = {e: sems[i] for i, e in enumerate(Sched.ENG)}
    dsem = sems[len(Sched.ENG):]
    waited = {e: {} for e in Sched.ENG}
    with st:
        for op in sch.ops:
            eo = engobj[op.eng]
            need = {}
            for d, kind in op.deps.items():
                if skip(op, d, kind):
                    continue
                if d.is_dma:
                    key = ("d", d.dsem)
                    val = d.dval
                else:
                    key = ("e", d.eng)
                    val = d.sigval
                if need.get(key, 0) < val:
                    need[key] = val
            for key, val in need.items():
                if waited[op.eng].get(key, 0) >= val:
                    continue
                waited[op.eng][key] = val
                sem = dsem[key[1]] if key[0] == "d" else esem[key[1]]
                eo.wait_ge(sem, val)
            if op.fn is None:
                continue
            inst = op.fn(eo)
            if op.is_dma:
                inst.then_inc(dsem[op.dsem], 16)
            elif op.sig:
                inst.then_inc(esem[op.eng], 1)
        for k in range(sch.n_dma):
            if sch.dma_count[k] > 0:
                nc.sync.wait_ge(dsem[k], 16 * sch.dma_count[k])


class Arena:
    LO = 16512
    HI = 229344

    def __init__(self, nc):
        self.nc = nc
        self.free = [(self.LO, self.HI)]
        self.pending = []
        self.n = 0
        self.live = {}

    def alloc(self, name, shape, dtype):
        esz = 2 if dtype == BF16 else 4
        nbytes = esz
        for d_ in shape[1:]:
            nbytes *= d_
        nbytes = (nbytes + 63) // 64 * 64
        for i, (lo, hi) in enumerate(self.free):
            if hi - lo >= nbytes:
                self.free[i] = (lo + nbytes, hi)
                self.n += 1
                t = self.nc.alloc_sbuf_tensor_at("%s_%d" % (name, self.n), list(shape), dtype, offset=lo)
                self.live[id(t)] = (lo, lo + nbytes)
                return t
        raise RuntimeError("SBUF arena out of memory for %s (%d bytes) free=%s" % (name, nbytes, self.free))

    def release(self, *tiles):
        for t in tiles:
            self.pending.append(self.live.pop(id(t)))

    def flush(self):
        segs = sorted(self.free + self.pending)
        self.pending = []
        out = []
        for lo, hi in segs:
            if lo == hi:
                continue
            if out and out[-1][1] == lo:
                out[-1] = (out[-1][0], hi)
            else:
                out.append((lo, hi))
        self.free = out


def build(debug=None):
    nc = bass.Bass("TRN2", target_bir_lowering=False)
    sch = Sched(nc)
    ar = Arena(nc)
    A = sch.add
    dbg_outs = []

    def din(name, shape, dtype=F32):
        return nc.dram_tensor(name, list(shape), dtype, kind="ExternalInput").ap()

    x_d = din("x", [S, D])
    pos_d = din("pos", [128, 512], I32)
    g0_d = din("emb_ln_g", [128, D])
    b0_d = din("emb_ln_b", [128, D])
    w_in_d = din("w_in", [D, 1696])
    qg_d = din("q_norm_g", [128, 3])
    wqb_d = din("w_qb", [384, 768])
    kvg_d = din("kv_norm_g", [128, 2])
    wkvb_d = din("w_kvb", [256, 1024])
    cw_d = din("conv_w", [128, 4 * 31])
    cb_d = din("conv_b", [128, 4])
    clg_d = din("conv_ln_g", [128, 4])
    clb_d = din("conv_ln_b", [128, 4])
    wo_d = din("w_o", [D, D])
    g1_d = din("ln1_g", [128, D])
    b1_d = din("ln1_b", [128, D])
    wr_d = din("w_router", [D, E])
    g1pk_d = din("ln1_g_pk", [128, 8])
    b1pk_d = din("ln1_b_pk", [128, 8])
    wg_d = din("w_gate", [E, D, FF])
    wu_d = din("w_up", [E, D, FF])
    wd_d = din("w_down", [E, FF, D])
    g2_d = din("ln2_g", [128, D])
    b2_d = din("ln2_b", [128, D])
    c_identf_d = din("c_identf", [128, 128])
    c_identb_d = din("c_identb", [128, 128], BF16)
    c_iota_d = din("c_iota", [128, 256])
    c_iotap_d = din("c_iotap", [128, 2])
    c_ustr_d = din("c_ustr", [128, 128], BF16)
    c_invf_d = din("c_invf", [128, 1])
    c_tp_d = din("c_tp", [128, NT * 2], BF16)
    c_gmat_d = din("c_gmat", [128, 128])
    out_d = nc.dram_tensor("out", [S, D], F32, kind="ExternalOutput").ap()

    def dump(name, tile_ap, shape, dtype=F32):
        if debug is None or name not in debug:
            return
        t = nc.dram_tensor("dbg_" + name, list(shape), dtype, kind="ExternalOutput").ap()
        dbg_outs.append("dbg_" + name)
        sch.barrier()
        A("sp", lambda e: e.dma_start(out=t, in_=tile_ap), r=[], w=[], dma=True)
        sch.barrier()

    def DMA(q, out, in_, r=(), w=()):
        return A(q, lambda e: e.dma_start(out=out, in_=in_), r=r, w=w, dma=True)

    pst = [nc.alloc_psum_tensor("ps%d" % i, [128, 1024], F32) for i in range(4)]

    def bank(i):
        return pst[i // 2][:, (i % 2) * 512:(i % 2 + 1) * 512]

    def bankb(i):
        return pst[i // 2].bitcast(BF16)[:, (i % 2) * 1024:(i % 2 + 1) * 1024]

    def BK(i):
        return "psum%d" % i

    identf = ar.alloc("identf", [128, 128], F32)
    identb = ar.alloc("identb", [128, 128], BF16)
    onesf = ar.alloc("onesf", [128, 128], F32)
    onesb = ar.alloc("onesb", [128, 128], BF16)
    iota_c = ar.alloc("iota_c", [128, 256], F32)
    iota_p = ar.alloc("iota_p", [128, 2], F32)
    ustr = ar.alloc("ustr", [128, 128], BF16)
    DMA("sp", identf[:], c_identf_d, w=["identf"])
    DMA("sp", identb[:], c_identb_d, w=["identb"])
    DMA("sp", iota_c[:], c_iota_d, w=["iota_c"])
    DMA("sp", iota_p[:], c_iotap_d, w=["iota_p"])
    DMA("sp", ustr[:], c_ustr_d, w=["ustr"])
    A("pool", lambda e: e.memset(onesf[:], 1.0), w=["onesf"])
    A("pool", lambda e: e.memset(onesb[:], 1.0), w=["onesb"])

    def swpipe(n, stages):
        ns = len(stages)
        for it in range(n + ns - 1):
            lists = []
            for si in range(ns):
                t_ = it - si
                if 0 <= t_ < n:
                    sch.capture = []
                    stages[si](t_)
                    lists.append(sch.capture)
                    sch.capture = None
            pos_ = [0] * len(lists)
            left = sum(len(l_) for l_ in lists)
            while left:
                for li, l_ in enumerate(lists):
                    if pos_[li] < len(l_):
                        eng, fn, r_, w_, dma_ = l_[pos_[li]]
                        pos_[li] += 1
                        left -= 1
                        sch.add(eng, fn, r_, w_, dma_)

    def ln_small(nm, nrot):
        return [dict(stats=ar.alloc(nm + "st", [128, 2, 6], F32), mv=ar.alloc(nm + "mv", [128, 2], F32), std=ar.alloc(nm + "sd", [128, 1], F32),
                     rstd=ar.alloc(nm + "rs", [128, 1], F32), nmr=ar.alloc(nm + "nm", [128, 1], F32), name="%s%d_" % (nm, i_)) for i_ in range(nrot)]

    def ln_small_free(lst):
        for d_ in lst:
            ar.release(d_["stats"], d_["mv"], d_["std"], d_["rstd"], d_["nmr"])

    def ln_part1a(T, src, src_res):
        n = T["name"]
        stats, mv, std, rstd, nmr = T["stats"], T["mv"], T["std"], T["rstd"], T["nmr"]

        def f_stats(e):
            e.bn_stats(out=stats[:, 0, :], in_=src[:, 0:512])
            return e.bn_stats(out=stats[:, 1, :], in_=src[:, 512:1024])
        A("dve", f_stats, r=list(src_res), w=[n + "st"])
        A("dve", lambda e: e.bn_aggr(out=mv[:], in_=stats[:]), r=[n + "st"], w=[n + "mv"])
        A("act", lambda e: e.activation(out=std[:], in_=mv[:, 1:2], func=AF.Ln, bias=epsln[:], scale=1.0), r=[n + "mv", "epsln"], w=[n + "sd"])
        A("act", lambda e: e.activation(out=rstd[:], in_=std[:], func=AF.Exp, scale=-0.5), r=[n + "sd"], w=[n + "rs"])
        A("dve", lambda e: e.scalar_tensor_tensor(out=nmr[:], in0=mv[:, 0:1], scalar=-1.0, in1=rstd[:], op0=ALU.mult, op1=ALU.mult), r=[n + "mv", n + "rs"], w=[n + "nm"])

    def ln_part1b(T, src, src_res, xn, xn_res):
        n = T["name"]
        rstd, nmr = T["rstd"], T["nmr"]
        A("act", lambda e: e.activation(out=xn, in_=src, func=AF.Identity, bias=nmr[:], scale=rstd[:]), r=list(src_res) + [n + "nm", n + "rs"], w=list(xn_res))

    def ln_part2(xn, xn_res, dst, dst_res, g_bc, b_bc, gres, bres):
        A("pool", lambda e: e.tensor_tensor(out=xn, in0=xn, in1=g_bc[:], op=ALU.mult), r=list(xn_res) + [gres], w=list(xn_res))
        A("dve", lambda e: e.tensor_tensor(out=dst, in0=xn, in1=b_bc[:], op=ALU.add), r=list(xn_res) + [bres], w=list(dst_res))

    epsln = ar.alloc("epsln", [128, 1], F32)
    epsrms = ar.alloc("epsrms", [128, 1], F32)
    A("pool", lambda e: e.memset(epsln[:], LN_EPS), w=["epsln"])
    A("pool", lambda e: e.memset(epsrms[:], RMS_EPS), w=["epsrms"])

    wq_f = ar.alloc("wq_f", [128, 3, 768], F32)
    wkv_f = ar.alloc("wkv_f", [128, 2, 1024], F32)
    qg = ar.alloc("qg", [128, 3], F32)
    kvg = ar.alloc("kvg", [128, 2], F32)
    Wq2 = ar.alloc("Wq2", [128, 3, 8, 128], BF16)
    Wk2 = ar.alloc("Wk2", [128, 2, 8, 64], BF16)
    Wv = ar.alloc("Wv", [128, 2, 8, 64], BF16)
    DMA("sp", wq_f[:], wqb_d.rearrange("(k p) f -> p k f", p=128), w=["wq_f"])
    DMA("sp", wkv_f[:], wkvb_d.rearrange("(k p) f -> p k f", p=128), w=["wkv_f"])
    DMA("sp", qg[:], qg_d, w=["qg"])
    DMA("sp", kvg[:], kvg_d, w=["kvg"])
    for k in range(3):
        src = wq_f[:, k, :].rearrange("p (h c) -> p h c", c=96)
        sc = qg[:, k:k + 1]
        A("dve", lambda e, src=src, sc=sc, k=k: e.tensor_scalar(out=Wq2[:, k, :, 64:96], in0=src[:, :, 64:96], scalar1=sc, scalar2=None, op0=ALU.mult),
          r=["wq_f", "qg"], w=["Wq2"])
        A("dve", lambda e, src=src, sc=sc, k=k: e.tensor_scalar(out=Wq2[:, k, :, 0:64], in0=src[:, :, 0:64], scalar1=sc, scalar2=None, op0=ALU.mult),
          r=["wq_f", "qg"], w=["Wq2"])
        A("dve", lambda e, src=src, sc=sc, k=k: e.tensor_scalar(out=Wq2[:, k, :, 96:112], in0=src[:, :, 80:96], scalar1=sc, scalar2=-1.0, op0=ALU.mult, op1=ALU.mult),
          r=["wq_f", "qg"], w=["Wq2"])
        A("dve", lambda e, src=src, sc=sc, k=k: e.tensor_scalar(out=Wq2[:, k, :, 112:128], in0=src[:, :, 64:80], scalar1=sc, scalar2=None, op0=ALU.mult),
          r=["wq_f", "qg"], w=["Wq2"])
    for k in range(2):
        src = wkv_f[:, k, :].rearrange("p (h c) -> p h c", c=128)
        sc = kvg[:, k:k + 1]
        A("dve", lambda e, src=src, sc=sc, k=k: e.tensor_scalar(out=Wk2[:, k, :, :], in0=src[:, :, 0:64], scalar1=sc, scalar2=None, op0=ALU.mult),
          r=["wkv_f", "kvg"], w=["Wk2"])
        A("dve", lambda e, src=src, sc=sc, k=k: e.tensor_scalar(out=Wv[:, k, :, :], in0=src[:, :, 64:128], scalar1=sc, scalar2=None, op0=ALU.mult),
          r=["wkv_f", "kvg"], w=["Wv"])

    cosT = ar.alloc("cosT", [96, S], F32)
    sinT = ar.alloc("sinT", [96, S], F32)
    pos_i = ar.alloc("pos_i", [128, 512], I32)
    ang = ar.alloc("ang", [128, 512], F32)
    rt0 = ar.alloc("rt0", [128, 512], F32)
    rt1 = ar.alloc("rt1", [128, 512], F32)
    rti = ar.alloc("rti", [128, 512], I32)
    rsc = [ar.alloc("rsc", [128, 512], F32) for _ in range(2)]
    invf = ar.alloc("invf", [128, 1], F32)
    DMA("sp", pos_i[:], pos_d, w=["pos_i"])
    DMA("sp", invf[:], c_invf_d, w=["invf"])
    A("dve", lambda e: e.tensor_copy(out=ang[:], in_=pos_i[:]), r=["pos_i"], w=["ang"])
    A("dve", lambda e: e.tensor_scalar(out=ang[:], in0=ang[:], scalar1=invf[:], scalar2=None, op0=ALU.mult), r=["ang", "invf"], w=["ang"])

    def range_reduce_sin(dst, dres, shift, tmp, tres):
        A("dve", lambda e: e.tensor_scalar(out=rt0[:], in0=ang[:], scalar1=shift, scalar2=1.0 / TWO_PI, op0=ALU.add, op1=ALU.mult), r=["ang"], w=["rt0"])
        A("dve", lambda e: e.tensor_copy(out=rti[:], in_=rt0[:]), r=["rt0"], w=["rti"])
        A("dve", lambda e: e.tensor_copy(out=rt0[:], in_=rti[:]), r=["rti"], w=["rt0"])
        A("dve", lambda e: e.tensor_scalar(out=rt1[:], in0=ang[:], scalar1=shift, scalar2=None, op0=ALU.add), r=["ang"], w=["rt1"])
        A("dve", lambda e: e.scalar_tensor_tensor(out=rt1[:], in0=rt0[:], scalar=-TWO_PI, in1=rt1[:], op0=ALU.mult, op1=ALU.add), r=["rt0", "rt1"], w=["rt1"])
        A("dve", lambda e: e.tensor_scalar(out=rt0[:], in0=rt1[:], scalar1=PI, scalar2=None, op0=ALU.is_gt), r=["rt1"], w=["rt0"])
        A("dve", lambda e: e.scalar_tensor_tensor(out=rt1[:], in0=rt0[:], scalar=-TWO_PI, in1=rt1[:], op0=ALU.mult, op1=ALU.add), r=["rt0", "rt1"], w=["rt1"])
        A("dve", lambda e: e.tensor_scalar(out=rt0[:], in0=rt1[:], scalar1=-PI, scalar2=None, op0=ALU.is_lt), r=["rt1"], w=["rt0"])
        A("dve", lambda e: e.scalar_tensor_tensor(out=rt1[:], in0=rt0[:], scalar=TWO_PI, in1=rt1[:], op0=ALU.mult, op1=ALU.add), r=["rt0", "rt1"], w=["rt1"])
        A("dve", lambda e: e.tensor_scalar(out=rt1[:], in0=rt1[:], scalar1=-3.1415925, scalar2=3.1415925, op0=ALU.max, op1=ALU.min), r=["rt1"], w=["rt1"])
        A("act", lambda e: e.activation(out=tmp[:], in_=rt1[:], func=AF.Sin), r=["rt1"], w=[tres])
        for blk in range(4):
            DMA("sp", dst[64:96, blk * 512:(blk + 1) * 512], tmp[blk * 32:(blk + 1) * 32, :], r=[tres], w=[dres])

    range_reduce_sin(sinT, "sinT", 0.0, rsc[0], "rsc0")
    range_reduce_sin(cosT, "cosT", PI / 2.0, rsc[1], "rsc1")

    cw = ar.alloc("cw", [128, 4 * 31], F32)
    Dg = ar.alloc("Dg", [128, 4 * 31, 128], BF16)
    DMA("sp", cw[:], cw_d, w=["cw"])
    for m in range(4):
        A("dve", lambda e, m=m: e.tensor_tensor(out=Dg[:, m * 31:(m + 1) * 31, :], in0=identb[:].unsqueeze(1).to_broadcast([128, 31, 128]),
                                               in1=cw[:, m * 31:(m + 1) * 31].unsqueeze(2).to_broadcast([128, 31, 128]), op=ALU.mult),
          r=["identb", "cw"], w=["Dg%d" % m])

    hT = ar.alloc("hT", [128, 8, S], BF16)
    g0 = ar.alloc("g0", [128, D], F32)
    b0 = ar.alloc("b0", [128, D], F32)
    DMA("sp", g0[:], g0_d, w=["g0"])
    DMA("sp", b0[:], b0_d, w=["b0"])
    NR1 = 6
    xt = [ar.alloc("xt", [128, D], F32) for _ in range(NR1)]
    hb = [ar.alloc("hb", [128, D], BF16) for _ in range(NR1)]
    lnA = ln_small("lnA", NR1)

    def p1_s0(t):
        p = t % NR1
        DMA("sp", xt[p][:], x_d[t * 128:(t + 1) * 128, :], w=["xt%d" % p])

    def p1_s0b(t):
        pass

    def p1_s1(t):
        p = t % NR1
        ln_part1a(lnA[p], xt[p][:], ["xt%d" % p])

    def p1_s1b(t):
        p = t % NR1
        ln_part1b(lnA[p], xt[p][:], ["xt%d" % p], xt[p][:], ["xt%d" % p])

    def p1_s2(t):
        p = t % NR1
        ln_part2(xt[p][:], ["xt%d" % p], hb[p][:], ["hb%d" % p], g0, b0, "g0", "b0")

    def p1_s3(t):
        p = t % NR1
        pb_ = t % 2

        def f_tr(e, p=p, pb_=pb_):
            for k in range(8):
                i_ = e.transpose(out=bankb(pb_)[:, k * 128:(k + 1) * 128], in_=hb[p][:, k * 128:(k + 1) * 128], identity=identb[:])
            return i_
        A("pe", f_tr, r=["hb%d" % p, "identb"], w=[BK(pb_)])
        A("act", lambda e, pb_=pb_, t=t: e.activation(out=hT[:, :, t * 128:(t + 1) * 128], in_=bankb(pb_).rearrange("p (k c) -> p k c", c=128), func=AF.Copy),
          r=[BK(pb_)], w=["hT%d" % (t // 4)])
    swpipe(NT, [p1_s0, p1_s0b, p1_s1, p1_s1b, p1_s2, p1_s3])
    if debug and "hT" in debug:
        dump("hT", hT[:], [128, 8, S], BF16)
    sch.barrier()
    ar.release(wq_f, wkv_f, qg, kvg, pos_i, ang, rt0, rt1, rti, invf, *rsc, *xt, *hb, g0, b0)
    ln_small_free(lnA)
    ar.flush()

    convT = ar.alloc("convT", [128, 4, S], BF16)
    wc = ar.alloc("wc", [128, 8, 1024], BF16)
    hcp = ar.alloc("hcp", [128, 4, S + 30], BF16)
    cb = ar.alloc("cb", [128, 4], F32)
    clg = ar.alloc("clg", [128, 4], F32)
    clb = ar.alloc("clb", [128, 4], F32)
    sig = [ar.alloc("sig", [128, 512], F32) for _ in range(2)]
    DMA("pool", wc[:], w_in_d.rearrange("(k p) f -> p k f", p=128)[:, :, 672:1696], w=["wc"])
    DMA("sp", cb[:], cb_d, w=["cb"])
    DMA("sp", clg[:], clg_d, w=["clg"])
    DMA("sp", clb[:], clb_d, w=["clb"])
    A("pool", lambda e: e.memset(hcp[:, :, 0:15], 0.0), w=["hcp_lo"])
    A("pool", lambda e: e.memset(hcp[:, :, S + 15:S + 30], 0.0), w=["hcp_hi"])
    it = 0
    for b in range(4):
        for m in range(4):
            pa = (it % 2) * 2
            pg = pa + 1
            sp_ = it % 2
            it += 1

            def f_glu(e, m=m, b=b, pa=pa, pg=pg):
                for k in range(8):
                    e.matmul(bank(pa), lhsT=wc[:, k, m * 128:(m + 1) * 128], rhs=hT[:, k, b * 512:(b + 1) * 512], start=(k == 0), stop=(k == 7))
                for k in range(8):
                    i_ = e.matmul(bank(pg), lhsT=wc[:, k, 512 + m * 128:512 + (m + 1) * 128], rhs=hT[:, k, b * 512:(b + 1) * 512], start=(k == 0), stop=(k == 7))
                return i_
            A("pe", f_glu, r=["wc", "hT%d" % b], w=[BK(pa), BK(pg)])
            A("act", lambda e, pg=pg, sp_=sp_: e.activation(out=sig[sp_][:], in_=bank(pg), func=AF.Sigmoid), r=[BK(pg)], w=["sig%d" % sp_])
            A("dve", lambda e, pa=pa, sp_=sp_, m=m, b=b: e.tensor_tensor(out=hcp[:, m, 15 + b * 512:15 + (b + 1) * 512], in0=bank(pa), in1=sig[sp_][:], op=ALU.mult),
              r=[BK(pa), "sig%d" % sp_], w=["hcp%d_%d" % (m, b)])
    yc = [ar.alloc("yc", [128, 4, 512], F32) for _ in range(2)]
    ysq = [ar.alloc("ysq", [128, 512], F32) for _ in range(2)]
    cmean = [ar.alloc("cmean", [128, 512], F32) for _ in range(2)]
    cm2 = [ar.alloc("cm2", [128, 512], F32) for _ in range(2)]
    crstd = [ar.alloc("crstd", [128, 512], F32) for _ in range(2)]
    ctmp = [ar.alloc("ctmp", [128, 512], F32) for _ in range(2)]
    seq = [(b, m) for b in range(4) for m in range(4)]

    def conv_step(i):
        b, m = seq[i]
        pb = b % 2
        pc = i % 2
        rds = ["Dg%d" % m, "hcp%d_%d" % (m, b)]
        rds.append("hcp%d_%d" % (m, b - 1) if b > 0 else "hcp_lo")
        rds.append("hcp%d_%d" % (m, b + 1) if b < 3 else "hcp_hi")

        def f_conv(e, m=m, b=b, pc=pc):
            for k in range(31):
                i_ = e.matmul(bank(pc), lhsT=Dg[:, m * 31 + k, :], rhs=hcp[:, m, b * 512 + k:b * 512 + k + 512], start=(k == 0), stop=(k == 30))
            return i_
        A("pe", f_conv, r=rds, w=[BK(pc)])
        A("act", lambda e, m=m, pb=pb, pc=pc: e.activation(out=yc[pb][:, m, :], in_=bank(pc), func=AF.Identity, bias=cb[:, m:m + 1], scale=1.0),
          r=[BK(pc), "cb"], w=["yc%d_%d" % (pb, m)])
        A("act", lambda e, m=m, pc=pc: e.activation(out=ysq[pc][:], in_=bank(pc), func=AF.Square, bias=cb[:, m:m + 1], scale=1.0),
          r=[BK(pc), "cb"], w=["ysq%d" % pc])

    def stat_step(i):
        b, m = seq[i]
        pb = b % 2
        pc = i % 2
        s1 = 4 + pb * 2
        s2 = 5 + pb * 2

        def f_st(e, m=m, pb=pb, pc=pc, s1=s1, s2=s2):
            e.matmul(bank(s1), lhsT=onesf[:], rhs=yc[pb][:, m, :], start=(m == 0), stop=(m == 3))
            return e.matmul(bank(s2), lhsT=onesf[:], rhs=ysq[pc][:], start=(m == 0), stop=(m == 3))
        A("pe", f_st, r=["onesf", "yc%d_%d" % (pb, m), "ysq%d" % pc], w=[BK(s1), BK(s2)])

    def norm_block(b):
        pb = b % 2
        s1 = 4 + pb * 2
        s2 = 5 + pb * 2
        A("dve", lambda e, pb=pb, s1=s1: e.tensor_scalar(out=cmean[pb][:], in0=bank(s1), scalar1=1.0 / 512.0, scalar2=None, op0=ALU.mult), r=[BK(s1)], w=["cmean%d" % pb])
        A("dve", lambda e, pb=pb: e.tensor_tensor(out=cm2[pb][:], in0=cmean[pb][:], in1=cmean[pb][:], op=ALU.mult), r=["cmean%d" % pb], w=["cm2%d" % pb])
        A("dve", lambda e, pb=pb, s2=s2: e.scalar_tensor_tensor(out=cm2[pb][:], in0=bank(s2), scalar=1.0 / 512.0, in1=cm2[pb][:], op0=ALU.mult, op1=ALU.subtract),
          r=[BK(s2), "cm2%d" % pb], w=["cm2%d" % pb])
        A("act", lambda e, pb=pb: e.activation(out=crstd[pb][:], in_=cm2[pb][:], func=AF.Ln, bias=epsln[:], scale=1.0), r=["cm2%d" % pb, "epsln"], w=["crstd%d" % pb])
        A("act", lambda e, pb=pb: e.activation(out=crstd[pb][:], in_=crstd[pb][:], func=AF.Exp, scale=-0.5), r=["crstd%d" % pb], w=["crstd%d" % pb])
        for m in range(4):
            pc = m % 2
            A("dve", lambda e, pb=pb, m=m, pc=pc: e.tensor_tensor(out=ctmp[pc][:], in0=yc[pb][:, m, :], in1=cmean[pb][:], op=ALU.subtract),
              r=["yc%d_%d" % (pb, m), "cmean%d" % pb], w=["ctmp%d" % pc])
            A("pool", lambda e, pb=pb, pc=pc: e.tensor_tensor(out=ctmp[pc][:], in0=ctmp[pc][:], in1=crstd[pb][:], op=ALU.mult),
              r=["ctmp%d" % pc, "crstd%d" % pb], w=["ctmp%d" % pc])
            A("act", lambda e, m=m, b=b, pc=pc: e.activation(out=convT[:, m, b * 512:(b + 1) * 512], in_=ctmp[pc][:], func=AF.Silu, bias=clb[:, m:m + 1], scale=clg[:, m:m + 1]),
              r=["ctmp%d" % pc, "clg", "clb"], w=["convT%d" % b])

    for i in range(17):
        if i < 16:
            conv_step(i)
        if i >= 1:
            stat_step(i - 1)
        if i >= 6 and (i - 6) % 4 == 0:
            norm_block((i - 6) // 4)
    norm_block(3)
    if debug and "convT" in debug:
        dump("convT", convT[:], [128, 4, S], BF16)
    sch.barrier()
    ar.release(wc, hcp, cw, cb, clg, clb, Dg, *sig, *yc, *ysq, *cmean, *cm2, *crstd, *ctmp)
    ar.flush()

    qT = ar.alloc("qT", [128, 8, S], BF16)
    kT = ar.alloc("kT", [128, 8, S], BF16)
    vaug = ar.alloc("vaug", [128, NT, 8, 65], BF16)
    wl = ar.alloc("wl", [128, 8, 704], BF16)
    DMA("pool", wl[:, :, 0:672], w_in_d.rearrange("(k p) f -> p k f", p=128)[:, :, 0:672], w=["wl"])
    A("dve", lambda e: e.tensor_scalar(out=wl[:, :, 672:688], in0=wl[:, :, 656:672], scalar1=-1.0, scalar2=None, op0=ALU.mult), r=["wl"], w=["wl"])
    A("dve", lambda e: e.tensor_copy(out=wl[:, :, 688:704], in_=wl[:, :, 640:656]), r=["wl"], w=["wl"])
    A("pool", lambda e: e.memset(vaug[:, :, :, 64:65], 1.0), w=["vaug1"])
    cqb = ar.alloc("cqb", [128, 3, 512], BF16)
    ckvb = ar.alloc("ckvb", [128, 2, 512], BF16)
    cqn = [ar.alloc("cqn", [128, 3, 512], BF16) for _ in range(2)]
    ckvn = [ar.alloc("ckvn", [128, 2, 512], BF16) for _ in range(2)]
    sqq = [ar.alloc("sqq", [128, 512], F32) for _ in range(2)]
    rq = ar.alloc("rq", [128, 512], F32)
    rk = ar.alloc("rk", [128, 512], F32)
    t1 = ar.alloc("t1", [96, 512], F32)
    t2 = ar.alloc("t2", [96, 512], F32)
    sq_ctr = [0]

    def proj_chunk(b, col0, dst, dres, j, sbank, nj, jj):
        pj = sq_ctr[0] % 2
        sq_ctr[0] += 1

        def f_p(e, pj=pj, b=b, col0=col0):
            for k in range(8):
                i_ = e.matmul(bank(pj), lhsT=wl[:, k, col0:col0 + 128], rhs=hT[:, k, b * 512:(b + 1) * 512], start=(k == 0), stop=(k == 7))
            return i_
        A("pe", f_p, r=["wl", "hT%d" % b], w=[BK(pj)])
        A("act", lambda e, pj=pj, j=j: e.activation(out=dst[:, j, :], in_=bank(pj), func=AF.Copy), r=[BK(pj)], w=[dres + "%d" % j])
        A("act", lambda e, pj=pj: e.activation(out=sqq[pj][:], in_=bank(pj), func=AF.Square), r=[BK(pj)], w=["sqq%d" % pj])
        return lambda: A("pe", lambda e, pj=pj: e.matmul(bank(sbank), lhsT=onesf[:], rhs=sqq[pj][:], start=(jj == 0), stop=(jj == nj - 1)), r=["onesf", "sqq%d" % pj], w=[BK(sbank)])

    def rstd_ops(sbank, n, r_, rres):
        A("act", lambda e: e.activation(out=r_[:], in_=bank(sbank), func=AF.Ln, bias=epsrms[:], scale=1.0 / n), r=[BK(sbank), "epsrms"], w=[rres])
        A("act", lambda e: e.activation(out=r_[:], in_=r_[:], func=AF.Exp, scale=-0.5), r=[rres], w=[rres])

    def stage_P(b):
        pb = b % 2
        bs = slice(b * 512, (b + 1) * 512)
        pend = None
        for j in range(3):
            nxt = proj_chunk(b, j * 128, cqb, "cqb", j, 2, 3, j)
            if pend:
                pend()
            pend = nxt
        for j in range(2):
            nxt = proj_chunk(b, 384 + j * 128, ckvb, "ckvb", j, 3, 2, j)
            pend()
            pend = nxt

        def f_kr2(e, b=b):
            for k in range(8):
                e.matmul(bank(4)[64:96, :], lhsT=wl[:, k, 640:672], rhs=hT[:, k, b * 512:(b + 1) * 512], start=(k == 0), stop=(k == 7))
            for k in range(8):
                i_ = e.matmul(bank(5)[64:96, :], lhsT=wl[:, k, 672:704], rhs=hT[:, k, b * 512:(b + 1) * 512], start=(k == 0), stop=(k == 7))
            return i_
        A("pe", f_kr2, r=["wl", "hT%d" % b], w=[BK(4), BK(5)])
        pend()
        rstd_ops(2, 384.0, rq, "rq")
        rstd_ops(3, 256.0, rk, "rk")
        for j in range(3):
            A("dve", lambda e, j=j, pb=pb: e.tensor_tensor(out=cqn[pb][:, j, :], in0=cqb[:, j, :], in1=rq[:], op=ALU.mult), r=["cqb%d" % j, "rq"], w=["cqn%d_%d" % (pb, j)])
        for j in range(2):
            A("dve", lambda e, j=j, pb=pb: e.tensor_tensor(out=ckvn[pb][:, j, :], in0=ckvb[:, j, :], in1=rk[:], op=ALU.mult), r=["ckvb%d" % j, "rk"], w=["ckvn%d_%d" % (pb, j)])
        A("dve", lambda e, bs=bs: e.tensor_tensor(out=t1[64:96, :], in0=bank(4)[64:96, :], in1=cosT[64:96, bs], op=ALU.mult), r=[BK(4), "cosT"], w=["t1"])
        A("dve", lambda e, bs=bs: e.tensor_tensor(out=t2[64:96, :], in0=bank(5)[64:96, :], in1=sinT[64:96, bs], op=ALU.mult), r=[BK(5), "sinT"], w=["t2"])
        A("dve", lambda e, bs=bs: e.tensor_tensor(out=kT[64:96, 0, bs], in0=t1[64:96, :], in1=t2[64:96, :], op=ALU.add), r=["t1", "t2"], w=["kTr0"])
        for h in range(1, H):
            A("dve" if h % 2 else "act", (lambda e, bs=bs, h=h: e.tensor_copy(out=kT[64:96, h, bs], in_=kT[64:96, 0, bs])) if h % 2 else
              (lambda e, bs=bs, h=h: e.activation(out=kT[64:96, h, bs], in_=kT[64:96, 0, bs], func=AF.Copy)), r=["kTr0"], w=["kTr%d" % h])

    def stage_H(b):
        pb = b % 2
        bs = slice(b * 512, (b + 1) * 512)
        cq_r = ["cqn%d_%d" % (pb, j) for j in range(3)]
        ckv_r = ["ckvn%d_%d" % (pb, j) for j in range(2)]
        for h in range(H):
            pq = 6 + (h % 2)
            pr = 4 + (h % 2)

            def f_q(e, h=h, pq=pq, pr=pr, pb=pb):
                for k in range(3):
                    e.matmul(bank(pq)[0:96, :], lhsT=Wq2[:, k, h, 0:96], rhs=cqn[pb][:, k, :], start=(k == 0), stop=(k == 2))
                for k in range(3):
                    i_ = e.matmul(bank(pr)[64:96, :], lhsT=Wq2[:, k, h, 96:128], rhs=cqn[pb][:, k, :], start=(k == 0), stop=(k == 2))
                return i_
            A("pe", f_q, r=["Wq2"] + cq_r, w=[BK(pq), BK(pr)])
            A("act", lambda e, h=h, pq=pq, bs=bs: e.activation(out=qT[0:64, h, bs], in_=bank(pq)[0:64, :], func=AF.Copy), r=[BK(pq)], w=["qT%d_%d" % (h, b)])
            A("dve", lambda e, pq=pq, bs=bs: e.tensor_tensor(out=t1[64:96, :], in0=bank(pq)[64:96, :], in1=cosT[64:96, bs], op=ALU.mult), r=[BK(pq), "cosT"], w=["t1"])
            A("dve", lambda e, pr=pr, bs=bs: e.tensor_tensor(out=t2[64:96, :], in0=bank(pr)[64:96, :], in1=sinT[64:96, bs], op=ALU.mult), r=[BK(pr), "sinT"], w=["t2"])
            A("dve", lambda e, h=h, bs=bs: e.tensor_tensor(out=qT[64:96, h, bs], in0=t1[64:96, :], in1=t2[64:96, :], op=ALU.add), r=["t1", "t2"], w=["qTr%d_%d" % (h, b)])
        for h in range(H):
            pk = 6 + (h % 2)

            def f_k(e, h=h, pk=pk, pb=pb):
                for k in range(2):
                    i_ = e.matmul(bank(pk)[0:64, :], lhsT=Wk2[:, k, h, :], rhs=ckvn[pb][:, k, :], start=(k == 0), stop=(k == 1))
                return i_
            A("pe", f_k, r=["Wk2"] + ckv_r, w=[BK(pk)])
            A("act", lambda e, h=h, pk=pk, bs=bs: e.activation(out=kT[0:64, h, bs], in_=bank(pk)[0:64, :], func=AF.Copy), r=[BK(pk)], w=["kT%d_%d" % (h, b)])
        for tt in range(4):
            pv = 4 + (tt % 2)
            t = b * 4 + tt

            def f_v(e, tt=tt, pv=pv, pb=pb):
                for k in range(2):
                    i_ = e.matmul(bank(pv), lhsT=ckvn[pb][:, k, tt * 128:(tt + 1) * 128], rhs=Wv[:, k, :, :].rearrange("p h c -> p (h c)"), start=(k == 0), stop=(k == 1))
                return i_
            A("pe", f_v, r=["Wv"] + ckv_r, w=[BK(pv)])
            A("act", lambda e, pv=pv, t=t: e.activation(out=vaug[:, t, :, 0:64], in_=bank(pv).rearrange("p (h c) -> p h c", c=64), func=AF.Copy),
              r=[BK(pv)], w=["vaug%d" % t])

    stage_P(0)
    for b in range(4):
        if b + 1 < 4:
            stage_P(b + 1)
        stage_H(b)
    if debug and "qT" in debug:
        dump("qT", qT[:], [128, 8, S], BF16)
        dump("kT", kT[:], [128, 8, S], BF16)
        dump("vaug", vaug[:], [128, NT, 8, 65], BF16)
    sch.barrier()
    ar.release(hT, wl, cqb, ckvb, *cqn, *ckvn, *sqq, rq, rk, t1, t2, cosT, sinT, Wq2, Wk2, Wv)
    ar.flush()

    attn_tok = ar.alloc("attn_tok", [128, NT, 512], BF16)
    ptb = [ar.alloc("ptb", [128, 1024], BF16) for _ in range(3)]
    rec = [ar.alloc("rec", [128, 4], F32) for _ in range(2)]
    steps = []
    for h in range(H):
        for qb in range(4):
            for kp in range(8):
                steps.append((h, qb, kp))

    def emit_scores(i):
        h, qb, kp = steps[i]
        sp_ = i % 2

        def f_s(e, h=h, qb=qb, kp=kp, sp_=sp_):
            for half in range(2):
                kt = kp * 2 + half
                i_ = e.matmul(pst[sp_][:, half * 512:(half + 1) * 512], lhsT=kT[0:96, h, kt * 128:(kt + 1) * 128], rhs=qT[0:96, h, qb * 512:(qb + 1) * 512], start=True, stop=True)
            return i_
        rds = ["kT%d_%d" % (h, (kp * 2) // 4), "kTr%d" % h, "qT%d_%d" % (h, qb), "qTr%d_%d" % (h, qb)]
        A("pe", f_s, r=rds, w=[BK(2 * sp_), BK(2 * sp_ + 1)])
        pp = i % 3
        A("act", lambda e, sp_=sp_, pp=pp: e.activation(out=ptb[pp][:], in_=pst[sp_][:], func=AF.Exp, scale=SCALE), r=[BK(2 * sp_), BK(2 * sp_ + 1)], w=["ptb%d" % pp])

    def emit_pv(i):
        h, qb, kp = steps[i]
        pp = i % 3
        g = i // 8
        ob = 4 + (g % 2)
        O = bank(ob).rearrange("p (q c) -> p q c", c=128)

        def f_pv(e, h=h, kp=kp, pp=pp, O=O):
            for half in range(2):
                kt = kp * 2 + half
                for qt in range(4):
                    i_ = e.matmul(O[:, qt, 0:65], lhsT=ptb[pp][:, half * 512 + qt * 128:half * 512 + (qt + 1) * 128], rhs=vaug[:, kt, h, :],
                                  start=(kt == 0 and qt == 0), stop=(kt == 15), skip_group_check=True)
            return i_
        rds = ["ptb%d" % pp, "vaug1"] + ["vaug%d" % (kp * 2), "vaug%d" % (kp * 2 + 1)]
        A("pe", f_pv, r=rds, w=[BK(ob)])
        if kp == 7:
            pr_ = g % 2
            A("dve", lambda e, O=O, pr_=pr_: e.reciprocal(out=rec[pr_][:], in_=O[:, :, 64]), r=[BK(ob)], w=["rec%d" % pr_])
            for qt in range(4):
                t = qb * 4 + qt
                A("dve", lambda e, O=O, pr_=pr_, qt=qt, t=t, h=h: e.tensor_scalar(out=attn_tok[:, t, h * 64:(h + 1) * 64], in0=O[:, qt, 0:64], scalar1=rec[pr_][:, qt:qt + 1], scalar2=None, op0=ALU.mult),
                  r=[BK(ob), "rec%d" % pr_], w=["attn_tok%d" % t])

    nst = len(steps)
    emit_scores(0)
    for i in range(nst):
        if i + 1 < nst:
            emit_scores(i + 1)
        emit_pv(i)
    if debug and "attn_tok" in debug:
        dump("attn_tok", attn_tok[:], [128, NT, 512], BF16)
    sch.barrier()
    ar.release(qT, kT, vaug, *ptb, *rec)
    ar.flush()

    attnT = ar.alloc("attnT", [128, 4, S], BF16)
    for t in range(NT):
        p = t % 2

        def f_tr2(e, t=t, p=p):
            for c in range(4):
                i_ = e.transpose(out=bankb(p)[:, c * 128:(c + 1) * 128], in_=attn_tok[:, t, c * 128:(c + 1) * 128], identity=identb[:])
            return i_
        A("pe", f_tr2, r=["attn_tok%d" % t, "identb"], w=[BK(p)])
        A("act", lambda e, t=t, p=p: e.activation(out=attnT[:, :, t * 128:(t + 1) * 128], in_=bankb(p)[:, 0:512].rearrange("p (k c) -> p k c", c=128), func=AF.Copy),
          r=[BK(p)], w=["attnT%d" % t])
    sch.barrier()
    ar.release(attn_tok)
    ar.flush()

    macc = nc.dram_tensor("moe_acc", [S, D], F32).ap()
    h1b = ar.alloc("h1b", [128, NT, D], BF16)
    wo = ar.alloc("wo", [128, 8, D], BF16)
    wr = ar.alloc("wr", [128, 8, E], F32)
    wrg = ar.alloc("wrg", [128, 8, E], F32)
    g0 = ar.alloc("g0", [128, D], F32)
    b0 = ar.alloc("b0", [128, D], F32)
    g1 = ar.alloc("g1", [128, D], F32)
    b1 = ar.alloc("b1", [128, D], F32)
    g1pk = ar.alloc("g1pk", [128, 8], F32)
    b1pk = ar.alloc("b1pk", [128, 8], F32)
    brow = ar.alloc("brow", [1, E], F32)
    aff = ar.alloc("aff", [128, NT, E], F32)
    DMA("pool", wo[:], wo_d.rearrange("(k p) f -> p k f", p=128), w=["wo"])
    DMA("sp", wr[:], wr_d.rearrange("(k p) f -> p k f", p=128), w=["wr"])
    DMA("sp", g0[:], g0_d, w=["g0"])
    DMA("sp", b0[:], b0_d, w=["b0"])
    DMA("sp", g1[:], g1_d, w=["g1"])
    DMA("sp", b1[:], b1_d, w=["b1"])
    DMA("sp", g1pk[:], g1pk_d, w=["g1pk"])
    DMA("sp", b1pk[:], b1pk_d, w=["b1pk"])
    A("dve", lambda e: e.tensor_scalar(out=g0[:], in0=g0[:], scalar1=ALPHA, scalar2=None, op0=ALU.mult), r=["g0"], w=["g0"])
    A("dve", lambda e: e.tensor_scalar(out=b0[:], in0=b0[:], scalar1=ALPHA, scalar2=None, op0=ALU.mult), r=["b0"], w=["b0"])
    bh = ar.alloc("bh", [1, D], BF16)
    bl = ar.alloc("bl", [1, D], BF16)
    blf = ar.alloc("blf", [1, D], F32)
    A("dve", lambda e: e.tensor_copy(out=bh[:], in_=b0[0:1, :]), r=["b0"], w=["bh"])
    A("dve", lambda e: e.tensor_tensor(out=blf[:], in0=b0[0:1, :], in1=bh[:], op=ALU.subtract), r=["b0", "bh"], w=["blf"])
    A("dve", lambda e: e.tensor_copy(out=bl[:], in_=blf[:]), r=["blf"], w=["bl"])
    A("dve", lambda e: e.tensor_scalar(out=g1[:], in0=g1[:], scalar1=ALPHA, scalar2=None, op0=ALU.mult), r=["g1"], w=["g1"])
    A("dve", lambda e: e.tensor_scalar(out=b1[:], in0=b1[:], scalar1=ALPHA, scalar2=None, op0=ALU.mult), r=["b1"], w=["b1"])
    for k in range(8):
        A("dve", lambda e, k=k: e.tensor_scalar(out=wrg[:, k, :], in0=wr[:, k, :], scalar1=g1pk[:, k:k + 1], scalar2=None, op0=ALU.mult), r=["wr", "g1pk"], w=["wrg"])

    def f_brow(e):
        for k in range(8):
            i_ = e.matmul(bank(7)[0:1, 0:E], lhsT=b1pk[:, k:k + 1], rhs=wr[:, k, :], start=(k == 0), stop=(k == 7))
        return i_
    A("pe", f_brow, r=["b1pk", "wr"], w=[BK(7)])
    A("dve", lambda e: e.tensor_copy(out=brow[:], in_=bank(7)[0:1, 0:E]), r=[BK(7)], w=["brow"])
    NR4 = 6
    NRX = 7
    NRZ = 5
    lgb = [bank(6)[:, 0:E], bank(7)[:, 0:E]]
    xt = [ar.alloc("xt", [128, D], F32) for _ in range(NRX)]
    zt = [ar.alloc("zt", [128, D], F32) for _ in range(NRZ)]
    r1 = [ar.alloc("r1", [128, D], F32) for _ in range(2)]
    h1T = [ar.alloc("h1T", [128, D], F32) for _ in range(2)]
    smx = [ar.alloc("smx", [128, 1], F32) for _ in range(2)]
    ssum = [ar.alloc("ssum", [128, 1], F32) for _ in range(2)]
    sex = [ar.alloc("sex", [128, E], F32) for _ in range(2)]
    lnB = ln_small("lnB", NR4)
    lnC = ln_small("lnC", NR4)

    def p4_s0(t):
        p = t % NRX
        DMA("sp", xt[p][:], x_d[t * 128:(t + 1) * 128, :], w=["xt%d" % p])

    def p4_s0b(t):
        pass

    def p4_s1(t):
        p = t % NRX
        ln_part1a(lnB[t % NR4], xt[p][:], ["xt%d" % p])

    def p4_s1b(t):
        p = t % NRX
        ln_part1b(lnB[t % NR4], xt[p][:], ["xt%d" % p], xt[p][:], ["xt%d" % p])

    def p4_s2a(t):
        p = t % NRX
        A("pool", lambda e, p=p: e.tensor_tensor(out=xt[p][:], in0=xt[p][:], in1=g0[:], op=ALU.mult), r=["xt%d" % p, "g0"], w=["xt%d" % p])
        for half in range(2):
            pw = 2 * (t % 2) + half

            def f_wo(e, t=t, half=half, pw=pw):
                for k in range(8):
                    src = attnT[:, k, t * 128:(t + 1) * 128] if k < 4 else convT[:, k - 4, t * 128:(t + 1) * 128]
                    e.matmul(bank(pw), lhsT=src, rhs=wo[:, k, half * 512:(half + 1) * 512], start=(k == 0), stop=False)
                e.matmul(bank(pw), lhsT=onesb[0:1, :], rhs=bh[0:1, half * 512:(half + 1) * 512], start=False, stop=False)
                return e.matmul(bank(pw), lhsT=onesb[0:1, :], rhs=bl[0:1, half * 512:(half + 1) * 512], start=False, stop=True)
            A("pe", f_wo, r=["attnT%d" % t, "convT%d" % (t // 4), "wo", "onesb", "bh", "bl"], w=[BK(pw)])

    def p4_s2b(t):
        p = t % NRX
        pz = t % NRZ
        for half in range(2):
            pw = 2 * (t % 2) + half
            A("dve", lambda e, p=p, pz=pz, half=half, pw=pw: e.tensor_tensor(out=zt[pz][:, half * 512:(half + 1) * 512], in0=xt[p][:, half * 512:(half + 1) * 512], in1=bank(pw), op=ALU.add),
              r=["xt%d" % p, BK(pw)], w=["zt%d_%d" % (pz, half)])

    def p4_s3(t):
        p = t % NRZ
        ln_part1a(lnC[t % NR4], zt[p][:], ["zt%d_0" % p, "zt%d_1" % p])

    def p4_s3b(t):
        p = t % NRZ
        ln_part1b(lnC[t % NR4], zt[p][:], ["zt%d_0" % p, "zt%d_1" % p], zt[p][:], ["zt%d_0" % p, "zt%d_1" % p])

    def p4_s4(t):
        p = t % NRZ
        zr = ["zt%d_0" % p, "zt%d_1" % p]
        A("act", lambda e, t=t, p=p: e.activation(out=h1b[:, t, :], in_=zt[p][:], func=AF.Copy), r=zr, w=["h1b%d" % t])
        pt_ = 2

        def f_trf(e, p=p, pt_=pt_):
            for k in range(8):
                i_ = e.transpose(out=pst[pt_][:, k * 128:(k + 1) * 128], in_=zt[p][:, k * 128:(k + 1) * 128], identity=identf[:])
            return i_
        A("pe", f_trf, r=zr + ["identf"], w=[BK(2 * pt_), BK(2 * pt_ + 1)])
        pr_ = t % 2
        A("pool", lambda e, p=p, pr_=pr_: e.tensor_tensor(out=r1[pr_][:], in0=zt[p][:], in1=g1[:], op=ALU.mult), r=zr + ["g1"], w=["r1%d" % pr_])
        A("pool", lambda e, pr_=pr_: e.tensor_tensor(out=r1[pr_][:], in0=r1[pr_][:], in1=b1[:], op=ALU.add), r=["r1%d" % pr_, "b1"], w=["r1%d" % pr_])
        DMA("sp", macc[t * 128:(t + 1) * 128, :], r1[pr_][:], r=["r1%d" % pr_], w=["macc"])
        ph_ = t % 2
        A("act", lambda e, ph_=ph_, pt_=pt_: e.activation(out=h1T[ph_][:], in_=pst[pt_][:], func=AF.Copy), r=[BK(2 * pt_), BK(2 * pt_ + 1)], w=["h1T%d" % ph_])

    def p4_s5b(t):
        p = t % 2
        lg = lgb[p]

        def f_lg(e, p=p, lg=lg):
            for k in range(8):
                e.matmul(lg, lhsT=h1T[p][:, k * 128:(k + 1) * 128], rhs=wrg[:, k, :], start=(k == 0), stop=False)
            return e.matmul(lg, lhsT=onesf[0:1, :], rhs=brow[:], start=False, stop=True)
        A("pe", f_lg, r=["h1T%d" % p, "wrg", "brow", "onesf"], w=[BK(6 + p)])
        A("dve", lambda e, p=p, lg=lg: e.tensor_reduce(out=smx[p][:], in_=lg, axis=AX.X, op=ALU.max), r=[BK(6 + p)], w=["smx%d" % p])
        A("dve", lambda e, p=p: e.tensor_scalar(out=smx[p][:], in0=smx[p][:], scalar1=-1.0, scalar2=None, op0=ALU.mult), r=["smx%d" % p], w=["smx%d" % p])

    def p4_s5c(t):
        p = t % 2
        lg = lgb[p]
        A("act", lambda e, p=p, lg=lg: e.activation(out=sex[p][:], in_=lg, func=AF.Exp, bias=smx[p][:], scale=1.0),
          r=[BK(6 + p), "smx%d" % p], w=["sex%d" % p])
        A("dve", lambda e, p=p: e.tensor_reduce(out=ssum[p][:], in_=sex[p][:], axis=AX.X, op=ALU.add), r=["sex%d" % p], w=["ssum%d" % p])
        A("dve", lambda e, p=p: e.reciprocal(out=ssum[p][:], in_=ssum[p][:]), r=["ssum%d" % p], w=["ssum%d" % p])
        A("dve", lambda e, p=p, t=t: e.tensor_scalar(out=aff[:, t, :], in0=sex[p][:], scalar1=ssum[p][:], scalar2=None, op0=ALU.mult), r=["sex%d" % p, "ssum%d" % p], w=["aff%d" % t])
    swpipe(NT, [p4_s0, p4_s0b, p4_s1, p4_s1b, p4_s2a, p4_s2b, p4_s3, p4_s3b, p4_s4, p4_s5b, p4_s5c])
    if debug and "h1" in debug:
        dump("h1n", h1b[:], [128, NT, D], BF16)
        dump("aff", aff[:], [128, NT, E], F32)
    sch.barrier()
    ar.release(attnT, convT, wo, wr, wrg, g0, b0, g1, b1, brow, bh, bl, blf, *xt, *zt, *r1, *h1T, *smx, *ssum, *sex)
    ln_small_free(lnB)
    ln_small_free(lnC)
    ar.flush()

    wgb = [ar.alloc("wgb", [128, 8, FG], BF16) for _ in range(NWB)]
    wub = [ar.alloc("wub", [128, 8, FG], BF16) for _ in range(NWB)]
    wdb = [ar.alloc("wdb", [128, FG // 128, D], BF16) for _ in range(NWB)]
    NCH = FF // FG
    FPC = FG // 128

    def load_chunk(c):
        e_, fg = divmod(c, NCH)
        s = c % NWB
        DMA("pool", wgb[s][:], wg_d[e_].rearrange("(k p) f -> p k f", p=128)[:, :, fg * FG:(fg + 1) * FG], w=["wg%d" % s])
        DMA("pool", wub[s][:], wu_d[e_].rearrange("(k p) f -> p k f", p=128)[:, :, fg * FG:(fg + 1) * FG], w=["wu%d" % s])
        DMA("pool", wdb[s][:], wd_d[e_][fg * FG:(fg + 1) * FG, :].rearrange("(j p) d -> p j d", p=128), w=["wd%d" % s])

    for c in range(NWB):
        load_chunk(c)

    tp = ar.alloc("tp", [128, NT, 2], BF16)
    DMA("sp", tp[:].rearrange("p t c -> p (t c)"), c_tp_d, w=["tp"])
    A8 = ar.alloc("A8", [128, 256], F32)
    junk = ar.alloc("junk", [128, 256], F32)
    gmat = ar.alloc("gmat", [128, 128], F32)
    cand = ar.alloc("cand", [128, 1], F32)
    cnt = ar.alloc("cnt", [128, 1], F32)
    dlt = ar.alloc("dlt", [128, 1], F32)
    cnt2 = ar.alloc("cnt2", [128, 1], F32)
    thr = ar.alloc("thr", [128, 1], F32)
    mask8 = ar.alloc("mask8", [128, 256], BF16)
    mask_tok = ar.alloc("mask_tok", [128, NT * E], BF16)
    posm = ar.alloc("posm", [128, NT * E], F32)
    ahl = ar.alloc("ahl", [128, NT * E, 4], BF16)
    DMA("sp", gmat[:], c_gmat_d, w=["gmat"])

    affp = ar.alloc("affp", [128, 2, 128], F32)
    for h in range(2):
        A("dve", lambda e, h=h: e.tensor_copy(out=affp[:, h, :].rearrange("p (e g) -> p e g", g=8), in_=aff[:, h::2, :].rearrange("p g e -> p e g")),
          r=["aff%d" % t for t in range(NT)], w=["affp"])

    def f_a8(e):
        for h in range(2):
            i_ = e.transpose(out=bank(0)[:, h * 128:(h + 1) * 128], in_=affp[:, h, :], identity=identf[:])
        return i_
    A("pe", f_a8, r=["affp", "identf"], w=[BK(0)])
    A("dve", lambda e: e.tensor_copy(out=A8[:], in_=bank(0)[:, 0:256]), r=[BK(0)], w=["A8"])
    A("dve", lambda e: e.memset(thr[:], 0.0), w=["thr"])
    A("dve", lambda e: e.memset(cand[:], 0.5), w=["cand"])
    for i in range(1, NBIS + 1):
        step = 2.0 ** (-i)
        pb_ = 1 + (i % 2)
        A("dve", lambda e: e.tensor_scalar(out=junk[:], in0=A8[:], scalar1=cand[:], scalar2=None, op0=ALU.is_ge, op1=ALU.add, accum_out=cnt[:]),
          r=["A8", "cand"], w=["junk", "cnt"])
        A("dve", lambda e: e.tensor_copy(out=cnt2[:], in_=cnt[:]), r=["cnt"], w=["cnt2"])
        A("pe", lambda e, pb_=pb_: e.matmul(bank(pb_)[:, 0:1], lhsT=gmat[:], rhs=cnt2[:], start=True, stop=True), r=["gmat", "cnt2"], w=[BK(pb_)])
        A("dve", lambda e, pb_=pb_, step=step: e.tensor_scalar(out=dlt[:], in0=bank(pb_)[:, 0:1], scalar1=float(CAP) - 0.5, scalar2=step, op0=ALU.is_ge, op1=ALU.mult),
          r=[BK(pb_)], w=["dlt"])
        A("dve", lambda e: e.tensor_tensor(out=thr[:], in0=thr[:], in1=dlt[:], op=ALU.add), r=["thr", "dlt"], w=["thr"])
        A("dve", lambda e, step=step: e.tensor_scalar(out=cand[:], in0=thr[:], scalar1=step * 0.5, scalar2=None, op0=ALU.add), r=["thr"], w=["cand"])
    A("dve", lambda e: e.tensor_scalar(out=mask8[:], in0=A8[:], scalar1=thr[:], scalar2=None, op0=ALU.is_ge), r=["A8", "thr"], w=["mask8"])

    def f_mtok(e):
        for h in range(2):
            i_ = e.transpose(out=bankb(4)[:, h * 128:(h + 1) * 128], in_=mask8[:, h * 128:(h + 1) * 128], identity=identb[:])
        return i_
    A("pe", f_mtok, r=["mask8", "identb"], w=[BK(4)])
    mtv = mask_tok[:].rearrange("p (t e) -> p t e", e=E)
    for h in range(2):
        A("dve", lambda e, h=h: e.tensor_copy(out=mtv[:, h::2, :], in_=bankb(4)[:, h * 128:(h + 1) * 128].rearrange("p (e g) -> p g e", g=8)), r=[BK(4)], w=["mask_tok"])

    def f_pos(e):
        for t in range(NT):
            for t2_ in range(t + 1):
                lhs = ustr[:] if t2_ == t else onesb[:]
                i_ = e.matmul(bank(5)[:, t * 16:(t + 1) * 16], lhsT=lhs, rhs=mask_tok[:, t2_ * 16:(t2_ + 1) * 16], start=(t2_ == 0), stop=(t2_ == t))
        return i_
    A("pe", f_pos, r=["mask_tok", "ustr", "onesb"], w=[BK(5)])
    A("dve", lambda e: e.scalar_tensor_tensor(out=posm[:], in0=bank(5)[:, 0:NT * E], scalar=1.0, in1=mask_tok[:], op0=ALU.add, op1=ALU.mult), r=[BK(5), "mask_tok"], w=["posm"])
    A("dve", lambda e: e.tensor_scalar(out=posm[:], in0=posm[:], scalar1=-1.0, scalar2=None, op0=ALU.add), r=["posm"], w=["posm"])
    affv = aff[:].rearrange("p t e -> p (t e)")
    A("dve", lambda e: e.tensor_copy(out=ahl[:, :, 0], in_=affv), r=["aff%d" % t for t in range(NT)], w=["ahl"])
    A("dve", lambda e: e.tensor_tensor(out=ahl[:, :, 1], in0=affv, in1=ahl[:, :, 0], op=ALU.subtract), r=["ahl"] + ["aff%d" % t for t in range(NT)], w=["ahl"])
    A("dve", lambda e: e.tensor_copy(out=ahl[:].rearrange("p (t e) c -> p t e c", e=E)[:, :, :, 2:4], in_=tp[:].unsqueeze(2).to_broadcast([128, NT, E, 2])), r=["tp", "ahl"], w=["ahl"])
    if debug and "posm" in debug:
        dump("posm", posm[:], [128, NT * E], F32)
    sch.barrier()
    ar.release(A8, junk, gmat, cand, thr, cnt, cnt2, dlt, mask8, mask_tok, tp, affp)
    ar.flush()

    Pm = [ar.alloc("Pm", [128, NT, CAP], BF16) for _ in range(2)]
    gate = [ar.alloc("gate", [128, 2], F32) for _ in range(2)]
    gi = [ar.alloc("gi", [128, 8], F32) for _ in range(2)]
    idxf = [ar.alloc("idxf", [128, 2], F32) for _ in range(2)]
    idxi = [ar.alloc("idxi", [128, 2], I32) for _ in range(2)]
    xgT = ar.alloc("xgT", [128, 8, CAP], BF16)
    yg = [ar.alloc("yg", [128, 2, D], F32) for _ in range(2)]
    sa = [ar.alloc("sa", [128, CAP], F32) for _ in range(2)]
    actT = [ar.alloc("actT", [128, CAP], BF16) for _ in range(4)]
    misc_ctr = [0]

    def misc_bank():
        b_ = 6 + (misc_ctr[0] % 2)
        misc_ctr[0] += 1
        return b_

    def prep_expert(e_):
        pe_ = e_ % 2
        for t in range(NT):
            A("dve", lambda e, t=t, pe_=pe_, e_=e_: e.tensor_scalar(out=Pm[pe_][:, t, :], in0=iota_c[:], scalar1=posm[:, t * E + e_:t * E + e_ + 1], scalar2=None, op0=ALU.is_equal),
              r=["iota_c", "posm"], w=["Pm%d_%d" % (pe_, t)])
        mb = misc_bank()

        def f_gate(e, mb=mb, pe_=pe_, e_=e_):
            for ct in range(2):
                for t in range(NT):
                    i_ = e.matmul(bank(mb)[:, ct * 4:ct * 4 + 4], lhsT=Pm[pe_][:, t, ct * 128:(ct + 1) * 128], rhs=ahl[:, t * E + e_, :], start=(t == 0), stop=(t == NT - 1))
            return i_
        A("pe", f_gate, r=["ahl"] + ["Pm%d_%d" % (pe_, t) for t in range(NT)], w=[BK(mb)])
        A("dve", lambda e, mb=mb, pe_=pe_: e.tensor_copy(out=gi[pe_][:], in_=bank(mb)[:, 0:8]), r=[BK(mb)], w=["gi%d" % pe_])
        giv = gi[pe_][:].rearrange("p (c f) -> p c f", f=4)
        A("dve", lambda e, pe_=pe_, giv=giv: e.tensor_tensor(out=gate[pe_][:], in0=giv[:, :, 0], in1=giv[:, :, 1], op=ALU.add), r=["gi%d" % pe_], w=["gate%d" % pe_])
        A("dve", lambda e, pe_=pe_, giv=giv: e.scalar_tensor_tensor(out=idxf[pe_][:], in0=giv[:, :, 2], scalar=128.0, in1=giv[:, :, 3], op0=ALU.mult, op1=ALU.add),
          r=["gi%d" % pe_], w=["idxf%d" % pe_])
        A("dve", lambda e, pe_=pe_: e.tensor_copy(out=idxi[pe_][:], in_=idxf[pe_][:]), r=["idxf%d" % pe_], w=["idxi%d" % pe_])

    def gather_expert(e_):
        pe_ = e_ % 2
        for dk in range(8):
            mb = misc_bank()

            def f_g(e, mb=mb, dk=dk, pe_=pe_):
                for t in range(NT):
                    i_ = e.matmul(bank(mb)[:, 0:CAP], lhsT=h1b[:, t, dk * 128:(dk + 1) * 128], rhs=Pm[pe_][:, t, :], start=(t == 0), stop=(t == NT - 1))
                return i_
            A("pe", f_g, r=["h1b%d" % t for t in range(NT)] + ["Pm%d_%d" % (pe_, t) for t in range(NT)], w=[BK(mb)])
            A("act", lambda e, mb=mb, dk=dk: e.activation(out=xgT[:, dk, :], in_=bank(mb)[:, 0:CAP], func=AF.Identity, bias=b1pk[:, dk:dk + 1], scale=g1pk[:, dk:dk + 1]),
              r=[BK(mb), "g1pk", "b1pk"], w=["xgT%d" % dk])

    def gu(e_, ft):
        c = e_ * NCH + ft // FPC
        s = c % NWB
        j = ft % FPC
        gb = 4 + (ft % 2)

        def f_gu(e, s=s, j=j, gb=gb):
            for k in range(8):
                e.matmul(bank(gb)[:, 0:CAP], lhsT=wgb[s][:, k, j * 128:(j + 1) * 128], rhs=xgT[:, k, :], start=(k == 0), stop=(k == 7))
            for k in range(8):
                i_ = e.matmul(bank(gb)[:, CAP:2 * CAP], lhsT=wub[s][:, k, j * 128:(j + 1) * 128], rhs=xgT[:, k, :], start=(k == 0), stop=(k == 7))
            return i_
        A("pe", f_gu, r=["wg%d" % s, "wu%d" % s] + ["xgT%d" % dk for dk in range(8)], w=[BK(gb)])
        ps_ = ft % 2
        pa_ = ft % 4
        A("act", lambda e, gb=gb, ps_=ps_: e.activation(out=sa[ps_][:], in_=bank(gb)[:, 0:CAP], func=AF.Silu), r=[BK(gb)], w=["sa%d" % ps_])
        A("dve", lambda e, gb=gb, ps_=ps_, pa_=pa_: e.tensor_tensor(out=actT[pa_][:], in0=bank(gb)[:, CAP:2 * CAP], in1=sa[ps_][:], op=ALU.mult), r=[BK(gb), "sa%d" % ps_], w=["actT%d" % pa_])

    def down(e_, ft):
        c = e_ * NCH + ft // FPC
        s = c % NWB
        j = ft % FPC
        pa_ = ft % 4

        def f_d(e, s=s, j=j, pa_=pa_, ft=ft):
            for ct in range(2):
                for dh in range(2):
                    i_ = e.matmul(pst[ct][:, dh * 512:(dh + 1) * 512], lhsT=actT[pa_][:, ct * 128:(ct + 1) * 128], rhs=wdb[s][:, j, dh * 512:(dh + 1) * 512], start=(ft == 0), stop=(ft == NT - 1))
            return i_
        A("pe", f_d, r=["actT%d" % pa_, "wd%d" % s], w=[BK(0), BK(1), BK(2), BK(3)])
        if j == FPC - 1 and c + NWB < E * NCH:
            load_chunk(c + NWB)

    def yevac_scatter(e_):
        pe_ = e_ % 2
        for ct in range(2):
            A("act", lambda e, ct=ct, pe_=pe_: e.activation(out=yg[pe_][:, ct, :], in_=pst[ct][:], func=AF.Copy, scale=gate[pe_][:, ct:ct + 1]),
              r=[BK(2 * ct), BK(2 * ct + 1), "gate%d" % pe_], w=["yg%d_%d" % (pe_, ct)])
        for ct in range(2):
            A("pool", lambda e, ct=ct, pe_=pe_: e.indirect_dma_start(out=macc, out_offset=bass.IndirectOffsetOnAxis(ap=idxi[pe_][:, ct:ct + 1], axis=0),
                                                                     in_=yg[pe_][:, ct, :], in_offset=None, bounds_check=S - 1, oob_is_err=True, compute_op=ALU.add),
              r=["yg%d_%d" % (pe_, ct), "idxi%d" % pe_, "macc"], w=["macc"], dma=True)

    SKEW = 2
    prep_expert(0)
    gather_expert(0)
    for e_ in range(E):
        for ft in range(NT):
            gu(e_, ft)
            if ft >= SKEW:
                down(e_, ft - SKEW)
        if e_ + 1 < E:
            prep_expert(e_ + 1)
        for ft in range(NT - SKEW, NT):
            down(e_, ft)
        yevac_scatter(e_)
        if e_ + 1 < E:
            gather_expert(e_ + 1)
    sch.barrier()
    ar.release(h1b, *Pm, *gate, *gi, *idxf, *idxi, xgT, *yg, *sa, *actT, *wgb, *wub, *wdb, posm, ahl, aff, g1pk, b1pk)
    ar.flush()

    g2 = ar.alloc("g2", [128, D], F32)
    b2 = ar.alloc("b2", [128, D], F32)
    DMA("sp", g2[:], g2_d, w=["g2"])
    DMA("sp", b2[:], b2_d, w=["b2"])
    NR7 = 6
    rt = [ar.alloc("rt", [128, D], F32) for _ in range(NR7)]
    ot = [ar.alloc("ot", [128, D], F32) for _ in range(NR7)]
    lnD = ln_small("lnD", NR7)

    def p7_s0(t):
        p = t % NR7
        DMA("sp", rt[p][:], macc[t * 128:(t + 1) * 128, :], r=["macc"], w=["rt%d" % p])

    def p7_s0b(t):
        pass

    def p7_s1(t):
        p = t % NR7
        ln_part1a(lnD[p], rt[p][:], ["rt%d" % p])

    def p7_s1b(t):
        p = t % NR7
        ln_part1b(lnD[p], rt[p][:], ["rt%d" % p], rt[p][:], ["rt%d" % p])

    def p7_s2(t):
        p = t % NR7
        ln_part2(rt[p][:], ["rt%d" % p], ot[p][:], ["ot%d" % p], g2, b2, "g2", "b2")
        DMA("sp", out_d[t * 128:(t + 1) * 128, :], ot[p][:], r=["ot%d" % p])
    swpipe(NT, [p7_s0, p7_s0b, p7_s1, p7_s1b, p7_s2])
    emit_program(sch, nc)
    return nc, dbg_outs


def _consts():
    bf = ml_dtypes.bfloat16
    ident = np.eye(128, dtype=np.float32)
    iota = np.broadcast_to(np.arange(256, dtype=np.float32)[None, :], (128, 256)).copy()
    iotap = np.stack([np.arange(128, dtype=np.float32), np.arange(128, dtype=np.float32) + 128.0], axis=1)
    ustr = np.triu(np.ones((128, 128), dtype=np.float32), k=1)
    half = 16
    invf = (np.float32(10000.0) ** (-np.arange(half, dtype=np.float32) / np.float32(half))).astype(np.float32)
    invf = np.concatenate([invf] * 8).reshape(128, 1)
    tp = np.zeros((128, 16, 2), dtype=np.float32)
    tp[:, :, 0] = np.arange(16, dtype=np.float32)[None, :]
    tp[:, :, 1] = np.arange(128, dtype=np.float32)[:, None]
    return {
        "c_identf": ident, "c_identb": ident.astype(bf), "c_iota": iota, "c_iotap": np.ascontiguousarray(iotap),
        "c_ustr": ustr.astype(bf), "c_invf": invf, "c_tp": tp.reshape(128, 32).astype(bf), "c_gmat": np.kron(np.eye(16, dtype=np.float32), np.ones((8, 8), dtype=np.float32)),
    }


def _bc(v):
    return np.ascontiguousarray(np.broadcast_to(np.asarray(v, dtype=np.float32).reshape(1, -1), (128, v.size)))


def _pk(v, k):
    return np.ascontiguousarray(np.asarray(v, dtype=np.float32).reshape(k, 128).T)


_CACHE = {}


def make_in_maps(inputs, cores):
    f = lambda a: np.ascontiguousarray(np.asarray(a))
    shared = {
        "emb_ln_g": _bc(f(inputs["emb_ln_g"])), "emb_ln_b": _bc(f(inputs["emb_ln_b"])),
        "w_in": f(inputs["w_in"])[0], "q_norm_g": _pk(f(inputs["q_norm_g"])[0], 3), "w_qb": f(inputs["w_qb"])[0],
        "kv_norm_g": _pk(f(inputs["kv_norm_g"])[0], 2), "w_kvb": f(inputs["w_kvb"])[0],
        "conv_w": np.ascontiguousarray(f(inputs["conv_w"])[0].reshape(31, 4, 128).transpose(2, 1, 0).reshape(128, 4 * 31)),
        "conv_b": _pk(f(inputs["conv_b"])[0], 4), "conv_ln_g": _pk(f(inputs["conv_ln_g"])[0], 4), "conv_ln_b": _pk(f(inputs["conv_ln_b"])[0], 4),
        "w_o": f(inputs["w_o"])[0], "ln1_g": _bc(f(inputs["ln1_g"])[0]), "ln1_b": _bc(f(inputs["ln1_b"])[0]),
        "ln1_g_pk": _pk(f(inputs["ln1_g"])[0], 8), "ln1_b_pk": _pk(f(inputs["ln1_b"])[0], 8),
        "w_router": f(inputs["w_router"])[0], "w_gate": f(inputs["w_gate"])[0], "w_up": f(inputs["w_up"])[0], "w_down": f(inputs["w_down"])[0],
        "ln2_g": _bc(f(inputs["ln2_g"])[0]), "ln2_b": _bc(f(inputs["ln2_b"])[0]),
    }
    shared.update(_consts())
    x = f(inputs["x"])
    pos = f(inputs["positions"]).astype(np.int32)
    maps = []
    for c in cores:
        m = dict(shared)
        m["x"] = np.ascontiguousarray(x[c])
        m["pos"] = np.ascontiguousarray(np.broadcast_to(pos[c].reshape(4, 1, 512), (4, 32, 512)).reshape(128, 512))
        maps.append(m)
    return maps


def kernel(**inputs):
    if "nc" not in _CACHE:
        _CACHE["nc"] = build()[0]
    nc = _CACHE["nc"]
    cores = list(range(8))
    in_maps = make_in_maps(inputs, cores)
    res = run_bass_kernel_spmd(nc, in_maps, core_ids=cores)
    out = np.stack([np.asarray(r["out"]) for r in res.results], axis=0)
    return out.astype(np.float32)
```

```python
import numpy as np
import ml_dtypes
import concourse.bass as bass
import concourse.mybir as mybir
from concourse.bass_utils import run_bass_kernel_spmd

F32 = mybir.dt.float32
BF16 = mybir.dt.bfloat16
I32 = mybir.dt.int32
AF = mybir.ActivationFunctionType
ALU = mybir.AluOpType
AX = mybir.AxisListType

S = 2048
D = 1024
NT = 16
H = 8
E = 16
CAP = 256
FF = 2048
ALPHA = float(2.0 ** 0.25)
LN_EPS = 1e-5
RMS_EPS = 1e-6
SCALE = float(96.0 ** -0.5)
PI = float(np.pi)
TWO_PI = float(2.0 * np.pi)
NBIS = 28
FG = 512
NWB = 5
NPC = 7
ORDER = [7, 0, 8, 1, 9, 2, 10, 3, 11, 4, 12, 5, 13, 6, 14, 15]


class _Op:
    __slots__ = ("eng", "fn", "deps", "sig", "sigval", "is_dma", "dsem", "dval")


class Sched:
    ENG = ("pe", "act", "dve", "pool", "sp")

    def __init__(self, nc, n_dma_sems=42):
        self.nc = nc
        self.ops = []
        self.last_w = {}
        self.readers = {}
        self.n_dma = n_dma_sems
        self.dma_rr = 0
        self.dma_rrq = {}
        self.dma_last = [None] * n_dma_sems
        self.dma_count = [0] * n_dma_sems
        self.eng_last = {e: None for e in self.ENG}
        self.out_dmas = []
        self.capture = None
        self.precast_mode = False

    def add(self, eng, fn, r=(), w=(), dma=False):
        if self.capture is not None:
            self.capture.append((eng, fn, tuple(r), tuple(w), dma))
            return None
        op = _Op()
        op.eng = eng
        op.fn = fn
        op.deps = {}
        op.sig = False
        op.sigval = 0
        op.is_dma = dma
        for x in r:
            wr = self.last_w.get(x)
            if wr is not None:
                op.deps[wr] = "raw"
        for x in w:
            wr = self.last_w.get(x)
            if wr is not None and wr not in op.deps:
                op.deps[wr] = "waw"
            for rd in self.readers.get(x, ()):
                if rd is not op and rd not in op.deps:
                    op.deps[rd] = "war"
        for x in r:
            self.readers.setdefault(x, []).append(op)
        for x in w:
            self.last_w[x] = op
            self.readers[x] = []
        if dma:
            third = self.n_dma // 3
            qk = "pc" if self.precast_mode else eng
            base = {"sp": 0, "pool": third, "pc": 2 * third}.get(qk, 0)
            k = base + self.dma_rrq.get(qk, 0)
            self.dma_rrq[qk] = (self.dma_rrq.get(qk, 0) + 1) % third
            prev = self.dma_last[k]
            if prev is not None:
                op.deps[prev] = "raw"
            self.dma_count[k] += 1
            op.dsem = k
            op.dval = 16 * self.dma_count[k]
            self.dma_last[k] = op
        else:
            self.eng_last[eng] = op
        self.ops.append(op)
        return op

    def barrier(self):
        lasts = [o for o in self.eng_last.values() if o is not None]
        third = self.n_dma // 3
        lasts += [o for k_, o in enumerate(self.dma_last) if o is not None and k_ < 2 * third]
        for e in self.ENG:
            op = _Op()
            op.eng = e
            op.fn = None
            op.deps = {o: "raw" for o in lasts}
            op.sig = False
            op.sigval = 0
            op.is_dma = False
            self.ops.append(op)


def _mk_sems(nc, names):
    import contextlib
    st = contextlib.ExitStack()
    sems = [st.enter_context(nc.semaphore(n)) for n in names]
    return st, sems


def emit_program(sch, nc):
    engobj = {"pe": nc.tensor, "act": nc.scalar, "dve": nc.vector, "pool": nc.gpsimd, "sp": nc.sync}

    def skip(op, d, kind):
        if d.is_dma or op.is_dma:
            return False
        if d.eng != op.eng:
            return False
        if op.fn is None:
            return True
        if op.eng == "pe":
            return True
        return False

    for op in sch.ops:
        for d, kind in op.deps.items():
            if d.is_dma or skip(op, d, kind):
                continue
            d.sig = True
    cnt = {e: 0 for e in Sched.ENG}
    for op in sch.ops:
        if op.fn is not None and (not op.is_dma) and op.sig:
            cnt[op.eng] += 1
            op.sigval = cnt[op.eng]
    st, sems = _mk_sems(nc, ["se_" + e for e in Sched.ENG] + ["sd_%d" % i for i in range(sch.n_dma)])
    esem = {e: sems[i] for i, e in enumerate(Sched.ENG)}
    dsem = sems[len(Sched.ENG):]
    waited = {e: {} for e in Sched.ENG}
    with st:
        for op in sch.ops:
            eo = engobj[op.eng]
            need = {}
            for d, kind in op.deps.items():
                if skip(op, d, kind):
                    continue
                if d.is_dma:
                    key = ("d", d.dsem)
                    val = d.dval
                else:
                    key = ("e", d.eng)
                    val = d.sigval
                if need.get(key, 0) < val:
                    need[key] = val
            for key, val in need.items():
                if waited[op.eng].get(key, 0) >= val:
                    continue
                waited[op.eng][key] = val
                sem = dsem[key[1]] if key[0] == "d" else esem[key[1]]
                eo.wait_ge(sem, val)
            if op.fn is None:
                continue
            inst = op.fn(eo)
            if op.is_dma:
                inst.then_inc(dsem[op.dsem], 16)
            elif op.sig:
                inst.then_inc(esem[op.eng], 1)
        for k in range(sch.n_dma):
            if sch.dma_count[k] > 0:
                nc.sync.wait_ge(dsem[k], 16 * sch.dma_count[k])


class Arena:
    LO = 16512
    HI = 229344

    def __init__(self, nc):
        self.nc = nc
        self.free = [(self.LO, self.HI)]
        self.pending = []
        self.n = 0
        self.live = {}

    def alloc(self, name, shape, dtype):
        esz = 2 if dtype == BF16 else 4
        nbytes = esz
        for d_ in shape[1:]:
            nbytes *= d_
        nbytes = (nbytes + 63) // 64 * 64
        for i, (lo, hi) in enumerate(self.free):
            if hi - lo >= nbytes:
                self.free[i] = (lo + nbytes, hi)
                self.n += 1
                t = self.nc.alloc_sbuf_tensor_at("%s_%d" % (name, self.n), list(shape), dtype, offset=lo)
                self.live[id(t)] = (lo, lo + nbytes)
                return t
        raise RuntimeError("SBUF arena out of memory for %s (%d bytes) free=%s" % (name, nbytes, self.free))

    def release(self, *tiles):
        for t in tiles:
            self.pending.append(self.live.pop(id(t)))

    def flush(self):
        segs = sorted(self.free + self.pending)
        self.pending = []
        out = []
        for lo, hi in segs:
            if lo == hi:
                continue
            if out and out[-1][1] == lo:
                out[-1] = (out[-1][0], hi)
            else:
                out.append((lo, hi))
        self.free = out


def build(debug=None):
    nc = bass.Bass("TRN2", target_bir_lowering=False)
    sch = Sched(nc)
    ar = Arena(nc)
    A = sch.add
    dbg_outs = []

    def din(name, shape, dtype=F32):
        return nc.dram_tensor(name, list(shape), dtype, kind="ExternalInput").ap()

    x_d = din("x", [S, D])
    pos_d = din("pos", [128, 512], I32)
    g0_d = din("emb_ln_g", [128, D])
    b0_d = din("emb_ln_b", [128, D])
    w_in_d = din("w_in", [D, 1696])
    qg_d = din("q_norm_g", [128, 3])
    wqb_d = din("w_qb", [384, 768])
    kvg_d = din("kv_norm_g", [128, 2])
    wkvb_d = din("w_kvb", [256, 1024])
    cw_d = din("conv_w", [128, 4 * 31])
    cb_d = din("conv_b", [128, 4])
    clg_d = din("conv_ln_g", [128, 4])
    clb_d = din("conv_ln_b", [128, 4])
    wo_d = din("w_o", [D, D])
    g1_d = din("ln1_g", [128, D])
    b1_d = din("ln1_b", [128, D])
    wr_d = din("w_router", [D, E])
    g1pk_d = din("ln1_g_pk", [128, 8])
    b1pk_d = din("ln1_b_pk", [128, 8])
    wg_d = din("w_gate", [E, D, FF])
    wu_d = din("w_up", [E, D, FF])
    wd_d = din("w_down", [E, FF, D])
    g2_d = din("ln2_g", [128, D])
    b2_d = din("ln2_b", [128, D])
    c_identf_d = din("c_identf", [128, 128])
    c_identb_d = din("c_identb", [128, 128], BF16)
    c_iota_d = din("c_iota", [128, 256])
    c_iotap_d = din("c_iotap", [128, 2])
    c_ustr_d = din("c_ustr", [128, 128], BF16)
    c_invf_d = din("c_invf", [128, 1])
    c_tp_d = din("c_tp", [128, NT * 2], BF16)
    c_gmat_d = din("c_gmat", [128, 128])
    out_d = nc.dram_tensor("out", [S, D], F32, kind="ExternalOutput").ap()

    def dump(name, tile_ap, shape, dtype=F32):
        if debug is None or name not in debug:
            return
        t = nc.dram_tensor("dbg_" + name, list(shape), dtype, kind="ExternalOutput").ap()
        dbg_outs.append("dbg_" + name)
        sch.barrier()
        A("sp", lambda e: e.dma_start(out=t, in_=tile_ap), r=[], w=[], dma=True)
        sch.barrier()

    def DMA(q, out, in_, r=(), w=()):
        return A(q, lambda e: e.dma_start(out=out, in_=in_), r=r, w=w, dma=True)

    pst = [nc.alloc_psum_tensor("ps%d" % i, [128, 1024], F32) for i in range(4)]

    def bank(i):
        return pst[i // 2][:, (i % 2) * 512:(i % 2 + 1) * 512]

    def bankb(i):
        return pst[i // 2].bitcast(BF16)[:, (i % 2) * 1024:(i % 2 + 1) * 1024]

    def BK(i):
        return "psum%d" % i

    wpc = {}
    for e_ in range(NPC):
        wpc[("g", e_)] = nc.dram_tensor("wpc_g%d" % e_, [D, FF], BF16).ap()
        wpc[("u", e_)] = nc.dram_tensor("wpc_u%d" % e_, [D, FF], BF16).ap()
        wpc[("d", e_)] = nc.dram_tensor("wpc_d%d" % e_, [FF, D], BF16).ap()
    pc_jobs = []
    for e_ in range(NPC):
        for kind, src in (("g", wg_d), ("u", wu_d), ("d", wd_d)):
            rows = D if kind != "d" else FF
            for hh in range(2):
                pc_jobs.append((kind, e_, src, hh * rows // 2, (hh + 1) * rows // 2))
    pc_pos = [0]

    def precast(n):
        sch.precast_mode = True
        for _ in range(n):
            if pc_pos[0] >= len(pc_jobs):
                break
            kind, e_, src, r0, r1_ = pc_jobs[pc_pos[0]]
            pc_pos[0] += 1
            DMA("pool", wpc[(kind, e_)][r0:r1_, :], src[e_][r0:r1_, :], w=["wpc_%s%d_%d" % (kind, e_, r0)])
        sch.precast_mode = False

    identf = ar.alloc("identf", [128, 128], F32)
    identb = ar.alloc("identb", [128, 128], BF16)
    onesf = ar.alloc("onesf", [128, 128], F32)
    onesb = ar.alloc("onesb", [128, 128], BF16)
    iota_c = ar.alloc("iota_c", [128, 256], F32)
    iota_p = ar.alloc("iota_p", [128, 2], F32)
    ustr = ar.alloc("ustr", [128, 128], BF16)
    DMA("sp", identf[:], c_identf_d, w=["identf"])
    DMA("sp", identb[:], c_identb_d, w=["identb"])
    DMA("sp", iota_c[:], c_iota_d, w=["iota_c"])
    DMA("sp", iota_p[:], c_iotap_d, w=["iota_p"])
    DMA("sp", ustr[:], c_ustr_d, w=["ustr"])
    A("pool", lambda e: e.memset(onesf[:], 1.0), w=["onesf"])
    A("pool", lambda e: e.memset(onesb[:], 1.0), w=["onesb"])

    def swpipe(n, stages):
        ns = len(stages)
        for it in range(n + ns - 1):
            lists = []
            for si in range(ns):
                t_ = it - si
                if 0 <= t_ < n:
                    sch.capture = []
                    stages[si](t_)
                    lists.append(sch.capture)
                    sch.capture = None
            pos_ = [0] * len(lists)
            left = sum(len(l_) for l_ in lists)
            while left:
                for li, l_ in enumerate(lists):
                    if pos_[li] < len(l_):
                        eng, fn, r_, w_, dma_ = l_[pos_[li]]
                        pos_[li] += 1
                        left -= 1
                        sch.add(eng, fn, r_, w_, dma_)

    def ln_small(nm, nrot):
        return [dict(stats=ar.alloc(nm + "st", [128, 2, 6], F32), mv=ar.alloc(nm + "mv", [128, 2], F32), std=ar.alloc(nm + "sd", [128, 1], F32),
                     rstd=ar.alloc(nm + "rs", [128, 1], F32), nmr=ar.alloc(nm + "nm", [128, 1], F32), name="%s%d_" % (nm, i_)) for i_ in range(nrot)]

    def ln_small_free(lst):
        for d_ in lst:
            ar.release(d_["stats"], d_["mv"], d_["std"], d_["rstd"], d_["nmr"])

    def ln_part1a(T, src, src_res):
        n = T["name"]
        stats, mv, std, rstd, nmr = T["stats"], T["mv"], T["std"], T["rstd"], T["nmr"]

        def f_stats(e):
            e.bn_stats(out=stats[:, 0, :], in_=src[:, 0:512])
            return e.bn_stats(out=stats[:, 1, :], in_=src[:, 512:1024])
        A("dve", f_stats, r=list(src_res), w=[n + "st"])
        A("dve", lambda e: e.bn_aggr(out=mv[:], in_=stats[:]), r=[n + "st"], w=[n + "mv"])
        A("act", lambda e: e.activation(out=std[:], in_=mv[:, 1:2], func=AF.Ln, bias=epsln[:], scale=1.0), r=[n + "mv", "epsln"], w=[n + "sd"])
        A("act", lambda e: e.activation(out=rstd[:], in_=std[:], func=AF.Exp, scale=-0.5), r=[n + "sd"], w=[n + "rs"])
        A("dve", lambda e: e.scalar_tensor_tensor(out=nmr[:], in0=mv[:, 0:1], scalar=-1.0, in1=rstd[:], op0=ALU.mult, op1=ALU.mult), r=[n + "mv", n + "rs"], w=[n + "nm"])

    def ln_part1b(T, src, src_res, xn, xn_res):
        n = T["name"]
        rstd, nmr = T["rstd"], T["nmr"]
        A("act", lambda e: e.activation(out=xn, in_=src, func=AF.Identity, bias=nmr[:], scale=rstd[:]), r=list(src_res) + [n + "nm", n + "rs"], w=list(xn_res))

    def ln_part2(xn, xn_res, dst, dst_res, g_bc, b_bc, gres, bres):
        A("pool", lambda e: e.tensor_tensor(out=xn, in0=xn, in1=g_bc[:], op=ALU.mult), r=list(xn_res) + [gres], w=list(xn_res))
        A("dve", lambda e: e.tensor_tensor(out=dst, in0=xn, in1=b_bc[:], op=ALU.add), r=list(xn_res) + [bres], w=list(dst_res))

    epsln = ar.alloc("epsln", [128, 1], F32)
    epsrms = ar.alloc("epsrms", [128, 1], F32)
    A("pool", lambda e: e.memset(epsln[:], LN_EPS), w=["epsln"])
    A("pool", lambda e: e.memset(epsrms[:], RMS_EPS), w=["epsrms"])

    wq_f = ar.alloc("wq_f", [128, 3, 768], F32)
    wkv_f = ar.alloc("wkv_f", [128, 2, 1024], F32)
    qg = ar.alloc("qg", [128, 3], F32)
    kvg = ar.alloc("kvg", [128, 2], F32)
    Wq2 = ar.alloc("Wq2", [128, 3, 8, 128], BF16)
    Wk2 = ar.alloc("Wk2", [128, 2, 8, 64], BF16)
    Wv = ar.alloc("Wv", [128, 2, 8, 64], BF16)
    DMA("sp", wq_f[:], wqb_d.rearrange("(k p) f -> p k f", p=128), w=["wq_f"])
    DMA("sp", wkv_f[:], wkvb_d.rearrange("(k p) f -> p k f", p=128), w=["wkv_f"])
    DMA("sp", qg[:], qg_d, w=["qg"])
    DMA("sp", kvg[:], kvg_d, w=["kvg"])
    for k in range(3):
        src = wq_f[:, k, :].rearrange("p (h c) -> p h c", c=96)
        sc = qg[:, k:k + 1]
        A("dve", lambda e, src=src, sc=sc, k=k: e.tensor_scalar(out=Wq2[:, k, :, 64:96], in0=src[:, :, 64:96], scalar1=sc, scalar2=None, op0=ALU.mult),
          r=["wq_f", "qg"], w=["Wq2"])
        A("dve", lambda e, src=src, sc=sc, k=k: e.tensor_scalar(out=Wq2[:, k, :, 0:64], in0=src[:, :, 0:64], scalar1=sc, scalar2=None, op0=ALU.mult),
          r=["wq_f", "qg"], w=["Wq2"])
        A("dve", lambda e, src=src, sc=sc, k=k: e.tensor_scalar(out=Wq2[:, k, :, 96:112], in0=src[:, :, 80:96], scalar1=sc, scalar2=-1.0, op0=ALU.mult, op1=ALU.mult),
          r=["wq_f", "qg"], w=["Wq2"])
        A("dve", lambda e, src=src, sc=sc, k=k: e.tensor_scalar(out=Wq2[:, k, :, 112:128], in0=src[:, :, 64:80], scalar1=sc, scalar2=None, op0=ALU.mult),
          r=["wq_f", "qg"], w=["Wq2"])
    for k in range(2):
        src = wkv_f[:, k, :].rearrange("p (h c) -> p h c", c=128)
        sc = kvg[:, k:k + 1]
        A("dve", lambda e, src=src, sc=sc, k=k: e.tensor_scalar(out=Wk2[:, k, :, :], in0=src[:, :, 0:64], scalar1=sc, scalar2=None, op0=ALU.mult),
          r=["wkv_f", "kvg"], w=["Wk2"])
        A("dve", lambda e, src=src, sc=sc, k=k: e.tensor_scalar(out=Wv[:, k, :, :], in0=src[:, :, 64:128], scalar1=sc, scalar2=None, op0=ALU.mult),
          r=["wkv_f", "kvg"], w=["Wv"])

    cosT = ar.alloc("cosT", [96, S], F32)
    sinT = ar.alloc("sinT", [96, S], F32)
    pos_i = ar.alloc("pos_i", [128, 512], I32)
    ang = ar.alloc("ang", [128, 512], F32)
    rt0 = ar.alloc("rt0", [128, 512], F32)
    rt1 = ar.alloc("rt1", [128, 512], F32)
    rti = ar.alloc("rti", [128, 512], I32)
    rsc = [ar.alloc("rsc", [128, 512], F32) for _ in range(2)]
    invf = ar.alloc("invf", [128, 1], F32)
    DMA("sp", pos_i[:], pos_d, w=["pos_i"])
    DMA("sp", invf[:], c_invf_d, w=["invf"])
    A("dve", lambda e: e.tensor_copy(out=ang[:], in_=pos_i[:]), r=["pos_i"], w=["ang"])
    A("dve", lambda e: e.tensor_scalar(out=ang[:], in0=ang[:], scalar1=invf[:], scalar2=None, op0=ALU.mult), r=["ang", "invf"], w=["ang"])

    def range_reduce_sin(dst, dres, shift, tmp, tres):
        A("dve", lambda e: e.tensor_scalar(out=rt0[:], in0=ang[:], scalar1=shift, scalar2=1.0 / TWO_PI, op0=ALU.add, op1=ALU.mult), r=["ang"], w=["rt0"])
        A("dve", lambda e: e.tensor_copy(out=rti[:], in_=rt0[:]), r=["rt0"], w=["rti"])
        A("dve", lambda e: e.tensor_copy(out=rt0[:], in_=rti[:]), r=["rti"], w=["rt0"])
        A("dve", lambda e: e.tensor_scalar(out=rt1[:], in0=ang[:], scalar1=shift, scalar2=None, op0=ALU.add), r=["ang"], w=["rt1"])
        A("dve", lambda e: e.scalar_tensor_tensor(out=rt1[:], in0=rt0[:], scalar=-TWO_PI, in1=rt1[:], op0=ALU.mult, op1=ALU.add), r=["rt0", "rt1"], w=["rt1"])
        A("dve", lambda e: e.tensor_scalar(out=rt0[:], in0=rt1[:], scalar1=PI, scalar2=None, op0=ALU.is_gt), r=["rt1"], w=["rt0"])
        A("dve", lambda e: e.scalar_tensor_tensor(out=rt1[:], in0=rt0[:], scalar=-TWO_PI, in1=rt1[:], op0=ALU.mult, op1=ALU.add), r=["rt0", "rt1"], w=["rt1"])
        A("dve", lambda e: e.tensor_scalar(out=rt0[:], in0=rt1[:], scalar1=-PI, scalar2=None, op0=ALU.is_lt), r=["rt1"], w=["rt0"])
        A("dve", lambda e: e.scalar_tensor_tensor(out=rt1[:], in0=rt0[:], scalar=TWO_PI, in1=rt1[:], op0=ALU.mult, op1=ALU.add), r=["rt0", "rt1"], w=["rt1"])
        A("dve", lambda e: e.tensor_scalar(out=rt1[:], in0=rt1[:], scalar1=-3.1415925, scalar2=3.1415925, op0=ALU.max, op1=ALU.min), r=["rt1"], w=["rt1"])
        A("act", lambda e: e.activation(out=tmp[:], in_=rt1[:], func=AF.Sin), r=["rt1"], w=[tres])
        for blk in range(4):
            DMA("sp", dst[64:96, blk * 512:(blk + 1) * 512], tmp[blk * 32:(blk + 1) * 32, :], r=[tres], w=[dres])

    range_reduce_sin(sinT, "sinT", 0.0, rsc[0], "rsc0")
    range_reduce_sin(cosT, "cosT", PI / 2.0, rsc[1], "rsc1")

    cw = ar.alloc("cw", [128, 4 * 31], F32)
    Dg = ar.alloc("Dg", [128, 4 * 31, 128], BF16)
    DMA("sp", cw[:], cw_d, w=["cw"])
    for m in range(4):
        A("dve", lambda e, m=m: e.tensor_tensor(out=Dg[:, m * 31:(m + 1) * 31, :], in0=identb[:].unsqueeze(1).to_broadcast([128, 31, 128]),
                                               in1=cw[:, m * 31:(m + 1) * 31].unsqueeze(2).to_broadcast([128, 31, 128]), op=ALU.mult),
          r=["identb", "cw"], w=["Dg%d" % m])

    hT = ar.alloc("hT", [128, 8, S], BF16)
    g0 = ar.alloc("g0", [128, D], F32)
    b0 = ar.alloc("b0", [128, D], F32)
    DMA("sp", g0[:], g0_d, w=["g0"])
    DMA("sp", b0[:], b0_d, w=["b0"])
    NR1 = 6
    xt = [ar.alloc("xt", [128, D], F32) for _ in range(NR1)]
    hb = [ar.alloc("hb", [128, D], BF16) for _ in range(NR1)]
    lnA = ln_small("lnA", NR1)

    def p1_s0(t):
        p = t % NR1
        DMA("sp", xt[p][:], x_d[t * 128:(t + 1) * 128, :], w=["xt%d" % p])

    def p1_s0b(t):
        pass

    def p1_s1(t):
        p = t % NR1
        ln_part1a(lnA[p], xt[p][:], ["xt%d" % p])

    def p1_s1b(t):
        p = t % NR1
        ln_part1b(lnA[p], xt[p][:], ["xt%d" % p], xt[p][:], ["xt%d" % p])

    def p1_s2(t):
        p = t % NR1
        ln_part2(xt[p][:], ["xt%d" % p], hb[p][:], ["hb%d" % p], g0, b0, "g0", "b0")

    def p1_s3(t):
        p = t % NR1
        pb_ = t % 2

        def f_tr(e, p=p, pb_=pb_):
            for k in range(8):
                i_ = e.transpose(out=bankb(pb_)[:, k * 128:(k + 1) * 128], in_=hb[p][:, k * 128:(k + 1) * 128], identity=identb[:])
            return i_
        A("pe", f_tr, r=["hb%d" % p, "identb"], w=[BK(pb_)])
        A("act", lambda e, pb_=pb_, t=t: e.activation(out=hT[:, :, t * 128:(t + 1) * 128], in_=bankb(pb_).rearrange("p (k c) -> p k c", c=128), func=AF.Copy),
          r=[BK(pb_)], w=["hT%d" % (t // 4)])
    swpipe(NT, [p1_s0, p1_s0b, p1_s1, p1_s1b, p1_s2, p1_s3])
    if debug and "hT" in debug:
        dump("hT", hT[:], [128, 8, S], BF16)
    sch.barrier()
    ar.release(wq_f, wkv_f, qg, kvg, pos_i, ang, rt0, rt1, rti, invf, *rsc, *xt, *hb, g0, b0)
    ln_small_free(lnA)
    ar.flush()

    convT = ar.alloc("convT", [128, 4, S], BF16)
    wc = ar.alloc("wc", [128, 8, 1024], BF16)
    hcp = ar.alloc("hcp", [128, 4, S + 30], BF16)
    cb = ar.alloc("cb", [128, 4], F32)
    clg = ar.alloc("clg", [128, 4], F32)
    clb = ar.alloc("clb", [128, 4], F32)
    sig = [ar.alloc("sig", [128, 512], F32) for _ in range(2)]
    DMA("pool", wc[:], w_in_d.rearrange("(k p) f -> p k f", p=128)[:, :, 672:1696], w=["wc"])
    DMA("sp", cb[:], cb_d, w=["cb"])
    DMA("sp", clg[:], clg_d, w=["clg"])
    DMA("sp", clb[:], clb_d, w=["clb"])
    A("pool", lambda e: e.memset(hcp[:, :, 0:15], 0.0), w=["hcp_lo"])
    A("pool", lambda e: e.memset(hcp[:, :, S + 15:S + 30], 0.0), w=["hcp_hi"])
    precast(12)
    it = 0
    for b in range(4):
        for m in range(4):
            pa = (it % 2) * 2
            pg = pa + 1
            sp_ = it % 2
            it += 1

            def f_glu(e, m=m, b=b, pa=pa, pg=pg):
                for k in range(8):
                    e.matmul(bank(pa), lhsT=wc[:, k, m * 128:(m + 1) * 128], rhs=hT[:, k, b * 512:(b + 1) * 512], start=(k == 0), stop=(k == 7))
                for k in range(8):
                    i_ = e.matmul(bank(pg), lhsT=wc[:, k, 512 + m * 128:512 + (m + 1) * 128], rhs=hT[:, k, b * 512:(b + 1) * 512], start=(k == 0), stop=(k == 7))
                return i_
            A("pe", f_glu, r=["wc", "hT%d" % b], w=[BK(pa), BK(pg)])
            A("act", lambda e, pg=pg, sp_=sp_: e.activation(out=sig[sp_][:], in_=bank(pg), func=AF.Sigmoid), r=[BK(pg)], w=["sig%d" % sp_])
            A("dve", lambda e, pa=pa, sp_=sp_, m=m, b=b: e.tensor_tensor(out=hcp[:, m, 15 + b * 512:15 + (b + 1) * 512], in0=bank(pa), in1=sig[sp_][:], op=ALU.mult),
              r=[BK(pa), "sig%d" % sp_], w=["hcp%d_%d" % (m, b)])
    yc = [ar.alloc("yc", [128, 4, 512], F32) for _ in range(2)]
    ysq = [ar.alloc("ysq", [128, 512], F32) for _ in range(2)]
    cmean = [ar.alloc("cmean", [128, 512], F32) for _ in range(2)]
    cm2 = [ar.alloc("cm2", [128, 512], F32) for _ in range(2)]
    crstd = [ar.alloc("crstd", [128, 512], F32) for _ in range(2)]
    ctmp = [ar.alloc("ctmp", [128, 512], F32) for _ in range(2)]
    seq = [(b, m) for b in range(4) for m in range(4)]

    def conv_step(i):
        b, m = seq[i]
        pb = b % 2
        pc = i % 2
        rds = ["Dg%d" % m, "hcp%d_%d" % (m, b)]
        rds.append("hcp%d_%d" % (m, b - 1) if b > 0 else "hcp_lo")
        rds.append("hcp%d_%d" % (m, b + 1) if b < 3 else "hcp_hi")

        def f_conv(e, m=m, b=b, pc=pc):
            for k in range(31):
                i_ = e.matmul(bank(pc), lhsT=Dg[:, m * 31 + k, :], rhs=hcp[:, m, b * 512 + k:b * 512 + k + 512], start=(k == 0), stop=(k == 30))
            return i_
        A("pe", f_conv, r=rds, w=[BK(pc)])
        A("act", lambda e, m=m, pb=pb, pc=pc: e.activation(out=yc[pb][:, m, :], in_=bank(pc), func=AF.Identity, bias=cb[:, m:m + 1], scale=1.0),
          r=[BK(pc), "cb"], w=["yc%d_%d" % (pb, m)])
        A("act", lambda e, m=m, pc=pc: e.activation(out=ysq[pc][:], in_=bank(pc), func=AF.Square, bias=cb[:, m:m + 1], scale=1.0),
          r=[BK(pc), "cb"], w=["ysq%d" % pc])

    def stat_step(i):
        b, m = seq[i]
        pb = b % 2
        pc = i % 2
        s1 = 4 + pb * 2
        s2 = 5 + pb * 2

        def f_st(e, m=m, pb=pb, pc=pc, s1=s1, s2=s2):
            e.matmul(bank(s1), lhsT=onesf[:], rhs=yc[pb][:, m, :], start=(m == 0), stop=(m == 3))
            return e.matmul(bank(s2), lhsT=onesf[:], rhs=ysq[pc][:], start=(m == 0), stop=(m == 3))
        A("pe", f_st, r=["onesf", "yc%d_%d" % (pb, m), "ysq%d" % pc], w=[BK(s1), BK(s2)])

    def norm_block(b):
        pb = b % 2
        s1 = 4 + pb * 2
        s2 = 5 + pb * 2
        A("dve", lambda e, pb=pb, s1=s1: e.tensor_scalar(out=cmean[pb][:], in0=bank(s1), scalar1=1.0 / 512.0, scalar2=None, op0=ALU.mult), r=[BK(s1)], w=["cmean%d" % pb])
        A("dve", lambda e, pb=pb: e.tensor_tensor(out=cm2[pb][:], in0=cmean[pb][:], in1=cmean[pb][:], op=ALU.mult), r=["cmean%d" % pb], w=["cm2%d" % pb])
        A("dve", lambda e, pb=pb, s2=s2: e.scalar_tensor_tensor(out=cm2[pb][:], in0=bank(s2), scalar=1.0 / 512.0, in1=cm2[pb][:], op0=ALU.mult, op1=ALU.subtract),
          r=[BK(s2), "cm2%d" % pb], w=["cm2%d" % pb])
        A("act", lambda e, pb=pb: e.activation(out=crstd[pb][:], in_=cm2[pb][:], func=AF.Ln, bias=epsln[:], scale=1.0), r=["cm2%d" % pb, "epsln"], w=["crstd%d" % pb])
        A("act", lambda e, pb=pb: e.activation(out=crstd[pb][:], in_=crstd[pb][:], func=AF.Exp, scale=-0.5), r=["crstd%d" % pb], w=["crstd%d" % pb])
        for m in range(4):
            pc = m % 2
            A("dve", lambda e, pb=pb, m=m, pc=pc: e.tensor_tensor(out=ctmp[pc][:], in0=yc[pb][:, m, :], in1=cmean[pb][:], op=ALU.subtract),
              r=["yc%d_%d" % (pb, m), "cmean%d" % pb], w=["ctmp%d" % pc])
            A("dve", lambda e, pb=pb, pc=pc: e.tensor_tensor(out=ctmp[pc][:], in0=ctmp[pc][:], in1=crstd[pb][:], op=ALU.mult),
              r=["ctmp%d" % pc, "crstd%d" % pb], w=["ctmp%d" % pc])
            A("act", lambda e, m=m, b=b, pc=pc: e.activation(out=convT[:, m, b * 512:(b + 1) * 512], in_=ctmp[pc][:], func=AF.Silu, bias=clb[:, m:m + 1], scale=clg[:, m:m + 1]),
              r=["ctmp%d" % pc, "clg", "clb"], w=["convT%d" % b])

    for i in range(17):
        if i < 16:
            conv_step(i)
        if i >= 1:
            stat_step(i - 1)
        if i >= 6 and (i - 6) % 4 == 0:
            norm_block((i - 6) // 4)
    norm_block(3)
    if debug and "convT" in debug:
        dump("convT", convT[:], [128, 4, S], BF16)
    sch.barrier()
    ar.release(wc, hcp, cw, cb, clg, clb, Dg, *sig, *yc, *ysq, *cmean, *cm2, *crstd, *ctmp)
    ar.flush()

    qT = ar.alloc("qT", [128, 8, S], BF16)
    kT = ar.alloc("kT", [128, 8, S], BF16)
    vaug = ar.alloc("vaug", [128, NT, 8, 65], BF16)
    wl = ar.alloc("wl", [128, 8, 704], BF16)
    DMA("pool", wl[:, :, 0:672], w_in_d.rearrange("(k p) f -> p k f", p=128)[:, :, 0:672], w=["wl"])
    A("dve", lambda e: e.tensor_scalar(out=wl[:, :, 672:688], in0=wl[:, :, 656:672], scalar1=-1.0, scalar2=None, op0=ALU.mult), r=["wl"], w=["wl"])
    A("dve", lambda e: e.tensor_copy(out=wl[:, :, 688:704], in_=wl[:, :, 640:656]), r=["wl"], w=["wl"])
    A("pool", lambda e: e.memset(vaug[:, :, :, 64:65], 1.0), w=["vaug1"])
    precast(12)
    cqb = ar.alloc("cqb", [128, 3, 512], BF16)
    ckvb = ar.alloc("ckvb", [128, 2, 512], BF16)
    cqn = [ar.alloc("cqn", [128, 3, 512], BF16) for _ in range(2)]
    ckvn = [ar.alloc("ckvn", [128, 2, 512], BF16) for _ in range(2)]
    sqq = [ar.alloc("sqq", [128, 512], F32) for _ in range(2)]
    rq = ar.alloc("rq", [128, 512], F32)
    rk = ar.alloc("rk", [128, 512], F32)
    t1 = ar.alloc("t1", [96, 512], F32)
    t2 = ar.alloc("t2", [96, 512], F32)
    sq_ctr = [0]

    def proj_chunk(b, col0, dst, dres, j, sbank, nj, jj):
        pj = sq_ctr[0] % 2
        sq_ctr[0] += 1

        def f_p(e, pj=pj, b=b, col0=col0):
            for k in range(8):
                i_ = e.matmul(bank(pj), lhsT=wl[:, k, col0:col0 + 128], rhs=hT[:, k, b * 512:(b + 1) * 512], start=(k == 0), stop=(k == 7))
            return i_
        A("pe", f_p, r=["wl", "hT%d" % b], w=[BK(pj)])
        A("act", lambda e, pj=pj, j=j: e.activation(out=dst[:, j, :], in_=bank(pj), func=AF.Copy), r=[BK(pj)], w=[dres + "%d" % j])
        A("act", lambda e, pj=pj: e.activation(out=sqq[pj][:], in_=bank(pj), func=AF.Square), r=[BK(pj)], w=["sqq%d" % pj])
        return lambda: A("pe", lambda e, pj=pj: e.matmul(bank(sbank), lhsT=onesf[:], rhs=sqq[pj][:], start=(jj == 0), stop=(jj == nj - 1)), r=["onesf", "sqq%d" % pj], w=[BK(sbank)])

    def rstd_ops(sbank, n, r_, rres):
        A("act", lambda e: e.activation(out=r_[:], in_=bank(sbank), func=AF.Ln, bias=epsrms[:], scale=1.0 / n), r=[BK(sbank), "epsrms"], w=[rres])
        A("act", lambda e: e.activation(out=r_[:], in_=r_[:], func=AF.Exp, scale=-0.5), r=[rres], w=[rres])

    def stage_P(b):
        pb = b % 2
        bs = slice(b * 512, (b + 1) * 512)
        pend = None
        for j in range(3):
            nxt = proj_chunk(b, j * 128, cqb, "cqb", j, 2, 3, j)
            if pend:
                pend()
            pend = nxt
        for j in range(2):
            nxt = proj_chunk(b, 384 + j * 128, ckvb, "ckvb", j, 3, 2, j)
            pend()
            pend = nxt

        def f_kr2(e, b=b):
            for k in range(8):
                e.matmul(bank(4)[64:96, :], lhsT=wl[:, k, 640:672], rhs=hT[:, k, b * 512:(b + 1) * 512], start=(k == 0), stop=(k == 7))
            for k in range(8):
                i_ = e.matmul(bank(5)[64:96, :], lhsT=wl[:, k, 672:704], rhs=hT[:, k, b * 512:(b + 1) * 512], start=(k == 0), stop=(k == 7))
            return i_
        A("pe", f_kr2, r=["wl", "hT%d" % b], w=[BK(4), BK(5)])
        pend()
        rstd_ops(2, 384.0, rq, "rq")
        rstd_ops(3, 256.0, rk, "rk")
        for j in range(3):
            A("dve", lambda e, j=j, pb=pb: e.tensor_tensor(out=cqn[pb][:, j, :], in0=cqb[:, j, :], in1=rq[:], op=ALU.mult), r=["cqb%d" % j, "rq"], w=["cqn%d_%d" % (pb, j)])
        for j in range(2):
            A("dve", lambda e, j=j, pb=pb: e.tensor_tensor(out=ckvn[pb][:, j, :], in0=ckvb[:, j, :], in1=rk[:], op=ALU.mult), r=["ckvb%d" % j, "rk"], w=["ckvn%d_%d" % (pb, j)])
        A("dve", lambda e, bs=bs: e.tensor_tensor(out=t1[64:96, :], in0=bank(4)[64:96, :], in1=cosT[64:96, bs], op=ALU.mult), r=[BK(4), "cosT"], w=["t1"])
        A("dve", lambda e, bs=bs: e.tensor_tensor(out=t2[64:96, :], in0=bank(5)[64:96, :], in1=sinT[64:96, bs], op=ALU.mult), r=[BK(5), "sinT"], w=["t2"])
        A("dve", lambda e, bs=bs: e.tensor_tensor(out=kT[64:96, 0, bs], in0=t1[64:96, :], in1=t2[64:96, :], op=ALU.add), r=["t1", "t2"], w=["kTr0"])
        for h in range(1, H):
            A("dve" if h % 2 else "act", (lambda e, bs=bs, h=h: e.tensor_copy(out=kT[64:96, h, bs], in_=kT[64:96, 0, bs])) if h % 2 else
              (lambda e, bs=bs, h=h: e.activation(out=kT[64:96, h, bs], in_=kT[64:96, 0, bs], func=AF.Copy)), r=["kTr0"], w=["kTr%d" % h])

    def stage_H(b):
        pb = b % 2
        bs = slice(b * 512, (b + 1) * 512)
        cq_r = ["cqn%d_%d" % (pb, j) for j in range(3)]
        ckv_r = ["ckvn%d_%d" % (pb, j) for j in range(2)]
        for h in range(H):
            pq = 6 + (h % 2)
            pr = 4 + (h % 2)

            def f_q(e, h=h, pq=pq, pr=pr, pb=pb):
                for k in range(3):
                    e.matmul(bank(pq)[0:96, :], lhsT=Wq2[:, k, h, 0:96], rhs=cqn[pb][:, k, :], start=(k == 0), stop=(k == 2))
                for k in range(3):
                    i_ = e.matmul(bank(pr)[64:96, :], lhsT=Wq2[:, k, h, 96:128], rhs=cqn[pb][:, k, :], start=(k == 0), stop=(k == 2))
                return i_
            A("pe", f_q, r=["Wq2"] + cq_r, w=[BK(pq), BK(pr)])
            A("act", lambda e, h=h, pq=pq, bs=bs: e.activation(out=qT[0:64, h, bs], in_=bank(pq)[0:64, :], func=AF.Copy), r=[BK(pq)], w=["qT%d_%d" % (h, b)])
            A("dve", lambda e, pq=pq, bs=bs: e.tensor_tensor(out=t1[64:96, :], in0=bank(pq)[64:96, :], in1=cosT[64:96, bs], op=ALU.mult), r=[BK(pq), "cosT"], w=["t1"])
            A("dve", lambda e, pr=pr, bs=bs: e.tensor_tensor(out=t2[64:96, :], in0=bank(pr)[64:96, :], in1=sinT[64:96, bs], op=ALU.mult), r=[BK(pr), "sinT"], w=["t2"])
            A("dve", lambda e, h=h, bs=bs: e.tensor_tensor(out=qT[64:96, h, bs], in0=t1[64:96, :], in1=t2[64:96, :], op=ALU.add), r=["t1", "t2"], w=["qTr%d_%d" % (h, b)])
        for h in range(H):
            pk = 6 + (h % 2)

            def f_k(e, h=h, pk=pk, pb=pb):
                for k in range(2):
                    i_ = e.matmul(bank(pk)[0:64, :], lhsT=Wk2[:, k, h, :], rhs=ckvn[pb][:, k, :], start=(k == 0), stop=(k == 1))
                return i_
            A("pe", f_k, r=["Wk2"] + ckv_r, w=[BK(pk)])
            A("act", lambda e, h=h, pk=pk, bs=bs: e.activation(out=kT[0:64, h, bs], in_=bank(pk)[0:64, :], func=AF.Copy), r=[BK(pk)], w=["kT%d_%d" % (h, b)])
        for tt in range(4):
            pv = 4 + (tt % 2)
            t = b * 4 + tt

            def f_v(e, tt=tt, pv=pv, pb=pb):
                for k in range(2):
                    i_ = e.matmul(bank(pv), lhsT=ckvn[pb][:, k, tt * 128:(tt + 1) * 128], rhs=Wv[:, k, :, :].rearrange("p h c -> p (h c)"), start=(k == 0), stop=(k == 1))
                return i_
            A("pe", f_v, r=["Wv"] + ckv_r, w=[BK(pv)])
            A("act", lambda e, pv=pv, t=t: e.activation(out=vaug[:, t, :, 0:64], in_=bank(pv).rearrange("p (h c) -> p h c", c=64), func=AF.Copy),
              r=[BK(pv)], w=["vaug%d" % t])

    stage_P(0)
    for b in range(4):
        if b + 1 < 4:
            stage_P(b + 1)
        stage_H(b)
    if debug and "qT" in debug:
        dump("qT", qT[:], [128, 8, S], BF16)
        dump("kT", kT[:], [128, 8, S], BF16)
        dump("vaug", vaug[:], [128, NT, 8, 65], BF16)
    sch.barrier()
    ar.release(hT, wl, cqb, ckvb, *cqn, *ckvn, *sqq, rq, rk, t1, t2, cosT, sinT, Wq2, Wk2, Wv)
    ar.flush()

    precast(18)
    attn_tok = ar.alloc("attn_tok", [128, NT, 512], BF16)
    ptb = [ar.alloc("ptb", [128, 1024], BF16) for _ in range(3)]
    rec = [ar.alloc("rec", [128, 4], F32) for _ in range(2)]
    steps = []
    for h in range(H):
        for qb in range(4):
            for kp in range(8):
                steps.append((h, qb, kp))

    def emit_scores(i):
        h, qb, kp = steps[i]
        sp_ = i % 2

        def f_s(e, h=h, qb=qb, kp=kp, sp_=sp_):
            for half in range(2):
                kt = kp * 2 + half
                i_ = e.matmul(pst[sp_][:, half * 512:(half + 1) * 512], lhsT=kT[0:96, h, kt * 128:(kt + 1) * 128], rhs=qT[0:96, h, qb * 512:(qb + 1) * 512], start=True, stop=True)
            return i_
        rds = ["kT%d_%d" % (h, (kp * 2) // 4), "kTr%d" % h, "qT%d_%d" % (h, qb), "qTr%d_%d" % (h, qb)]
        A("pe", f_s, r=rds, w=[BK(2 * sp_), BK(2 * sp_ + 1)])
        pp = i % 3
        A("act", lambda e, sp_=sp_, pp=pp: e.activation(out=ptb[pp][:], in_=pst[sp_][:], func=AF.Exp, scale=SCALE), r=[BK(2 * sp_), BK(2 * sp_ + 1)], w=["ptb%d" % pp])

    def emit_pv(i):
        h, qb, kp = steps[i]
        pp = i % 3
        g = i // 8
        ob = 4 + (g % 2)
        O = bank(ob).rearrange("p (q c) -> p q c", c=128)

        def f_pv(e, h=h, kp=kp, pp=pp, O=O):
            for half in range(2):
                kt = kp * 2 + half
                for qt in range(4):
                    i_ = e.matmul(O[:, qt, 0:65], lhsT=ptb[pp][:, half * 512 + qt * 128:half * 512 + (qt + 1) * 128], rhs=vaug[:, kt, h, :],
                                  start=(kt == 0 and qt == 0), stop=(kt == 15), skip_group_check=True)
            return i_
        rds = ["ptb%d" % pp, "vaug1"] + ["vaug%d" % (kp * 2), "vaug%d" % (kp * 2 + 1)]
        A("pe", f_pv, r=rds, w=[BK(ob)])
        if kp == 7:
            pr_ = g % 2
            A("dve", lambda e, O=O, pr_=pr_: e.reciprocal(out=rec[pr_][:], in_=O[:, :, 64]), r=[BK(ob)], w=["rec%d" % pr_])
            for qt in range(4):
                t = qb * 4 + qt
                A("dve", lambda e, O=O, pr_=pr_, qt=qt, t=t, h=h: e.tensor_scalar(out=attn_tok[:, t, h * 64:(h + 1) * 64], in0=O[:, qt, 0:64], scalar1=rec[pr_][:, qt:qt + 1], scalar2=None, op0=ALU.mult),
                  r=[BK(ob), "rec%d" % pr_], w=["attn_tok%d" % t])

    nst = len(steps)
    emit_scores(0)
    for i in range(nst):
        if i + 1 < nst:
            emit_scores(i + 1)
        emit_pv(i)
    if debug and "attn_tok" in debug:
        dump("attn_tok", attn_tok[:], [128, NT, 512], BF16)
    sch.barrier()
    ar.release(qT, kT, vaug, *ptb, *rec)
    ar.flush()

    attnT = ar.alloc("attnT", [128, 4, S], BF16)
    for t in range(NT):
        p = t % 2

        def f_tr2(e, t=t, p=p):
            for c in range(4):
                i_ = e.transpose(out=bankb(p)[:, c * 128:(c + 1) * 128], in_=attn_tok[:, t, c * 128:(c + 1) * 128], identity=identb[:])
            return i_
        A("pe", f_tr2, r=["attn_tok%d" % t, "identb"], w=[BK(p)])
        A("act", lambda e, t=t, p=p: e.activation(out=attnT[:, :, t * 128:(t + 1) * 128], in_=bankb(p)[:, 0:512].rearrange("p (k c) -> p k c", c=128), func=AF.Copy),
          r=[BK(p)], w=["attnT%d" % t])
    sch.barrier()
    ar.release(attn_tok)
    ar.flush()

    macc = nc.dram_tensor("moe_acc", [S, D], F32).ap()
    h1b = ar.alloc("h1b", [128, NT, D], BF16)
    wo = ar.alloc("wo", [128, 8, D], BF16)
    wr = ar.alloc("wr", [128, 8, E], F32)
    wrg = ar.alloc("wrg", [128, 8, E], F32)
    g0 = ar.alloc("g0", [128, D], F32)
    b0 = ar.alloc("b0", [128, D], F32)
    g1 = ar.alloc("g1", [128, D], F32)
    b1 = ar.alloc("b1", [128, D], F32)
    g1pk = ar.alloc("g1pk", [128, 8], F32)
    b1pk = ar.alloc("b1pk", [128, 8], F32)
    brow = ar.alloc("brow", [1, E], F32)
    aff = ar.alloc("aff", [128, NT, E], F32)
    DMA("pool", wo[:], wo_d.rearrange("(k p) f -> p k f", p=128), w=["wo"])
    DMA("sp", wr[:], wr_d.rearrange("(k p) f -> p k f", p=128), w=["wr"])
    DMA("sp", g0[:], g0_d, w=["g0"])
    DMA("sp", b0[:], b0_d, w=["b0"])
    DMA("sp", g1[:], g1_d, w=["g1"])
    DMA("sp", b1[:], b1_d, w=["b1"])
    DMA("sp", g1pk[:], g1pk_d, w=["g1pk"])
    DMA("sp", b1pk[:], b1pk_d, w=["b1pk"])
    A("dve", lambda e: e.tensor_scalar(out=g0[:], in0=g0[:], scalar1=ALPHA, scalar2=None, op0=ALU.mult), r=["g0"], w=["g0"])
    A("dve", lambda e: e.tensor_scalar(out=b0[:], in0=b0[:], scalar1=ALPHA, scalar2=None, op0=ALU.mult), r=["b0"], w=["b0"])
    bh = ar.alloc("bh", [1, D], BF16)
    bl = ar.alloc("bl", [1, D], BF16)
    blf = ar.alloc("blf", [1, D], F32)
    A("dve", lambda e: e.tensor_copy(out=bh[:], in_=b0[0:1, :]), r=["b0"], w=["bh"])
    A("dve", lambda e: e.tensor_tensor(out=blf[:], in0=b0[0:1, :], in1=bh[:], op=ALU.subtract), r=["b0", "bh"], w=["blf"])
    A("dve", lambda e: e.tensor_copy(out=bl[:], in_=blf[:]), r=["blf"], w=["bl"])
    A("dve", lambda e: e.tensor_scalar(out=g1[:], in0=g1[:], scalar1=ALPHA, scalar2=None, op0=ALU.mult), r=["g1"], w=["g1"])
    A("dve", lambda e: e.tensor_scalar(out=b1[:], in0=b1[:], scalar1=ALPHA, scalar2=None, op0=ALU.mult), r=["b1"], w=["b1"])
    for k in range(8):
        A("dve", lambda e, k=k: e.tensor_scalar(out=wrg[:, k, :], in0=wr[:, k, :], scalar1=g1pk[:, k:k + 1], scalar2=None, op0=ALU.mult), r=["wr", "g1pk"], w=["wrg"])

    def f_brow(e):
        for k in range(8):
            i_ = e.matmul(bank(7)[0:1, 0:E], lhsT=b1pk[:, k:k + 1], rhs=wr[:, k, :], start=(k == 0), stop=(k == 7))
        return i_
    A("pe", f_brow, r=["b1pk", "wr"], w=[BK(7)])
    A("dve", lambda e: e.tensor_copy(out=brow[:], in_=bank(7)[0:1, 0:E]), r=[BK(7)], w=["brow"])
    NR4 = 6
    NRX = 7
    NRZ = 5
    lgb = [bank(6)[:, 0:E], bank(7)[:, 0:E]]
    xt = [ar.alloc("xt", [128, D], F32) for _ in range(NRX)]
    zt = [ar.alloc("zt", [128, D], F32) for _ in range(NRZ)]
    r1 = [ar.alloc("r1", [128, D], F32) for _ in range(2)]
    h1T = [ar.alloc("h1T", [128, D], F32) for _ in range(2)]
    smx = [ar.alloc("smx", [128, 1], F32) for _ in range(2)]
    ssum = [ar.alloc("ssum", [128, 1], F32) for _ in range(2)]
    sex = [ar.alloc("sex", [128, E], F32) for _ in range(2)]
    lnB = ln_small("lnB", NR4)
    lnC = ln_small("lnC", NR4)

    def p4_s0(t):
        p = t % NRX
        DMA("sp", xt[p][:], x_d[t * 128:(t + 1) * 128, :], w=["xt%d" % p])

    def p4_s0b(t):
        pass

    def p4_s1(t):
        p = t % NRX
        ln_part1a(lnB[t % NR4], xt[p][:], ["xt%d" % p])

    def p4_s1b(t):
        p = t % NRX
        ln_part1b(lnB[t % NR4], xt[p][:], ["xt%d" % p], xt[p][:], ["xt%d" % p])

    def p4_s2a(t):
        p = t % NRX
        A("pool", lambda e, p=p: e.tensor_tensor(out=xt[p][:], in0=xt[p][:], in1=g0[:], op=ALU.mult), r=["xt%d" % p, "g0"], w=["xt%d" % p])
        for half in range(2):
            pw = 2 * (t % 2) + half

            def f_wo(e, t=t, half=half, pw=pw):
                for k in range(8):
                    src = attnT[:, k, t * 128:(t + 1) * 128] if k < 4 else convT[:, k - 4, t * 128:(t + 1) * 128]
                    e.matmul(bank(pw), lhsT=src, rhs=wo[:, k, half * 512:(half + 1) * 512], start=(k == 0), stop=False)
                e.matmul(bank(pw), lhsT=onesb[0:1, :], rhs=bh[0:1, half * 512:(half + 1) * 512], start=False, stop=False)
                return e.matmul(bank(pw), lhsT=onesb[0:1, :], rhs=bl[0:1, half * 512:(half + 1) * 512], start=False, stop=True)
            A("pe", f_wo, r=["attnT%d" % t, "convT%d" % (t // 4), "wo", "onesb", "bh", "bl"], w=[BK(pw)])

    def p4_s2b(t):
        p = t % NRX
        pz = t % NRZ
        for half in range(2):
            pw = 2 * (t % 2) + half
            A("dve", lambda e, p=p, pz=pz, half=half, pw=pw: e.tensor_tensor(out=zt[pz][:, half * 512:(half + 1) * 512], in0=xt[p][:, half * 512:(half + 1) * 512], in1=bank(pw), op=ALU.add),
              r=["xt%d" % p, BK(pw)], w=["zt%d_%d" % (pz, half)])

    def p4_s3(t):
        p = t % NRZ
        ln_part1a(lnC[t % NR4], zt[p][:], ["zt%d_0" % p, "zt%d_1" % p])

    def p4_s3b(t):
        p = t % NRZ
        ln_part1b(lnC[t % NR4], zt[p][:], ["zt%d_0" % p, "zt%d_1" % p], zt[p][:], ["zt%d_0" % p, "zt%d_1" % p])

    def p4_s4(t):
        p = t % NRZ
        zr = ["zt%d_0" % p, "zt%d_1" % p]
        A("act", lambda e, t=t, p=p: e.activation(out=h1b[:, t, :], in_=zt[p][:], func=AF.Copy), r=zr, w=["h1b%d" % t])
        pt_ = 2

        def f_trf(e, p=p, pt_=pt_):
            for k in range(8):
                i_ = e.transpose(out=pst[pt_][:, k * 128:(k + 1) * 128], in_=zt[p][:, k * 128:(k + 1) * 128], identity=identf[:])
            return i_
        A("pe", f_trf, r=zr + ["identf"], w=[BK(2 * pt_), BK(2 * pt_ + 1)])
        pr_ = t % 2
        A("pool", lambda e, p=p, pr_=pr_: e.tensor_tensor(out=r1[pr_][:], in0=zt[p][:], in1=g1[:], op=ALU.mult), r=zr + ["g1"], w=["r1%d" % pr_])
        A("pool", lambda e, pr_=pr_: e.tensor_tensor(out=r1[pr_][:], in0=r1[pr_][:], in1=b1[:], op=ALU.add), r=["r1%d" % pr_, "b1"], w=["r1%d" % pr_])
        DMA("sp", macc[t * 128:(t + 1) * 128, :], r1[pr_][:], r=["r1%d" % pr_], w=["macc"])
        ph_ = t % 2
        A("act", lambda e, ph_=ph_, pt_=pt_: e.activation(out=h1T[ph_][:], in_=pst[pt_][:], func=AF.Copy), r=[BK(2 * pt_), BK(2 * pt_ + 1)], w=["h1T%d" % ph_])

    def p4_s5b(t):
        p = t % 2
        lg = lgb[p]

        def f_lg(e, p=p, lg=lg):
            for k in range(8):
                e.matmul(lg, lhsT=h1T[p][:, k * 128:(k + 1) * 128], rhs=wrg[:, k, :], start=(k == 0), stop=False)
            return e.matmul(lg, lhsT=onesf[0:1, :], rhs=brow[:], start=False, stop=True)
        A("pe", f_lg, r=["h1T%d" % p, "wrg", "brow", "onesf"], w=[BK(6 + p)])
        A("dve", lambda e, p=p, lg=lg: e.tensor_reduce(out=smx[p][:], in_=lg, axis=AX.X, op=ALU.max), r=[BK(6 + p)], w=["smx%d" % p])
        A("dve", lambda e, p=p: e.tensor_scalar(out=smx[p][:], in0=smx[p][:], scalar1=-1.0, scalar2=None, op0=ALU.mult), r=["smx%d" % p], w=["smx%d" % p])

    def p4_s5c(t):
        p = t % 2
        lg = lgb[p]
        A("act", lambda e, p=p, lg=lg: e.activation(out=sex[p][:], in_=lg, func=AF.Exp, bias=smx[p][:], scale=1.0),
          r=[BK(6 + p), "smx%d" % p], w=["sex%d" % p])
        A("dve", lambda e, p=p: e.tensor_reduce(out=ssum[p][:], in_=sex[p][:], axis=AX.X, op=ALU.add), r=["sex%d" % p], w=["ssum%d" % p])
        A("dve", lambda e, p=p: e.reciprocal(out=ssum[p][:], in_=ssum[p][:]), r=["ssum%d" % p], w=["ssum%d" % p])
        A("dve", lambda e, p=p, t=t: e.tensor_scalar(out=aff[:, t, :], in0=sex[p][:], scalar1=ssum[p][:], scalar2=None, op0=ALU.mult), r=["sex%d" % p, "ssum%d" % p], w=["aff%d" % t])
    swpipe(NT, [p4_s0, p4_s0b, p4_s1, p4_s1b, p4_s2a, p4_s2b, p4_s3, p4_s3b, p4_s4, p4_s5b, p4_s5c])
    if debug and "h1" in debug:
        dump("h1n", h1b[:], [128, NT, D], BF16)
        dump("aff", aff[:], [128, NT, E], F32)
    sch.barrier()
    ar.release(attnT, convT, wo, wr, wrg, g0, b0, g1, b1, brow, bh, bl, blf, *xt, *zt, *r1, *h1T, *smx, *ssum, *sex)
    ln_small_free(lnB)
    ln_small_free(lnC)
    ar.flush()

    wgb = [ar.alloc("wgb", [128, 8, FG], BF16) for _ in range(NWB)]
    wub = [ar.alloc("wub", [128, 8, FG], BF16) for _ in range(NWB)]
    wdb = [ar.alloc("wdb", [128, FG // 128, D], BF16) for _ in range(NWB)]
    NCH = FF // FG
    FPC = FG // 128

    def load_chunk(c):
        pos_e, fg = divmod(c, NCH)
        eid = ORDER[pos_e]
        s = c % NWB
        if eid < NPC:
            f0, f1 = fg * FG, (fg + 1) * FG
            rg = ["wpc_g%d_%d" % (eid, 0), "wpc_g%d_%d" % (eid, D // 2)]
            ru = ["wpc_u%d_%d" % (eid, 0), "wpc_u%d_%d" % (eid, D // 2)]
            rd = ["wpc_d%d_%d" % (eid, 0), "wpc_d%d_%d" % (eid, FF // 2)]
            DMA("sp", wgb[s][:], wpc[("g", eid)].rearrange("(k p) f -> p k f", p=128)[:, :, f0:f1], r=rg, w=["wg%d" % s])
            DMA("sp", wub[s][:], wpc[("u", eid)].rearrange("(k p) f -> p k f", p=128)[:, :, f0:f1], r=ru, w=["wu%d" % s])
            DMA("sp", wdb[s][:], wpc[("d", eid)][f0:f1, :].rearrange("(j p) d -> p j d", p=128), r=rd, w=["wd%d" % s])
        else:
            DMA("pool", wgb[s][:], wg_d[eid].rearrange("(k p) f -> p k f", p=128)[:, :, fg * FG:(fg + 1) * FG], w=["wg%d" % s])
            DMA("pool", wub[s][:], wu_d[eid].rearrange("(k p) f -> p k f", p=128)[:, :, fg * FG:(fg + 1) * FG], w=["wu%d" % s])
            DMA("pool", wdb[s][:], wd_d[eid][fg * FG:(fg + 1) * FG, :].rearrange("(j p) d -> p j d", p=128), w=["wd%d" % s])

    for c in range(NWB):
        load_chunk(c)

    tp = ar.alloc("tp", [128, NT, 2], BF16)
    DMA("sp", tp[:].rearrange("p t c -> p (t c)"), c_tp_d, w=["tp"])
    A8 = ar.alloc("A8", [128, 256], F32)
    junk = ar.alloc("junk", [128, 256], F32)
    gmat = ar.alloc("gmat", [128, 128], F32)
    cand = ar.alloc("cand", [128, 1], F32)
    cnt = ar.alloc("cnt", [128, 1], F32)
    dlt = ar.alloc("dlt", [128, 1], F32)
    cnt2 = ar.alloc("cnt2", [128, 1], F32)
    thr = ar.alloc("thr", [128, 1], F32)
    mask8 = ar.alloc("mask8", [128, 256], BF16)
    mask_tok = ar.alloc("mask_tok", [128, NT * E], BF16)
    posm = ar.alloc("posm", [128, NT * E], F32)
    ahl = ar.alloc("ahl", [128, NT * E, 4], BF16)
    DMA("sp", gmat[:], c_gmat_d, w=["gmat"])

    affp = ar.alloc("affp", [128, 2, 128], F32)
    for h in range(2):
        A("dve", lambda e, h=h: e.tensor_copy(out=affp[:, h, :].rearrange("p (e g) -> p e g", g=8), in_=aff[:, h::2, :].rearrange("p g e -> p e g")),
          r=["aff%d" % t for t in range(NT)], w=["affp"])

    def f_a8(e):
        for h in range(2):
            i_ = e.transpose(out=bank(0)[:, h * 128:(h + 1) * 128], in_=affp[:, h, :], identity=identf[:])
        return i_
    A("pe", f_a8, r=["affp", "identf"], w=[BK(0)])
    A("dve", lambda e: e.tensor_copy(out=A8[:], in_=bank(0)[:, 0:256]), r=[BK(0)], w=["A8"])
    A("dve", lambda e: e.memset(thr[:], 0.0), w=["thr"])
    A("dve", lambda e: e.memset(cand[:], 0.5), w=["cand"])
    for i in range(1, NBIS + 1):
        step = 2.0 ** (-i)
        pb_ = 1 + (i % 2)
        A("dve", lambda e: e.tensor_scalar(out=junk[:], in0=A8[:], scalar1=cand[:], scalar2=None, op0=ALU.is_ge, op1=ALU.add, accum_out=cnt[:]),
          r=["A8", "cand"], w=["junk", "cnt"])
        A("dve", lambda e: e.tensor_copy(out=cnt2[:], in_=cnt[:]), r=["cnt"], w=["cnt2"])
        A("pe", lambda e, pb_=pb_: e.matmul(bank(pb_)[:, 0:1], lhsT=gmat[:], rhs=cnt2[:], start=True, stop=True), r=["gmat", "cnt2"], w=[BK(pb_)])
        A("dve", lambda e, pb_=pb_, step=step: e.tensor_scalar(out=dlt[:], in0=bank(pb_)[:, 0:1], scalar1=float(CAP) - 0.5, scalar2=step, op0=ALU.is_ge, op1=ALU.mult),
          r=[BK(pb_)], w=["dlt"])
        A("dve", lambda e: e.tensor_tensor(out=thr[:], in0=thr[:], in1=dlt[:], op=ALU.add), r=["thr", "dlt"], w=["thr"])
        A("dve", lambda e, step=step: e.tensor_scalar(out=cand[:], in0=thr[:], scalar1=step * 0.5, scalar2=None, op0=ALU.add), r=["thr"], w=["cand"])
    A("dve", lambda e: e.tensor_scalar(out=mask8[:], in0=A8[:], scalar1=thr[:], scalar2=None, op0=ALU.is_ge), r=["A8", "thr"], w=["mask8"])

    def f_mtok(e):
        for h in range(2):
            i_ = e.transpose(out=bankb(4)[:, h * 128:(h + 1) * 128], in_=mask8[:, h * 128:(h + 1) * 128], identity=identb[:])
        return i_
    A("pe", f_mtok, r=["mask8", "identb"], w=[BK(4)])
    mtv = mask_tok[:].rearrange("p (t e) -> p t e", e=E)
    for h in range(2):
        A("dve", lambda e, h=h: e.tensor_copy(out=mtv[:, h::2, :], in_=bankb(4)[:, h * 128:(h + 1) * 128].rearrange("p (e g) -> p g e", g=8)), r=[BK(4)], w=["mask_tok"])

    def f_pos(e):
        for t in range(NT):
            for t2_ in range(t + 1):
                lhs = ustr[:] if t2_ == t else onesb[:]
                i_ = e.matmul(bank(5)[:, t * 16:(t + 1) * 16], lhsT=lhs, rhs=mask_tok[:, t2_ * 16:(t2_ + 1) * 16], start=(t2_ == 0), stop=(t2_ == t))
        return i_
    A("pe", f_pos, r=["mask_tok", "ustr", "onesb"], w=[BK(5)])
    A("dve", lambda e: e.scalar_tensor_tensor(out=posm[:], in0=bank(5)[:, 0:NT * E], scalar=1.0, in1=mask_tok[:], op0=ALU.add, op1=ALU.mult), r=[BK(5), "mask_tok"], w=["posm"])
    A("dve", lambda e: e.tensor_scalar(out=posm[:], in0=posm[:], scalar1=-1.0, scalar2=None, op0=ALU.add), r=["posm"], w=["posm"])
    affv = aff[:].rearrange("p t e -> p (t e)")
    A("dve", lambda e: e.tensor_copy(out=ahl[:, :, 0], in_=affv), r=["aff%d" % t for t in range(NT)], w=["ahl"])
    A("dve", lambda e: e.tensor_tensor(out=ahl[:, :, 1], in0=affv, in1=ahl[:, :, 0], op=ALU.subtract), r=["ahl"] + ["aff%d" % t for t in range(NT)], w=["ahl"])
    A("dve", lambda e: e.tensor_copy(out=ahl[:].rearrange("p (t e) c -> p t e c", e=E)[:, :, :, 2:4], in_=tp[:].unsqueeze(2).to_broadcast([128, NT, E, 2])), r=["tp", "ahl"], w=["ahl"])
    if debug and "posm" in debug:
        dump("posm", posm[:], [128, NT * E], F32)
    sch.barrier()
    ar.release(A8, junk, gmat, cand, thr, cnt, cnt2, dlt, mask8, mask_tok, tp, affp)
    ar.flush()

    Pm = [ar.alloc("Pm", [128, NT, CAP], BF16) for _ in range(2)]
    gate = [ar.alloc("gate", [128, 2], F32) for _ in range(2)]
    gi = [ar.alloc("gi", [128, 8], F32) for _ in range(2)]
    idxf = [ar.alloc("idxf", [128, 2], F32) for _ in range(2)]
    idxi = [ar.alloc("idxi", [128, 2], I32) for _ in range(2)]
    xgT = ar.alloc("xgT", [128, 8, CAP], BF16)
    yg = [ar.alloc("yg", [128, 2, D], F32) for _ in range(2)]
    sa = [ar.alloc("sa", [128, CAP], F32) for _ in range(2)]
    actT = [ar.alloc("actT", [128, CAP], BF16) for _ in range(4)]
    misc_ctr = [0]

    def misc_bank():
        b_ = 6 + (misc_ctr[0] % 2)
        misc_ctr[0] += 1
        return b_

    def prep_expert(e_):
        pe_ = e_ % 2
        eid = ORDER[e_]
        for t in range(NT):
            A("dve", lambda e, t=t, pe_=pe_, eid=eid: e.tensor_scalar(out=Pm[pe_][:, t, :], in0=iota_c[:], scalar1=posm[:, t * E + eid:t * E + eid + 1], scalar2=None, op0=ALU.is_equal),
              r=["iota_c", "posm"], w=["Pm%d_%d" % (pe_, t)])
        mb = misc_bank()

        def f_gate(e, mb=mb, pe_=pe_, eid=eid):
            for ct in range(2):
                for t in range(NT):
                    i_ = e.matmul(bank(mb)[:, ct * 4:ct * 4 + 4], lhsT=Pm[pe_][:, t, ct * 128:(ct + 1) * 128], rhs=ahl[:, t * E + eid, :], start=(t == 0), stop=(t == NT - 1))
            return i_
        A("pe", f_gate, r=["ahl"] + ["Pm%d_%d" % (pe_, t) for t in range(NT)], w=[BK(mb)])
        A("dve", lambda e, mb=mb, pe_=pe_: e.tensor_copy(out=gi[pe_][:], in_=bank(mb)[:, 0:8]), r=[BK(mb)], w=["gi%d" % pe_])
        giv = gi[pe_][:].rearrange("p (c f) -> p c f", f=4)
        A("dve", lambda e, pe_=pe_, giv=giv: e.tensor_tensor(out=gate[pe_][:], in0=giv[:, :, 0], in1=giv[:, :, 1], op=ALU.add), r=["gi%d" % pe_], w=["gate%d" % pe_])
        A("dve", lambda e, pe_=pe_, giv=giv: e.scalar_tensor_tensor(out=idxf[pe_][:], in0=giv[:, :, 2], scalar=128.0, in1=giv[:, :, 3], op0=ALU.mult, op1=ALU.add),
          r=["gi%d" % pe_], w=["idxf%d" % pe_])
        A("dve", lambda e, pe_=pe_: e.tensor_copy(out=idxi[pe_][:], in_=idxf[pe_][:]), r=["idxf%d" % pe_], w=["idxi%d" % pe_])

    def gather_expert(e_):
        pe_ = e_ % 2
        for dk in range(8):
            mb = misc_bank()

            def f_g(e, mb=mb, dk=dk, pe_=pe_):
                for t in range(NT):
                    i_ = e.matmul(bank(mb)[:, 0:CAP], lhsT=h1b[:, t, dk * 128:(dk + 1) * 128], rhs=Pm[pe_][:, t, :], start=(t == 0), stop=(t == NT - 1))
                return i_
            A("pe", f_g, r=["h1b%d" % t for t in range(NT)] + ["Pm%d_%d" % (pe_, t) for t in range(NT)], w=[BK(mb)])
            A("act", lambda e, mb=mb, dk=dk: e.activation(out=xgT[:, dk, :], in_=bank(mb)[:, 0:CAP], func=AF.Identity, bias=b1pk[:, dk:dk + 1], scale=g1pk[:, dk:dk + 1]),
              r=[BK(mb), "g1pk", "b1pk"], w=["xgT%d" % dk])

    def gu(e_, ft):
        c = e_ * NCH + ft // FPC
        s = c % NWB
        j = ft % FPC
        gb = 4 + (ft % 2)

        def f_gu(e, s=s, j=j, gb=gb):
            for k in range(8):
                e.matmul(bank(gb)[:, 0:CAP], lhsT=wgb[s][:, k, j * 128:(j + 1) * 128], rhs=xgT[:, k, :], start=(k == 0), stop=(k == 7))
            for k in range(8):
                i_ = e.matmul(bank(gb)[:, CAP:2 * CAP], lhsT=wub[s][:, k, j * 128:(j + 1) * 128], rhs=xgT[:, k, :], start=(k == 0), stop=(k == 7))
            return i_
        A("pe", f_gu, r=["wg%d" % s, "wu%d" % s] + ["xgT%d" % dk for dk in range(8)], w=[BK(gb)])
        ps_ = ft % 2
        pa_ = ft % 4
        A("act", lambda e, gb=gb, ps_=ps_: e.activation(out=sa[ps_][:], in_=bank(gb)[:, 0:CAP], func=AF.Silu), r=[BK(gb)], w=["sa%d" % ps_])
        A("dve", lambda e, gb=gb, ps_=ps_, pa_=pa_: e.tensor_tensor(out=actT[pa_][:], in0=bank(gb)[:, CAP:2 * CAP], in1=sa[ps_][:], op=ALU.mult), r=[BK(gb), "sa%d" % ps_], w=["actT%d" % pa_])

    def down(e_, ft):
        c = e_ * NCH + ft // FPC
        s = c % NWB
        j = ft % FPC
        pa_ = ft % 4

        def f_d(e, s=s, j=j, pa_=pa_, ft=ft):
            for ct in range(2):
                for dh in range(2):
                    i_ = e.matmul(pst[ct][:, dh * 512:(dh + 1) * 512], lhsT=actT[pa_][:, ct * 128:(ct + 1) * 128], rhs=wdb[s][:, j, dh * 512:(dh + 1) * 512], start=(ft == 0), stop=(ft == NT - 1))
            return i_
        A("pe", f_d, r=["actT%d" % pa_, "wd%d" % s], w=[BK(0), BK(1), BK(2), BK(3)])
        if j == FPC - 1 and c + NWB < E * NCH:
            load_chunk(c + NWB)

    def yevac_scatter(e_):
        pe_ = e_ % 2
        for ct in range(2):
            A("act", lambda e, ct=ct, pe_=pe_: e.activation(out=yg[pe_][:, ct, :], in_=pst[ct][:], func=AF.Copy, scale=gate[pe_][:, ct:ct + 1]),
              r=[BK(2 * ct), BK(2 * ct + 1), "gate%d" % pe_], w=["yg%d_%d" % (pe_, ct)])
        for ct in range(2):
            A("pool", lambda e, ct=ct, pe_=pe_: e.indirect_dma_start(out=macc, out_offset=bass.IndirectOffsetOnAxis(ap=idxi[pe_][:, ct:ct + 1], axis=0),
                                                                     in_=yg[pe_][:, ct, :], in_offset=None, bounds_check=S - 1, oob_is_err=True, compute_op=ALU.add),
              r=["yg%d_%d" % (pe_, ct), "idxi%d" % pe_, "macc"], w=["macc"], dma=True)

    SKEW = 2
    prep_expert(0)
    gather_expert(0)
    for e_ in range(E):
        for ft in range(NT):
            gu(e_, ft)
            if ft >= SKEW:
                down(e_, ft - SKEW)
        if e_ + 1 < E:
            prep_expert(e_ + 1)
        for ft in range(NT - SKEW, NT):
            down(e_, ft)
        yevac_scatter(e_)
        if e_ + 1 < E:
            gather_expert(e_ + 1)
    sch.barrier()
    ar.release(h1b, *Pm, *gate, *gi, *idxf, *idxi, xgT, *yg, *sa, *actT, *wgb, *wub, *wdb, posm, ahl, aff, g1pk, b1pk)
    ar.flush()

    g2 = ar.alloc("g2", [128, D], F32)
    b2 = ar.alloc("b2", [128, D], F32)
    DMA("sp", g2[:], g2_d, w=["g2"])
    DMA("sp", b2[:], b2_d, w=["b2"])
    NR7 = 6
    rt = [ar.alloc("rt", [128, D], F32) for _ in range(NR7)]
    ot = [ar.alloc("ot", [128, D], F32) for _ in range(NR7)]
    lnD = ln_small("lnD", NR7)

    def p7_s0(t):
        p = t % NR7
        DMA("sp", rt[p][:], macc[t * 128:(t + 1) * 128, :], r=["macc"], w=["rt%d" % p])

    def p7_s0b(t):
        pass

    def p7_s1(t):
        p = t % NR7
        ln_part1a(lnD[p], rt[p][:], ["rt%d" % p])

    def p7_s1b(t):
        p = t % NR7
        ln_part1b(lnD[p], rt[p][:], ["rt%d" % p], rt[p][:], ["rt%d" % p])

    def p7_s2(t):
        p = t % NR7
        ln_part2(rt[p][:], ["rt%d" % p], ot[p][:], ["ot%d" % p], g2, b2, "g2", "b2")
        DMA("sp", out_d[t * 128:(t + 1) * 128, :], ot[p][:], r=["ot%d" % p])
    swpipe(NT, [p7_s0, p7_s0b, p7_s1, p7_s1b, p7_s2])
    emit_program(sch, nc)
    return nc, dbg_outs


def _consts():
    bf = ml_dtypes.bfloat16
    ident = np.eye(128, dtype=np.float32)
    iota = np.broadcast_to(np.arange(256, dtype=np.float32)[None, :], (128, 256)).copy()
    iotap = np.stack([np.arange(128, dtype=np.float32), np.arange(128, dtype=np.float32) + 128.0], axis=1)
    ustr = np.triu(np.ones((128, 128), dtype=np.float32), k=1)
    half = 16
    invf = (np.float32(10000.0) ** (-np.arange(half, dtype=np.float32) / np.float32(half))).astype(np.float32)
    invf = np.concatenate([invf] * 8).reshape(128, 1)
    tp = np.zeros((128, 16, 2), dtype=np.float32)
    tp[:, :, 0] = np.arange(16, dtype=np.float32)[None, :]
    tp[:, :, 1] = np.arange(128, dtype=np.float32)[:, None]
    return {
        "c_identf": ident, "c_identb": ident.astype(bf), "c_iota": iota, "c_iotap": np.ascontiguousarray(iotap),
        "c_ustr": ustr.astype(bf), "c_invf": invf, "c_tp": tp.reshape(128, 32).astype(bf), "c_gmat": np.kron(np.eye(16, dtype=np.float32), np.ones((8, 8), dtype=np.float32)),
    }


def _bc(v):
    return np.ascontiguousarray(np.broadcast_to(np.asarray(v, dtype=np.float32).reshape(1, -1), (128, v.size)))


def _pk(v, k):
    return np.ascontiguousarray(np.asarray(v, dtype=np.float32).reshape(k, 128).T)


_CACHE = {}


def make_in_maps(inputs, cores):
    f = lambda a: np.ascontiguousarray(np.asarray(a))
    shared = {
        "emb_ln_g": _bc(f(inputs["emb_ln_g"])), "emb_ln_b": _bc(f(inputs["emb_ln_b"])),
        "w_in": f(inputs["w_in"])[0], "q_norm_g": _pk(f(inputs["q_norm_g"])[0], 3), "w_qb": f(inputs["w_qb"])[0],
        "kv_norm_g": _pk(f(inputs["kv_norm_g"])[0], 2), "w_kvb": f(inputs["w_kvb"])[0],
        "conv_w": np.ascontiguousarray(f(inputs["conv_w"])[0].reshape(31, 4, 128).transpose(2, 1, 0).reshape(128, 4 * 31)),
        "conv_b": _pk(f(inputs["conv_b"])[0], 4), "conv_ln_g": _pk(f(inputs["conv_ln_g"])[0], 4), "conv_ln_b": _pk(f(inputs["conv_ln_b"])[0], 4),
        "w_o": f(inputs["w_o"])[0], "ln1_g": _bc(f(inputs["ln1_g"])[0]), "ln1_b": _bc(f(inputs["ln1_b"])[0]),
        "ln1_g_pk": _pk(f(inputs["ln1_g"])[0], 8), "ln1_b_pk": _pk(f(inputs["ln1_b"])[0], 8),
        "w_router": f(inputs["w_router"])[0], "w_gate": f(inputs["w_gate"])[0], "w_up": f(inputs["w_up"])[0], "w_down": f(inputs["w_down"])[0],
        "ln2_g": _bc(f(inputs["ln2_g"])[0]), "ln2_b": _bc(f(inputs["ln2_b"])[0]),
    }
    shared.update(_consts())
    x = f(inputs["x"])
    pos = f(inputs["positions"]).astype(np.int32)
    maps = []
    for c in cores:
        m = dict(shared)
        m["x"] = np.ascontiguousarray(x[c])
        m["pos"] = np.ascontiguousarray(np.broadcast_to(pos[c].reshape(4, 1, 512), (4, 32, 512)).reshape(128, 512))
        maps.append(m)
    return maps


def kernel(**inputs):
    if "nc" not in _CACHE:
        _CACHE["nc"] = build()[0]
    nc = _CACHE["nc"]
    cores = list(range(8))
    in_maps = make_in_maps(inputs, cores)
    res = run_bass_kernel_spmd(nc, in_maps, core_ids=cores)
    out = np.stack([np.asarray(r["out"]) for r in res.results], axis=0)
    return out.astype(np.float32)
```

```python
import numpy as np
import ml_dtypes
import concourse.bass as bass
import concourse.mybir as mybir
from concourse.bass_utils import run_bass_kernel_spmd

F32 = mybir.dt.float32
BF16 = mybir.dt.bfloat16
I32 = mybir.dt.int32
AF = mybir.ActivationFunctionType
ALU = mybir.AluOpType
AX = mybir.AxisListType

S = 2048
D = 1024
NT = 16
H = 8
E = 16
CAP = 256
FF = 2048
ALPHA = float(2.0 ** 0.25)
LN_EPS = 1e-5
RMS_EPS = 1e-6
SCALE = float(96.0 ** -0.5)
PI = float(np.pi)
TWO_PI = float(2.0 * np.pi)
NBIS = 28
FG = 512
NWB = 5
NPC = 10
ORDER = [10, 0, 11, 1, 12, 2, 13, 3, 14, 4, 15, 5, 6, 7, 8, 9]


class _Op:
    __slots__ = ("eng", "fn", "deps", "sig", "sigval", "is_dma", "dsem", "dval")


class Sched:
    ENG = ("pe", "act", "dve", "pool", "sp")

    def __init__(self, nc, n_dma_sems=42):
        self.nc = nc
        self.ops = []
        self.last_w = {}
        self.readers = {}
        self.n_dma = n_dma_sems
        self.dma_rr = 0
        self.dma_rrq = {}
        self.dma_last = [None] * n_dma_sems
        self.dma_count = [0] * n_dma_sems
        self.eng_last = {e: None for e in self.ENG}
        self.out_dmas = []
        self.capture = None
        self.precast_mode = False

    def add(self, eng, fn, r=(), w=(), dma=False):
        if self.capture is not None:
            self.capture.append((eng, fn, tuple(r), tuple(w), dma))
            return None
        op = _Op()
        op.eng = eng
        op.fn = fn
        op.deps = {}
        op.sig = False
        op.sigval = 0
        op.is_dma = dma
        for x in r:
            wr = self.last_w.get(x)
            if wr is not None:
                op.deps[wr] = "raw"
        for x in w:
            wr = self.last_w.get(x)
            if wr is not None and wr not in op.deps:
                op.deps[wr] = "waw"
            for rd in self.readers.get(x, ()):
                if rd is not op and rd not in op.deps:
                    op.deps[rd] = "war"
        for x in r:
            self.readers.setdefault(x, []).append(op)
        for x in w:
            self.last_w[x] = op
            self.readers[x] = []
        if dma:
            third = self.n_dma // 3
            qk = "pc" if self.precast_mode else eng
            base = {"sp": 0, "pool": third, "pc": 2 * third}.get(qk, 0)
            k = base + self.dma_rrq.get(qk, 0)
            self.dma_rrq[qk] = (self.dma_rrq.get(qk, 0) + 1) % third
            prev = self.dma_last[k]
            if prev is not None:
                op.deps[prev] = "raw"
            self.dma_count[k] += 1
            op.dsem = k
            op.dval = 16 * self.dma_count[k]
            self.dma_last[k] = op
        else:
            self.eng_last[eng] = op
        self.ops.append(op)
        return op

    def barrier(self):
        lasts = [o for o in self.eng_last.values() if o is not None]
        third = self.n_dma // 3
        lasts += [o for k_, o in enumerate(self.dma_last) if o is not None and k_ < 2 * third]
        for e in self.ENG:
            op = _Op()
            op.eng = e
            op.fn = None
            op.deps = {o: "raw" for o in lasts}
            op.sig = False
            op.sigval = 0
            op.is_dma = False
            self.ops.append(op)


def _mk_sems(nc, names):
    import contextlib
    st = contextlib.ExitStack()
    sems = [st.enter_context(nc.semaphore(n)) for n in names]
    return st, sems


def emit_program(sch, nc):
    engobj = {"pe": nc.tensor, "act": nc.scalar, "dve": nc.vector, "pool": nc.gpsimd, "sp": nc.sync}

    def skip(op, d, kind):
        if d.is_dma or op.is_dma:
            return False
        if d.eng != op.eng:
            return False
        if op.fn is None:
            return True
        if op.eng == "pe":
            return True
        return False

    for op in sch.ops:
        for d, kind in op.deps.items():
            if d.is_dma or skip(op, d, kind):
                continue
            d.sig = True
    cnt = {e: 0 for e in Sched.ENG}
    for op in sch.ops:
        if op.fn is not None and (not op.is_dma) and op.sig:
            cnt[op.eng] += 1
            op.sigval = cnt[op.eng]
    st, sems = _mk_sems(nc, ["se_" + e for e in Sched.ENG] + ["sd_%d" % i for i in range(sch.n_dma)])
    esem = {e: sems[i] for i, e in enumerate(Sched.ENG)}
    dsem = sems[len(Sched.ENG):]
    waited = {e: {} for e in Sched.ENG}
    with st:
        for op in sch.ops:
            eo = engobj[op.eng]
            need = {}
            for d, kind in op.deps.items():
                if skip(op, d, kind):
                    continue
                if d.is_dma:
                    key = ("d", d.dsem)
                    val = d.dval
                else:
                    key = ("e", d.eng)
                    val = d.sigval
                if need.get(key, 0) < val:
                    need[key] = val
            for key, val in need.items():
                if waited[op.eng].get(key, 0) >= val:
                    continue
                waited[op.eng][key] = val
                sem = dsem[key[1]] if key[0] == "d" else esem[key[1]]
                eo.wait_ge(sem, val)
            if op.fn is None:
                continue
            inst = op.fn(eo)
            if op.is_dma:
                inst.then_inc(dsem[op.dsem], 16)
            elif op.sig:
                inst.then_inc(esem[op.eng], 1)
        for k in range(sch.n_dma):
            if sch.dma_count[k] > 0:
                nc.sync.wait_ge(dsem[k], 16 * sch.dma_count[k])


class Arena:
    LO = 16512
    HI = 229344

    def __init__(self, nc):
        self.nc = nc
        self.free = [(self.LO, self.HI)]
        self.pending = []
        self.n = 0
        self.live = {}

    def alloc(self, name, shape, dtype):
        esz = 2 if dtype == BF16 else 4
        nbytes = esz
        for d_ in shape[1:]:
            nbytes *= d_
        nbytes = (nbytes + 63) // 64 * 64
        for i, (lo, hi) in enumerate(self.free):
            if hi - lo >= nbytes:
                self.free[i] = (lo + nbytes, hi)
                self.n += 1
                t = self.nc.alloc_sbuf_tensor_at("%s_%d" % (name, self.n), list(shape), dtype, offset=lo)
                self.live[id(t)] = (lo, lo + nbytes)
                return t
        raise RuntimeError("SBUF arena out of memory for %s (%d bytes) free=%s" % (name, nbytes, self.free))

    def release(self, *tiles):
        for t in tiles:
            self.pending.append(self.live.pop(id(t)))

    def flush(self):
        segs = sorted(self.free + self.pending)
        self.pending = []
        out = []
        for lo, hi in segs:
            if lo == hi:
                continue
            if out and out[-1][1] == lo:
                out[-1] = (out[-1][0], hi)
            else:
                out.append((lo, hi))
        self.free = out


def build(debug=None):
    nc = bass.Bass("TRN2", target_bir_lowering=False)
    sch = Sched(nc)
    ar = Arena(nc)
    A = sch.add
    dbg_outs = []

    def din(name, shape, dtype=F32):
        return nc.dram_tensor(name, list(shape), dtype, kind="ExternalInput").ap()

    x_d = din("x", [S, D])
    pos_d = din("pos", [128, 512], I32)
    g0_d = din("emb_ln_g", [128, D])
    b0_d = din("emb_ln_b", [128, D])
    w_in_d = din("w_in", [D, 1696])
    qg_d = din("q_norm_g", [128, 3])
    wqb_d = din("w_qb", [384, 768])
    kvg_d = din("kv_norm_g", [128, 2])
    wkvb_d = din("w_kvb", [256, 1024])
    cw_d = din("conv_w", [128, 4 * 31])
    cb_d = din("conv_b", [128, 4])
    clg_d = din("conv_ln_g", [128, 4])
    clb_d = din("conv_ln_b", [128, 4])
    wo_d = din("w_o", [D, D])
    g1_d = din("ln1_g", [128, D])
    b1_d = din("ln1_b", [128, D])
    wr_d = din("w_router", [D, E])
    g1pk_d = din("ln1_g_pk", [128, 8])
    b1pk_d = din("ln1_b_pk", [128, 8])
    wg_d = din("w_gate", [E, D, FF])
    wu_d = din("w_up", [E, D, FF])
    wd_d = din("w_down", [E, FF, D])
    g2_d = din("ln2_g", [128, D])
    b2_d = din("ln2_b", [128, D])
    c_identf_d = din("c_identf", [128, 128])
    c_identb_d = din("c_identb", [128, 128], BF16)
    c_iota_d = din("c_iota", [128, 256])
    c_iotap_d = din("c_iotap", [128, 2])
    c_ustr_d = din("c_ustr", [128, 128], BF16)
    c_invf_d = din("c_invf", [128, 1])
    c_tp_d = din("c_tp", [128, NT * 2], BF16)
    c_gmat_d = din("c_gmat", [128, 128])
    out_d = nc.dram_tensor("out", [S, D], F32, kind="ExternalOutput").ap()

    def dump(name, tile_ap, shape, dtype=F32):
        if debug is None or name not in debug:
            return
        t = nc.dram_tensor("dbg_" + name, list(shape), dtype, kind="ExternalOutput").ap()
        dbg_outs.append("dbg_" + name)
        sch.barrier()
        A("sp", lambda e: e.dma_start(out=t, in_=tile_ap), r=[], w=[], dma=True)
        sch.barrier()

    def DMA(q, out, in_, r=(), w=()):
        return A(q, lambda e: e.dma_start(out=out, in_=in_), r=r, w=w, dma=True)

    pst = [nc.alloc_psum_tensor("ps%d" % i, [128, 1024], F32) for i in range(4)]

    def bank(i):
        return pst[i // 2][:, (i % 2) * 512:(i % 2 + 1) * 512]

    def bankb(i):
        return pst[i // 2].bitcast(BF16)[:, (i % 2) * 1024:(i % 2 + 1) * 1024]

    def BK(i):
        return "psum%d" % i

    wpc = {}
    for e_ in range(NPC):
        wpc[("g", e_)] = nc.dram_tensor("wpc_g%d" % e_, [D, FF], BF16).ap()
        wpc[("u", e_)] = nc.dram_tensor("wpc_u%d" % e_, [D, FF], BF16).ap()
        wpc[("d", e_)] = nc.dram_tensor("wpc_d%d" % e_, [FF, D], BF16).ap()
    pc_jobs = []
    for e_ in range(NPC):
        for kind, src in (("g", wg_d), ("u", wu_d), ("d", wd_d)):
            rows = D if kind != "d" else FF
            for hh in range(2):
                pc_jobs.append((kind, e_, src, hh * rows // 2, (hh + 1) * rows // 2))
    pc_pos = [0]

    def precast(n):
        sch.precast_mode = True
        for _ in range(n):
            if pc_pos[0] >= len(pc_jobs):
                break
            kind, e_, src, r0, r1_ = pc_jobs[pc_pos[0]]
            pc_pos[0] += 1
            DMA("pool", wpc[(kind, e_)][r0:r1_, :], src[e_][r0:r1_, :], w=["wpc_%s%d_%d" % (kind, e_, r0)])
        sch.precast_mode = False

    identf = ar.alloc("identf", [128, 128], F32)
    identb = ar.alloc("identb", [128, 128], BF16)
    onesf = ar.alloc("onesf", [128, 128], F32)
    onesb = ar.alloc("onesb", [128, 128], BF16)
    iota_c = ar.alloc("iota_c", [128, 256], F32)
    iota_p = ar.alloc("iota_p", [128, 2], F32)
    ustr = ar.alloc("ustr", [128, 128], BF16)
    DMA("sp", identf[:], c_identf_d, w=["identf"])
    DMA("sp", identb[:], c_identb_d, w=["identb"])
    DMA("sp", iota_c[:], c_iota_d, w=["iota_c"])
    DMA("sp", iota_p[:], c_iotap_d, w=["iota_p"])
    DMA("sp", ustr[:], c_ustr_d, w=["ustr"])
    A("pool", lambda e: e.memset(onesf[:], 1.0), w=["onesf"])
    A("pool", lambda e: e.memset(onesb[:], 1.0), w=["onesb"])

    def swpipe(n, stages):
        ns = len(stages)
        for it in range(n + ns - 1):
            lists = []
            for si in range(ns):
                t_ = it - si
                if 0 <= t_ < n:
                    sch.capture = []
                    stages[si](t_)
                    lists.append(sch.capture)
                    sch.capture = None
            pos_ = [0] * len(lists)
            left = sum(len(l_) for l_ in lists)
            while left:
                for li, l_ in enumerate(lists):
                    if pos_[li] < len(l_):
                        eng, fn, r_, w_, dma_ = l_[pos_[li]]
                        pos_[li] += 1
                        left -= 1
                        sch.add(eng, fn, r_, w_, dma_)

    def ln_small(nm, nrot):
        return [dict(stats=ar.alloc(nm + "st", [128, 2, 6], F32), mv=ar.alloc(nm + "mv", [128, 2], F32), std=ar.alloc(nm + "sd", [128, 1], F32),
                     rstd=ar.alloc(nm + "rs", [128, 1], F32), nmr=ar.alloc(nm + "nm", [128, 1], F32), name="%s%d_" % (nm, i_)) for i_ in range(nrot)]

    def ln_small_free(lst):
        for d_ in lst:
            ar.release(d_["stats"], d_["mv"], d_["std"], d_["rstd"], d_["nmr"])

    def ln_part1a(T, src, src_res):
        n = T["name"]
        stats, mv, std, rstd, nmr = T["stats"], T["mv"], T["std"], T["rstd"], T["nmr"]

        def f_stats(e):
            e.bn_stats(out=stats[:, 0, :], in_=src[:, 0:512])
            return e.bn_stats(out=stats[:, 1, :], in_=src[:, 512:1024])
        A("dve", f_stats, r=list(src_res), w=[n + "st"])
        A("dve", lambda e: e.bn_aggr(out=mv[:], in_=stats[:]), r=[n + "st"], w=[n + "mv"])
        A("act", lambda e: e.activation(out=std[:], in_=mv[:, 1:2], func=AF.Ln, bias=epsln[:], scale=1.0), r=[n + "mv", "epsln"], w=[n + "sd"])
        A("act", lambda e: e.activation(out=rstd[:], in_=std[:], func=AF.Exp, scale=-0.5), r=[n + "sd"], w=[n + "rs"])
        A("dve", lambda e: e.scalar_tensor_tensor(out=nmr[:], in0=mv[:, 0:1], scalar=-1.0, in1=rstd[:], op0=ALU.mult, op1=ALU.mult), r=[n + "mv", n + "rs"], w=[n + "nm"])

    def ln_part1b(T, src, src_res, xn, xn_res):
        n = T["name"]
        rstd, nmr = T["rstd"], T["nmr"]
        A("act", lambda e: e.activation(out=xn, in_=src, func=AF.Identity, bias=nmr[:], scale=rstd[:]), r=list(src_res) + [n + "nm", n + "rs"], w=list(xn_res))

    def ln_part2(xn, xn_res, dst, dst_res, g_bc, b_bc, gres, bres):
        A("pool", lambda e: e.tensor_tensor(out=xn, in0=xn, in1=g_bc[:], op=ALU.mult), r=list(xn_res) + [gres], w=list(xn_res))
        A("dve", lambda e: e.tensor_tensor(out=dst, in0=xn, in1=b_bc[:], op=ALU.add), r=list(xn_res) + [bres], w=list(dst_res))

    epsln = ar.alloc("epsln", [128, 1], F32)
    epsrms = ar.alloc("epsrms", [128, 1], F32)
    A("pool", lambda e: e.memset(epsln[:], LN_EPS), w=["epsln"])
    A("pool", lambda e: e.memset(epsrms[:], RMS_EPS), w=["epsrms"])

    wq_f = ar.alloc("wq_f", [128, 3, 768], F32)
    wkv_f = ar.alloc("wkv_f", [128, 2, 1024], F32)
    qg = ar.alloc("qg", [128, 3], F32)
    kvg = ar.alloc("kvg", [128, 2], F32)
    Wq2 = ar.alloc("Wq2", [128, 3, 8, 128], BF16)
    Wk2 = ar.alloc("Wk2", [128, 2, 8, 64], BF16)
    Wv = ar.alloc("Wv", [128, 2, 8, 64], BF16)
    DMA("sp", wq_f[:], wqb_d.rearrange("(k p) f -> p k f", p=128), w=["wq_f"])
    DMA("sp", wkv_f[:], wkvb_d.rearrange("(k p) f -> p k f", p=128), w=["wkv_f"])
    DMA("sp", qg[:], qg_d, w=["qg"])
    DMA("sp", kvg[:], kvg_d, w=["kvg"])
    for k in range(3):
        src = wq_f[:, k, :].rearrange("p (h c) -> p h c", c=96)
        sc = qg[:, k:k + 1]
        A("dve", lambda e, src=src, sc=sc, k=k: e.tensor_scalar(out=Wq2[:, k, :, 64:96], in0=src[:, :, 64:96], scalar1=sc, scalar2=None, op0=ALU.mult),
          r=["wq_f", "qg"], w=["Wq2"])
        A("dve", lambda e, src=src, sc=sc, k=k: e.tensor_scalar(out=Wq2[:, k, :, 0:64], in0=src[:, :, 0:64], scalar1=sc, scalar2=None, op0=ALU.mult),
          r=["wq_f", "qg"], w=["Wq2"])
        A("dve", lambda e, src=src, sc=sc, k=k: e.tensor_scalar(out=Wq2[:, k, :, 96:112], in0=src[:, :, 80:96], scalar1=sc, scalar2=-1.0, op0=ALU.mult, op1=ALU.mult),
          r=["wq_f", "qg"], w=["Wq2"])
        A("dve", lambda e, src=src, sc=sc, k=k: e.tensor_scalar(out=Wq2[:, k, :, 112:128], in0=src[:, :, 64:80], scalar1=sc, scalar2=None, op0=ALU.mult),
          r=["wq_f", "qg"], w=["Wq2"])
    for k in range(2):
        src = wkv_f[:, k, :].rearrange("p (h c) -> p h c", c=128)
        sc = kvg[:, k:k + 1]
        A("dve", lambda e, src=src, sc=sc, k=k: e.tensor_scalar(out=Wk2[:, k, :, :], in0=src[:, :, 0:64], scalar1=sc, scalar2=None, op0=ALU.mult),
          r=["wkv_f", "kvg"], w=["Wk2"])
        A("dve", lambda e, src=src, sc=sc, k=k: e.tensor_scalar(out=Wv[:, k, :, :], in0=src[:, :, 64:128], scalar1=sc, scalar2=None, op0=ALU.mult),
          r=["wkv_f", "kvg"], w=["Wv"])

    cosT = ar.alloc("cosT", [96, S], F32)
    sinT = ar.alloc("sinT", [96, S], F32)
    pos_i = ar.alloc("pos_i", [128, 512], I32)
    ang = ar.alloc("ang", [128, 512], F32)
    rt0 = ar.alloc("rt0", [128, 512], F32)
    rt1 = ar.alloc("rt1", [128, 512], F32)
    rti = ar.alloc("rti", [128, 512], I32)
    rsc = [ar.alloc("rsc", [128, 512], F32) for _ in range(2)]
    invf = ar.alloc("invf", [128, 1], F32)
    DMA("sp", pos_i[:], pos_d, w=["pos_i"])
    DMA("sp", invf[:], c_invf_d, w=["invf"])
    A("dve", lambda e: e.tensor_copy(out=ang[:], in_=pos_i[:]), r=["pos_i"], w=["ang"])
    A("dve", lambda e: e.tensor_scalar(out=ang[:], in0=ang[:], scalar1=invf[:], scalar2=None, op0=ALU.mult), r=["ang", "invf"], w=["ang"])

    def range_reduce_sin(dst, dres, shift, tmp, tres):
        A("dve", lambda e: e.tensor_scalar(out=rt0[:], in0=ang[:], scalar1=shift, scalar2=1.0 / TWO_PI, op0=ALU.add, op1=ALU.mult), r=["ang"], w=["rt0"])
        A("dve", lambda e: e.tensor_copy(out=rti[:], in_=rt0[:]), r=["rt0"], w=["rti"])
        A("dve", lambda e: e.tensor_copy(out=rt0[:], in_=rti[:]), r=["rti"], w=["rt0"])
        A("dve", lambda e: e.tensor_scalar(out=rt1[:], in0=ang[:], scalar1=shift, scalar2=None, op0=ALU.add), r=["ang"], w=["rt1"])
        A("dve", lambda e: e.scalar_tensor_tensor(out=rt1[:], in0=rt0[:], scalar=-TWO_PI, in1=rt1[:], op0=ALU.mult, op1=ALU.add), r=["rt0", "rt1"], w=["rt1"])
        A("dve", lambda e: e.tensor_scalar(out=rt0[:], in0=rt1[:], scalar1=PI, scalar2=None, op0=ALU.is_gt), r=["rt1"], w=["rt0"])
        A("dve", lambda e: e.scalar_tensor_tensor(out=rt1[:], in0=rt0[:], scalar=-TWO_PI, in1=rt1[:], op0=ALU.mult, op1=ALU.add), r=["rt0", "rt1"], w=["rt1"])
        A("dve", lambda e: e.tensor_scalar(out=rt0[:], in0=rt1[:], scalar1=-PI, scalar2=None, op0=ALU.is_lt), r=["rt1"], w=["rt0"])
        A("dve", lambda e: e.scalar_tensor_tensor(out=rt1[:], in0=rt0[:], scalar=TWO_PI, in1=rt1[:], op0=ALU.mult, op1=ALU.add), r=["rt0", "rt1"], w=["rt1"])
        A("dve", lambda e: e.tensor_scalar(out=rt1[:], in0=rt1[:], scalar1=-3.1415925, scalar2=3.1415925, op0=ALU.max, op1=ALU.min), r=["rt1"], w=["rt1"])
        A("act", lambda e: e.activation(out=tmp[:], in_=rt1[:], func=AF.Sin), r=["rt1"], w=[tres])
        for blk in range(4):
            DMA("sp", dst[64:96, blk * 512:(blk + 1) * 512], tmp[blk * 32:(blk + 1) * 32, :], r=[tres], w=[dres])

    range_reduce_sin(sinT, "sinT", 0.0, rsc[0], "rsc0")
    range_reduce_sin(cosT, "cosT", PI / 2.0, rsc[1], "rsc1")

    cw = ar.alloc("cw", [128, 4 * 31], F32)
    Dg = ar.alloc("Dg", [128, 4 * 31, 128], BF16)
    DMA("sp", cw[:], cw_d, w=["cw"])
    for m in range(4):
        A("dve", lambda e, m=m: e.tensor_tensor(out=Dg[:, m * 31:(m + 1) * 31, :], in0=identb[:].unsqueeze(1).to_broadcast([128, 31, 128]),
                                               in1=cw[:, m * 31:(m + 1) * 31].unsqueeze(2).to_broadcast([128, 31, 128]), op=ALU.mult),
          r=["identb", "cw"], w=["Dg%d" % m])

    hT = ar.alloc("hT", [128, 8, S], BF16)
    g0 = ar.alloc("g0", [128, D], F32)
    b0 = ar.alloc("b0", [128, D], F32)
    DMA("sp", g0[:], g0_d, w=["g0"])
    DMA("sp", b0[:], b0_d, w=["b0"])
    NR1 = 6
    xt = [ar.alloc("xt", [128, D], F32) for _ in range(NR1)]
    hb = [ar.alloc("hb", [128, D], BF16) for _ in range(NR1)]
    lnA = ln_small("lnA", NR1)

    def p1_s0(t):
        p = t % NR1
        DMA("sp", xt[p][:], x_d[t * 128:(t + 1) * 128, :], w=["xt%d" % p])

    def p1_s0b(t):
        pass

    def p1_s1(t):
        p = t % NR1
        ln_part1a(lnA[p], xt[p][:], ["xt%d" % p])

    def p1_s1b(t):
        p = t % NR1
        ln_part1b(lnA[p], xt[p][:], ["xt%d" % p], xt[p][:], ["xt%d" % p])

    def p1_s2(t):
        p = t % NR1
        ln_part2(xt[p][:], ["xt%d" % p], hb[p][:], ["hb%d" % p], g0, b0, "g0", "b0")

    def p1_s3(t):
        p = t % NR1
        pb_ = t % 2

        def f_tr(e, p=p, pb_=pb_):
            for k in range(8):
                i_ = e.transpose(out=bankb(pb_)[:, k * 128:(k + 1) * 128], in_=hb[p][:, k * 128:(k + 1) * 128], identity=identb[:])
            return i_
        A("pe", f_tr, r=["hb%d" % p, "identb"], w=[BK(pb_)])
        A("act", lambda e, pb_=pb_, t=t: e.activation(out=hT[:, :, t * 128:(t + 1) * 128], in_=bankb(pb_).rearrange("p (k c) -> p k c", c=128), func=AF.Copy),
          r=[BK(pb_)], w=["hT%d" % (t // 4)])
    swpipe(NT, [p1_s0, p1_s0b, p1_s1, p1_s1b, p1_s2, p1_s3])
    if debug and "hT" in debug:
        dump("hT", hT[:], [128, 8, S], BF16)
    sch.barrier()
    ar.release(wq_f, wkv_f, qg, kvg, pos_i, ang, rt0, rt1, rti, invf, *rsc, *xt, *hb, g0, b0)
    ln_small_free(lnA)
    ar.flush()

    convT = ar.alloc("convT", [128, 4, S], BF16)
    wc = ar.alloc("wc", [128, 8, 1024], BF16)
    hcp = ar.alloc("hcp", [128, 4, S + 30], BF16)
    cb = ar.alloc("cb", [128, 4], F32)
    clg = ar.alloc("clg", [128, 4], F32)
    clb = ar.alloc("clb", [128, 4], F32)
    sig = [ar.alloc("sig", [128, 512], F32) for _ in range(2)]
    DMA("pool", wc[:], w_in_d.rearrange("(k p) f -> p k f", p=128)[:, :, 672:1696], w=["wc"])
    DMA("sp", cb[:], cb_d, w=["cb"])
    DMA("sp", clg[:], clg_d, w=["clg"])
    DMA("sp", clb[:], clb_d, w=["clb"])
    A("pool", lambda e: e.memset(hcp[:, :, 0:15], 0.0), w=["hcp_lo"])
    A("pool", lambda e: e.memset(hcp[:, :, S + 15:S + 30], 0.0), w=["hcp_hi"])
    precast(16)
    it = 0
    for b in range(4):
        for m in range(4):
            pa = (it % 2) * 2
            pg = pa + 1
            sp_ = it % 2
            it += 1

            def f_glu(e, m=m, b=b, pa=pa, pg=pg):
                for k in range(8):
                    e.matmul(bank(pa), lhsT=wc[:, k, m * 128:(m + 1) * 128], rhs=hT[:, k, b * 512:(b + 1) * 512], start=(k == 0), stop=(k == 7))
                for k in range(8):
                    i_ = e.matmul(bank(pg), lhsT=wc[:, k, 512 + m * 128:512 + (m + 1) * 128], rhs=hT[:, k, b * 512:(b + 1) * 512], start=(k == 0), stop=(k == 7))
                return i_
            A("pe", f_glu, r=["wc", "hT%d" % b], w=[BK(pa), BK(pg)])
            A("act", lambda e, pg=pg, sp_=sp_: e.activation(out=sig[sp_][:], in_=bank(pg), func=AF.Sigmoid), r=[BK(pg)], w=["sig%d" % sp_])
            A("dve", lambda e, pa=pa, sp_=sp_, m=m, b=b: e.tensor_tensor(out=hcp[:, m, 15 + b * 512:15 + (b + 1) * 512], in0=bank(pa), in1=sig[sp_][:], op=ALU.mult),
              r=[BK(pa), "sig%d" % sp_], w=["hcp%d_%d" % (m, b)])
    yc = [ar.alloc("yc", [128, 4, 512], F32) for _ in range(2)]
    ysq = [ar.alloc("ysq", [128, 512], F32) for _ in range(2)]
    cmean = [ar.alloc("cmean", [128, 512], F32) for _ in range(2)]
    cm2 = [ar.alloc("cm2", [128, 512], F32) for _ in range(2)]
    crstd = [ar.alloc("crstd", [128, 512], F32) for _ in range(2)]
    ctmp = [ar.alloc("ctmp", [128, 512], F32) for _ in range(2)]
    seq = [(b, m) for b in range(4) for m in range(4)]

    def conv_step(i):
        b, m = seq[i]
        pb = b % 2
        pc = i % 2
        rds = ["Dg%d" % m, "hcp%d_%d" % (m, b)]
        rds.append("hcp%d_%d" % (m, b - 1) if b > 0 else "hcp_lo")
        rds.append("hcp%d_%d" % (m, b + 1) if b < 3 else "hcp_hi")

        def f_conv(e, m=m, b=b, pc=pc):
            for k in range(31):
                i_ = e.matmul(bank(pc), lhsT=Dg[:, m * 31 + k, :], rhs=hcp[:, m, b * 512 + k:b * 512 + k + 512], start=(k == 0), stop=(k == 30))
            return i_
        A("pe", f_conv, r=rds, w=[BK(pc)])
        A("act", lambda e, m=m, pb=pb, pc=pc: e.activation(out=yc[pb][:, m, :], in_=bank(pc), func=AF.Identity, bias=cb[:, m:m + 1], scale=1.0),
          r=[BK(pc), "cb"], w=["yc%d_%d" % (pb, m)])
        A("act", lambda e, m=m, pc=pc: e.activation(out=ysq[pc][:], in_=bank(pc), func=AF.Square, bias=cb[:, m:m + 1], scale=1.0),
          r=[BK(pc), "cb"], w=["ysq%d" % pc])

    def stat_step(i):
        b, m = seq[i]
        pb = b % 2
        pc = i % 2
        s1 = 4 + pb * 2
        s2 = 5 + pb * 2

        def f_st(e, m=m, pb=pb, pc=pc, s1=s1, s2=s2):
            e.matmul(bank(s1), lhsT=onesf[:], rhs=yc[pb][:, m, :], start=(m == 0), stop=(m == 3))
            return e.matmul(bank(s2), lhsT=onesf[:], rhs=ysq[pc][:], start=(m == 0), stop=(m == 3))
        A("pe", f_st, r=["onesf", "yc%d_%d" % (pb, m), "ysq%d" % pc], w=[BK(s1), BK(s2)])

    def norm_block(b):
        pb = b % 2
        s1 = 4 + pb * 2
        s2 = 5 + pb * 2
        A("dve", lambda e, pb=pb, s1=s1: e.tensor_scalar(out=cmean[pb][:], in0=bank(s1), scalar1=1.0 / 512.0, scalar2=None, op0=ALU.mult), r=[BK(s1)], w=["cmean%d" % pb])
        A("dve", lambda e, pb=pb: e.tensor_tensor(out=cm2[pb][:], in0=cmean[pb][:], in1=cmean[pb][:], op=ALU.mult), r=["cmean%d" % pb], w=["cm2%d" % pb])
        A("dve", lambda e, pb=pb, s2=s2: e.scalar_tensor_tensor(out=cm2[pb][:], in0=bank(s2), scalar=1.0 / 512.0, in1=cm2[pb][:], op0=ALU.mult, op1=ALU.subtract),
          r=[BK(s2), "cm2%d" % pb], w=["cm2%d" % pb])
        A("act", lambda e, pb=pb: e.activation(out=crstd[pb][:], in_=cm2[pb][:], func=AF.Ln, bias=epsln[:], scale=1.0), r=["cm2%d" % pb, "epsln"], w=["crstd%d" % pb])
        A("act", lambda e, pb=pb: e.activation(out=crstd[pb][:], in_=crstd[pb][:], func=AF.Exp, scale=-0.5), r=["crstd%d" % pb], w=["crstd%d" % pb])
        for m in range(4):
            pc = m % 2
            A("dve", lambda e, pb=pb, m=m, pc=pc: e.tensor_tensor(out=ctmp[pc][:], in0=yc[pb][:, m, :], in1=cmean[pb][:], op=ALU.subtract),
              r=["yc%d_%d" % (pb, m), "cmean%d" % pb], w=["ctmp%d" % pc])
            A("dve", lambda e, pb=pb, pc=pc: e.tensor_tensor(out=ctmp[pc][:], in0=ctmp[pc][:], in1=crstd[pb][:], op=ALU.mult),
              r=["ctmp%d" % pc, "crstd%d" % pb], w=["ctmp%d" % pc])
            A("act", lambda e, m=m, b=b, pc=pc: e.activation(out=convT[:, m, b * 512:(b + 1) * 512], in_=ctmp[pc][:], func=AF.Silu, bias=clb[:, m:m + 1], scale=clg[:, m:m + 1]),
              r=["ctmp%d" % pc, "clg", "clb"], w=["convT%d" % b])

    for i in range(17):
        if i < 16:
            conv_step(i)
        if i >= 1:
            stat_step(i - 1)
        if i >= 6 and (i - 6) % 4 == 0:
            norm_block((i - 6) // 4)
    norm_block(3)
    if debug and "convT" in debug:
        dump("convT", convT[:], [128, 4, S], BF16)
    sch.barrier()
    ar.release(wc, hcp, cw, cb, clg, clb, Dg, *sig, *yc, *ysq, *cmean, *cm2, *crstd, *ctmp)
    ar.flush()

    qT = ar.alloc("qT", [128, 8, S], BF16)
    kT = ar.alloc("kT", [128, 8, S], BF16)
    vaug = ar.alloc("vaug", [128, NT, 8, 65], BF16)
    wl = ar.alloc("wl", [128, 8, 704], BF16)
    DMA("pool", wl[:, :, 0:672], w_in_d.rearrange("(k p) f -> p k f", p=128)[:, :, 0:672], w=["wl"])
    A("dve", lambda e: e.tensor_scalar(out=wl[:, :, 672:688], in0=wl[:, :, 656:672], scalar1=-1.0, scalar2=None, op0=ALU.mult), r=["wl"], w=["wl"])
    A("dve", lambda e: e.tensor_copy(out=wl[:, :, 688:704], in_=wl[:, :, 640:656]), r=["wl"], w=["wl"])
    A("pool", lambda e: e.memset(vaug[:, :, :, 64:65], 1.0), w=["vaug1"])
    precast(16)
    cqb = ar.alloc("cqb", [128, 3, 512], BF16)
    ckvb = ar.alloc("ckvb", [128, 2, 512], BF16)
    cqn = [ar.alloc("cqn", [128, 3, 512], BF16) for _ in range(2)]
    ckvn = [ar.alloc("ckvn", [128, 2, 512], BF16) for _ in range(2)]
    sqq = [ar.alloc("sqq", [128, 512], F32) for _ in range(2)]
    rq = ar.alloc("rq", [128, 512], F32)
    rk = ar.alloc("rk", [128, 512], F32)
    t1 = ar.alloc("t1", [96, 512], F32)
    t2 = ar.alloc("t2", [96, 512], F32)
    sq_ctr = [0]

    def proj_chunk(b, col0, dst, dres, j, sbank, nj, jj):
        pj = sq_ctr[0] % 2
        sq_ctr[0] += 1

        def f_p(e, pj=pj, b=b, col0=col0):
            for k in range(8):
                i_ = e.matmul(bank(pj), lhsT=wl[:, k, col0:col0 + 128], rhs=hT[:, k, b * 512:(b + 1) * 512], start=(k == 0), stop=(k == 7))
            return i_
        A("pe", f_p, r=["wl", "hT%d" % b], w=[BK(pj)])
        A("act", lambda e, pj=pj, j=j: e.activation(out=dst[:, j, :], in_=bank(pj), func=AF.Copy), r=[BK(pj)], w=[dres + "%d" % j])
        A("act", lambda e, pj=pj: e.activation(out=sqq[pj][:], in_=bank(pj), func=AF.Square), r=[BK(pj)], w=["sqq%d" % pj])
        return lambda: A("pe", lambda e, pj=pj: e.matmul(bank(sbank), lhsT=onesf[:], rhs=sqq[pj][:], start=(jj == 0), stop=(jj == nj - 1)), r=["onesf", "sqq%d" % pj], w=[BK(sbank)])

    def rstd_ops(sbank, n, r_, rres):
        A("act", lambda e: e.activation(out=r_[:], in_=bank(sbank), func=AF.Ln, bias=epsrms[:], scale=1.0 / n), r=[BK(sbank), "epsrms"], w=[rres])
        A("act", lambda e: e.activation(out=r_[:], in_=r_[:], func=AF.Exp, scale=-0.5), r=[rres], w=[rres])

    def stage_P(b):
        pb = b % 2
        bs = slice(b * 512, (b + 1) * 512)
        pend = None
        for j in range(3):
            nxt = proj_chunk(b, j * 128, cqb, "cqb", j, 2, 3, j)
            if pend:
                pend()
            pend = nxt
        for j in range(2):
            nxt = proj_chunk(b, 384 + j * 128, ckvb, "ckvb", j, 3, 2, j)
            pend()
            pend = nxt

        def f_kr2(e, b=b):
            for k in range(8):
                e.matmul(bank(4)[64:96, :], lhsT=wl[:, k, 640:672], rhs=hT[:, k, b * 512:(b + 1) * 512], start=(k == 0), stop=(k == 7))
            for k in range(8):
                i_ = e.matmul(bank(5)[64:96, :], lhsT=wl[:, k, 672:704], rhs=hT[:, k, b * 512:(b + 1) * 512], start=(k == 0), stop=(k == 7))
            return i_
        A("pe", f_kr2, r=["wl", "hT%d" % b], w=[BK(4), BK(5)])
        pend()
        rstd_ops(2, 384.0, rq, "rq")
        rstd_ops(3, 256.0, rk, "rk")
        for j in range(3):
            A("dve", lambda e, j=j, pb=pb: e.tensor_tensor(out=cqn[pb][:, j, :], in0=cqb[:, j, :], in1=rq[:], op=ALU.mult), r=["cqb%d" % j, "rq"], w=["cqn%d_%d" % (pb, j)])
        for j in range(2):
            A("dve", lambda e, j=j, pb=pb: e.tensor_tensor(out=ckvn[pb][:, j, :], in0=ckvb[:, j, :], in1=rk[:], op=ALU.mult), r=["ckvb%d" % j, "rk"], w=["ckvn%d_%d" % (pb, j)])
        A("dve", lambda e, bs=bs: e.tensor_tensor(out=t1[64:96, :], in0=bank(4)[64:96, :], in1=cosT[64:96, bs], op=ALU.mult), r=[BK(4), "cosT"], w=["t1"])
        A("dve", lambda e, bs=bs: e.tensor_tensor(out=t2[64:96, :], in0=bank(5)[64:96, :], in1=sinT[64:96, bs], op=ALU.mult), r=[BK(5), "sinT"], w=["t2"])
        A("dve", lambda e, bs=bs: e.tensor_tensor(out=kT[64:96, 0, bs], in0=t1[64:96, :], in1=t2[64:96, :], op=ALU.add), r=["t1", "t2"], w=["kTr0"])
        for h in range(1, H):
            A("dve" if h % 2 else "act", (lambda e, bs=bs, h=h: e.tensor_copy(out=kT[64:96, h, bs], in_=kT[64:96, 0, bs])) if h % 2 else
              (lambda e, bs=bs, h=h: e.activation(out=kT[64:96, h, bs], in_=kT[64:96, 0, bs], func=AF.Copy)), r=["kTr0"], w=["kTr%d" % h])

    def stage_H(b):
        pb = b % 2
        bs = slice(b * 512, (b + 1) * 512)
        cq_r = ["cqn%d_%d" % (pb, j) for j in range(3)]
        ckv_r = ["ckvn%d_%d" % (pb, j) for j in range(2)]
        for h in range(H):
            pq = 6 + (h % 2)
            pr = 4 + (h % 2)

            def f_q(e, h=h, pq=pq, pr=pr, pb=pb):
                for k in range(3):
                    e.matmul(bank(pq)[0:96, :], lhsT=Wq2[:, k, h, 0:96], rhs=cqn[pb][:, k, :], start=(k == 0), stop=(k == 2))
                for k in range(3):
                    i_ = e.matmul(bank(pr)[64:96, :], lhsT=Wq2[:, k, h, 96:128], rhs=cqn[pb][:, k, :], start=(k == 0), stop=(k == 2))
                return i_
            A("pe", f_q, r=["Wq2"] + cq_r, w=[BK(pq), BK(pr)])
            A("act", lambda e, h=h, pq=pq, bs=bs: e.activation(out=qT[0:64, h, bs], in_=bank(pq)[0:64, :], func=AF.Copy), r=[BK(pq)], w=["qT%d_%d" % (h, b)])
            A("dve", lambda e, pq=pq, bs=bs: e.tensor_tensor(out=t1[64:96, :], in0=bank(pq)[64:96, :], in1=cosT[64:96, bs], op=ALU.mult), r=[BK(pq), "cosT"], w=["t1"])
            A("dve", lambda e, pr=pr, bs=bs: e.tensor_tensor(out=t2[64:96, :], in0=bank(pr)[64:96, :], in1=sinT[64:96, bs], op=ALU.mult), r=[BK(pr), "sinT"], w=["t2"])
            A("dve", lambda e, h=h, bs=bs: e.tensor_tensor(out=qT[64:96, h, bs], in0=t1[64:96, :], in1=t2[64:96, :], op=ALU.add), r=["t1", "t2"], w=["qTr%d_%d" % (h, b)])
        for h in range(H):
            pk = 6 + (h % 2)

            def f_k(e, h=h, pk=pk, pb=pb):
                for k in range(2):
                    i_ = e.matmul(bank(pk)[0:64, :], lhsT=Wk2[:, k, h, :], rhs=ckvn[pb][:, k, :], start=(k == 0), stop=(k == 1))
                return i_
            A("pe", f_k, r=["Wk2"] + ckv_r, w=[BK(pk)])
            A("act", lambda e, h=h, pk=pk, bs=bs: e.activation(out=kT[0:64, h, bs], in_=bank(pk)[0:64, :], func=AF.Copy), r=[BK(pk)], w=["kT%d_%d" % (h, b)])
        for tt in range(4):
            pv = 4 + (tt % 2)
            t = b * 4 + tt

            def f_v(e, tt=tt, pv=pv, pb=pb):
                for k in range(2):
                    i_ = e.matmul(bank(pv), lhsT=ckvn[pb][:, k, tt * 128:(tt + 1) * 128], rhs=Wv[:, k, :, :].rearrange("p h c -> p (h c)"), start=(k == 0), stop=(k == 1))
                return i_
            A("pe", f_v, r=["Wv"] + ckv_r, w=[BK(pv)])
            A("act", lambda e, pv=pv, t=t: e.activation(out=vaug[:, t, :, 0:64], in_=bank(pv).rearrange("p (h c) -> p h c", c=64), func=AF.Copy),
              r=[BK(pv)], w=["vaug%d" % t])

    stage_P(0)
    for b in range(4):
        if b + 1 < 4:
            stage_P(b + 1)
        stage_H(b)
    if debug and "qT" in debug:
        dump("qT", qT[:], [128, 8, S], BF16)
        dump("kT", kT[:], [128, 8, S], BF16)
        dump("vaug", vaug[:], [128, NT, 8, 65], BF16)
    sch.barrier()
    ar.release(hT, wl, cqb, ckvb, *cqn, *ckvn, *sqq, rq, rk, t1, t2, cosT, sinT, Wq2, Wk2, Wv)
    ar.flush()

    precast(28)
    attn_tok = ar.alloc("attn_tok", [128, NT, 512], BF16)
    ptb = [ar.alloc("ptb", [128, 1024], BF16) for _ in range(3)]
    rec = [ar.alloc("rec", [128, 4], F32) for _ in range(2)]
    steps = []
    for h in range(H):
        for qb in range(4):
            for kp in range(8):
                steps.append((h, qb, kp))

    def emit_scores(i):
        h, qb, kp = steps[i]
        sp_ = i % 2

        def f_s(e, h=h, qb=qb, kp=kp, sp_=sp_):
            for half in range(2):
                kt = kp * 2 + half
                i_ = e.matmul(pst[sp_][:, half * 512:(half + 1) * 512], lhsT=kT[0:96, h, kt * 128:(kt + 1) * 128], rhs=qT[0:96, h, qb * 512:(qb + 1) * 512], start=True, stop=True)
            return i_
        rds = ["kT%d_%d" % (h, (kp * 2) // 4), "kTr%d" % h, "qT%d_%d" % (h, qb), "qTr%d_%d" % (h, qb)]
        A("pe", f_s, r=rds, w=[BK(2 * sp_), BK(2 * sp_ + 1)])
        pp = i % 3
        A("act", lambda e, sp_=sp_, pp=pp: e.activation(out=ptb[pp][:], in_=pst[sp_][:], func=AF.Exp, scale=SCALE), r=[BK(2 * sp_), BK(2 * sp_ + 1)], w=["ptb%d" % pp])

    def emit_pv(i):
        h, qb, kp = steps[i]
        pp = i % 3
        g = i // 8
        ob = 4 + (g % 2)
        O = bank(ob).rearrange("p (q c) -> p q c", c=128)

        def f_pv(e, h=h, kp=kp, pp=pp, O=O):
            for half in range(2):
                kt = kp * 2 + half
                for qt in range(4):
                    i_ = e.matmul(O[:, qt, 0:65], lhsT=ptb[pp][:, half * 512 + qt * 128:half * 512 + (qt + 1) * 128], rhs=vaug[:, kt, h, :],
                                  start=(kt == 0 and qt == 0), stop=(kt == 15), skip_group_check=True)
            return i_
        rds = ["ptb%d" % pp, "vaug1"] + ["vaug%d" % (kp * 2), "vaug%d" % (kp * 2 + 1)]
        A("pe", f_pv, r=rds, w=[BK(ob)])
        if kp == 7:
            pr_ = g % 2
            A("dve", lambda e, O=O, pr_=pr_: e.reciprocal(out=rec[pr_][:], in_=O[:, :, 64]), r=[BK(ob)], w=["rec%d" % pr_])
            for qt in range(4):
                t = qb * 4 + qt
                A("dve", lambda e, O=O, pr_=pr_, qt=qt, t=t, h=h: e.tensor_scalar(out=attn_tok[:, t, h * 64:(h + 1) * 64], in0=O[:, qt, 0:64], scalar1=rec[pr_][:, qt:qt + 1], scalar2=None, op0=ALU.mult),
                  r=[BK(ob), "rec%d" % pr_], w=["attn_tok%d" % t])

    nst = len(steps)
    emit_scores(0)
    for i in range(nst):
        if i + 1 < nst:
            emit_scores(i + 1)
        emit_pv(i)
    if debug and "attn_tok" in debug:
        dump("attn_tok", attn_tok[:], [128, NT, 512], BF16)
    sch.barrier()
    ar.release(qT, kT, vaug, *ptb, *rec)
    ar.flush()

    attnT = ar.alloc("attnT", [128, 4, S], BF16)
    for t in range(NT):
        p = t % 2

        def f_tr2(e, t=t, p=p):
            for c in range(4):
                i_ = e.transpose(out=bankb(p)[:, c * 128:(c + 1) * 128], in_=attn_tok[:, t, c * 128:(c + 1) * 128], identity=identb[:])
            return i_
        A("pe", f_tr2, r=["attn_tok%d" % t, "identb"], w=[BK(p)])
        A("act", lambda e, t=t, p=p: e.activation(out=attnT[:, :, t * 128:(t + 1) * 128], in_=bankb(p)[:, 0:512].rearrange("p (k c) -> p k c", c=128), func=AF.Copy),
          r=[BK(p)], w=["attnT%d" % t])
    sch.barrier()
    ar.release(attn_tok)
    ar.flush()

    macc = nc.dram_tensor("moe_acc", [S, D], F32).ap()
    h1b = ar.alloc("h1b", [128, NT, D], BF16)
    wo = ar.alloc("wo", [128, 8, D], BF16)
    wr = ar.alloc("wr", [128, 8, E], F32)
    wrg = ar.alloc("wrg", [128, 8, E], F32)
    g0 = ar.alloc("g0", [128, D], F32)
    b0 = ar.alloc("b0", [128, D], F32)
    g1 = ar.alloc("g1", [128, D], F32)
    b1 = ar.alloc("b1", [128, D], F32)
    g1pk = ar.alloc("g1pk", [128, 8], F32)
    b1pk = ar.alloc("b1pk", [128, 8], F32)
    brow = ar.alloc("brow", [1, E], F32)
    aff = ar.alloc("aff", [128, NT, E], F32)
    DMA("pool", wo[:], wo_d.rearrange("(k p) f -> p k f", p=128), w=["wo"])
    DMA("sp", wr[:], wr_d.rearrange("(k p) f -> p k f", p=128), w=["wr"])
    DMA("sp", g0[:], g0_d, w=["g0"])
    DMA("sp", b0[:], b0_d, w=["b0"])
    DMA("sp", g1[:], g1_d, w=["g1"])
    DMA("sp", b1[:], b1_d, w=["b1"])
    DMA("sp", g1pk[:], g1pk_d, w=["g1pk"])
    DMA("sp", b1pk[:], b1pk_d, w=["b1pk"])
    A("dve", lambda e: e.tensor_scalar(out=g0[:], in0=g0[:], scalar1=ALPHA, scalar2=None, op0=ALU.mult), r=["g0"], w=["g0"])
    A("dve", lambda e: e.tensor_scalar(out=b0[:], in0=b0[:], scalar1=ALPHA, scalar2=None, op0=ALU.mult), r=["b0"], w=["b0"])
    bh = ar.alloc("bh", [1, D], BF16)
    bl = ar.alloc("bl", [1, D], BF16)
    blf = ar.alloc("blf", [1, D], F32)
    A("dve", lambda e: e.tensor_copy(out=bh[:], in_=b0[0:1, :]), r=["b0"], w=["bh"])
    A("dve", lambda e: e.tensor_tensor(out=blf[:], in0=b0[0:1, :], in1=bh[:], op=ALU.subtract), r=["b0", "bh"], w=["blf"])
    A("dve", lambda e: e.tensor_copy(out=bl[:], in_=blf[:]), r=["blf"], w=["bl"])
    A("dve", lambda e: e.tensor_scalar(out=g1[:], in0=g1[:], scalar1=ALPHA, scalar2=None, op0=ALU.mult), r=["g1"], w=["g1"])
    A("dve", lambda e: e.tensor_scalar(out=b1[:], in0=b1[:], scalar1=ALPHA, scalar2=None, op0=ALU.mult), r=["b1"], w=["b1"])
    for k in range(8):
        A("dve", lambda e, k=k: e.tensor_scalar(out=wrg[:, k, :], in0=wr[:, k, :], scalar1=g1pk[:, k:k + 1], scalar2=None, op0=ALU.mult), r=["wr", "g1pk"], w=["wrg"])

    def f_brow(e):
        for k in range(8):
            i_ = e.matmul(bank(7)[0:1, 0:E], lhsT=b1pk[:, k:k + 1], rhs=wr[:, k, :], start=(k == 0), stop=(k == 7))
        return i_
    A("pe", f_brow, r=["b1pk", "wr"], w=[BK(7)])
    A("dve", lambda e: e.tensor_copy(out=brow[:], in_=bank(7)[0:1, 0:E]), r=[BK(7)], w=["brow"])
    NR4 = 6
    NRX = 7
    NRZ = 5
    lgb = [bank(6)[:, 0:E], bank(7)[:, 0:E]]
    xt = [ar.alloc("xt", [128, D], F32) for _ in range(NRX)]
    zt = [ar.alloc("zt", [128, D], F32) for _ in range(NRZ)]
    r1 = [ar.alloc("r1", [128, D], F32) for _ in range(2)]
    h1T = [ar.alloc("h1T", [128, D], F32) for _ in range(2)]
    smx = [ar.alloc("smx", [128, 1], F32) for _ in range(2)]
    ssum = [ar.alloc("ssum", [128, 1], F32) for _ in range(2)]
    sex = [ar.alloc("sex", [128, E], F32) for _ in range(2)]
    lnB = ln_small("lnB", NR4)
    lnC = ln_small("lnC", NR4)

    def p4_s0(t):
        p = t % NRX
        DMA("sp", xt[p][:], x_d[t * 128:(t + 1) * 128, :], w=["xt%d" % p])

    def p4_s0b(t):
        pass

    def p4_s1(t):
        p = t % NRX
        ln_part1a(lnB[t % NR4], xt[p][:], ["xt%d" % p])

    def p4_s1b(t):
        p = t % NRX
        ln_part1b(lnB[t % NR4], xt[p][:], ["xt%d" % p], xt[p][:], ["xt%d" % p])

    def p4_s2a(t):
        p = t % NRX
        A("pool", lambda e, p=p: e.tensor_tensor(out=xt[p][:], in0=xt[p][:], in1=g0[:], op=ALU.mult), r=["xt%d" % p, "g0"], w=["xt%d" % p])
        for half in range(2):
            pw = 2 * (t % 2) + half

            def f_wo(e, t=t, half=half, pw=pw):
                for k in range(8):
                    src = attnT[:, k, t * 128:(t + 1) * 128] if k < 4 else convT[:, k - 4, t * 128:(t + 1) * 128]
                    e.matmul(bank(pw), lhsT=src, rhs=wo[:, k, half * 512:(half + 1) * 512], start=(k == 0), stop=False)
                e.matmul(bank(pw), lhsT=onesb[0:1, :], rhs=bh[0:1, half * 512:(half + 1) * 512], start=False, stop=False)
                return e.matmul(bank(pw), lhsT=onesb[0:1, :], rhs=bl[0:1, half * 512:(half + 1) * 512], start=False, stop=True)
            A("pe", f_wo, r=["attnT%d" % t, "convT%d" % (t // 4), "wo", "onesb", "bh", "bl"], w=[BK(pw)])

    def p4_s2b(t):
        p = t % NRX
        pz = t % NRZ
        for half in range(2):
            pw = 2 * (t % 2) + half
            A("dve", lambda e, p=p, pz=pz, half=half, pw=pw: e.tensor_tensor(out=zt[pz][:, half * 512:(half + 1) * 512], in0=xt[p][:, half * 512:(half + 1) * 512], in1=bank(pw), op=ALU.add),
              r=["xt%d" % p, BK(pw)], w=["zt%d_%d" % (pz, half)])

    def p4_s3(t):
        p = t % NRZ
        ln_part1a(lnC[t % NR4], zt[p][:], ["zt%d_0" % p, "zt%d_1" % p])

    def p4_s3b(t):
        p = t % NRZ
        ln_part1b(lnC[t % NR4], zt[p][:], ["zt%d_0" % p, "zt%d_1" % p], zt[p][:], ["zt%d_0" % p, "zt%d_1" % p])

    def p4_s4(t):
        p = t % NRZ
        zr = ["zt%d_0" % p, "zt%d_1" % p]
        A("act", lambda e, t=t, p=p: e.activation(out=h1b[:, t, :], in_=zt[p][:], func=AF.Copy), r=zr, w=["h1b%d" % t])
        pt_ = 2

        def f_trf(e, p=p, pt_=pt_):
            for k in range(8):
                i_ = e.transpose(out=pst[pt_][:, k * 128:(k + 1) * 128], in_=zt[p][:, k * 128:(k + 1) * 128], identity=identf[:])
            return i_
        A("pe", f_trf, r=zr + ["identf"], w=[BK(2 * pt_), BK(2 * pt_ + 1)])
        pr_ = t % 2
        A("pool", lambda e, p=p, pr_=pr_: e.tensor_tensor(out=r1[pr_][:], in0=zt[p][:], in1=g1[:], op=ALU.mult), r=zr + ["g1"], w=["r1%d" % pr_])
        A("pool", lambda e, pr_=pr_: e.tensor_tensor(out=r1[pr_][:], in0=r1[pr_][:], in1=b1[:], op=ALU.add), r=["r1%d" % pr_, "b1"], w=["r1%d" % pr_])
        DMA("sp", macc[t * 128:(t + 1) * 128, :], r1[pr_][:], r=["r1%d" % pr_], w=["macc"])
        ph_ = t % 2
        A("act", lambda e, ph_=ph_, pt_=pt_: e.activation(out=h1T[ph_][:], in_=pst[pt_][:], func=AF.Copy), r=[BK(2 * pt_), BK(2 * pt_ + 1)], w=["h1T%d" % ph_])

    def p4_s5b(t):
        p = t % 2
        lg = lgb[p]

        def f_lg(e, p=p, lg=lg):
            for k in range(8):
                e.matmul(lg, lhsT=h1T[p][:, k * 128:(k + 1) * 128], rhs=wrg[:, k, :], start=(k == 0), stop=False)
            return e.matmul(lg, lhsT=onesf[0:1, :], rhs=brow[:], start=False, stop=True)
        A("pe", f_lg, r=["h1T%d" % p, "wrg", "brow", "onesf"], w=[BK(6 + p)])
        A("dve", lambda e, p=p, lg=lg: e.tensor_reduce(out=smx[p][:], in_=lg, axis=AX.X, op=ALU.max), r=[BK(6 + p)], w=["smx%d" % p])
        A("dve", lambda e, p=p: e.tensor_scalar(out=smx[p][:], in0=smx[p][:], scalar1=-1.0, scalar2=None, op0=ALU.mult), r=["smx%d" % p], w=["smx%d" % p])

    def p4_s5c(t):
        p = t % 2
        lg = lgb[p]
        A("act", lambda e, p=p, lg=lg: e.activation(out=sex[p][:], in_=lg, func=AF.Exp, bias=smx[p][:], scale=1.0),
          r=[BK(6 + p), "smx%d" % p], w=["sex%d" % p])
        A("dve", lambda e, p=p: e.tensor_reduce(out=ssum[p][:], in_=sex[p][:], axis=AX.X, op=ALU.add), r=["sex%d" % p], w=["ssum%d" % p])
        A("dve", lambda e, p=p: e.reciprocal(out=ssum[p][:], in_=ssum[p][:]), r=["ssum%d" % p], w=["ssum%d" % p])
        A("dve", lambda e, p=p, t=t: e.tensor_scalar(out=aff[:, t, :], in0=sex[p][:], scalar1=ssum[p][:], scalar2=None, op0=ALU.mult), r=["sex%d" % p, "ssum%d" % p], w=["aff%d" % t])
    swpipe(NT, [p4_s0, p4_s0b, p4_s1, p4_s1b, p4_s2a, p4_s2b, p4_s3, p4_s3b, p4_s4, p4_s5b, p4_s5c])
    if debug and "h1" in debug:
        dump("h1n", h1b[:], [128, NT, D], BF16)
        dump("aff", aff[:], [128, NT, E], F32)
    sch.barrier()
    ar.release(attnT, convT, wo, wr, wrg, g0, b0, g1, b1, brow, bh, bl, blf, *xt, *zt, *r1, *h1T, *smx, *ssum, *sex)
    ln_small_free(lnB)
    ln_small_free(lnC)
    ar.flush()

    wgb = [ar.alloc("wgb", [128, 8, FG], BF16) for _ in range(NWB)]
    wub = [ar.alloc("wub", [128, 8, FG], BF16) for _ in range(NWB)]
    wdb = [ar.alloc("wdb", [128, FG // 128, D], BF16) for _ in range(NWB)]
    NCH = FF // FG
    FPC = FG // 128

    def load_chunk(c):
        pos_e, fg = divmod(c, NCH)
        eid = ORDER[pos_e]
        s = c % NWB
        if eid < NPC:
            f0, f1 = fg * FG, (fg + 1) * FG
            rg = ["wpc_g%d_%d" % (eid, 0), "wpc_g%d_%d" % (eid, D // 2)]
            ru = ["wpc_u%d_%d" % (eid, 0), "wpc_u%d_%d" % (eid, D // 2)]
            rd = ["wpc_d%d_%d" % (eid, 0), "wpc_d%d_%d" % (eid, FF // 2)]
            DMA("sp", wgb[s][:], wpc[("g", eid)].rearrange("(k p) f -> p k f", p=128)[:, :, f0:f1], r=rg, w=["wg%d" % s])
            DMA("sp", wub[s][:], wpc[("u", eid)].rearrange("(k p) f -> p k f", p=128)[:, :, f0:f1], r=ru, w=["wu%d" % s])
            DMA("sp", wdb[s][:], wpc[("d", eid)][f0:f1, :].rearrange("(j p) d -> p j d", p=128), r=rd, w=["wd%d" % s])
        else:
            DMA("pool", wgb[s][:], wg_d[eid].rearrange("(k p) f -> p k f", p=128)[:, :, fg * FG:(fg + 1) * FG], w=["wg%d" % s])
            DMA("pool", wub[s][:], wu_d[eid].rearrange("(k p) f -> p k f", p=128)[:, :, fg * FG:(fg + 1) * FG], w=["wu%d" % s])
            DMA("pool", wdb[s][:], wd_d[eid][fg * FG:(fg + 1) * FG, :].rearrange("(j p) d -> p j d", p=128), w=["wd%d" % s])

    for c in range(NWB):
        load_chunk(c)

    tp = ar.alloc("tp", [128, NT, 2], BF16)
    DMA("sp", tp[:].rearrange("p t c -> p (t c)"), c_tp_d, w=["tp"])
    A8 = ar.alloc("A8", [128, 256], F32)
    junk = ar.alloc("junk", [128, 256], F32)
    gmat = ar.alloc("gmat", [128, 128], F32)
    cand = ar.alloc("cand", [128, 1], F32)
    cnt = ar.alloc("cnt", [128, 1], F32)
    dlt = ar.alloc("dlt", [128, 1], F32)
    cnt2 = ar.alloc("cnt2", [128, 1], F32)
    thr = ar.alloc("thr", [128, 1], F32)
    mask8 = ar.alloc("mask8", [128, 256], BF16)
    mask_tok = ar.alloc("mask_tok", [128, NT * E], BF16)
    posm = ar.alloc("posm", [128, NT * E], F32)
    ahl = ar.alloc("ahl", [128, NT * E, 4], BF16)
    DMA("sp", gmat[:], c_gmat_d, w=["gmat"])

    affp = ar.alloc("affp", [128, 2, 128], F32)
    for h in range(2):
        A("dve", lambda e, h=h: e.tensor_copy(out=affp[:, h, :].rearrange("p (e g) -> p e g", g=8), in_=aff[:, h::2, :].rearrange("p g e -> p e g")),
          r=["aff%d" % t for t in range(NT)], w=["affp"])

    def f_a8(e):
        for h in range(2):
            i_ = e.transpose(out=bank(0)[:, h * 128:(h + 1) * 128], in_=affp[:, h, :], identity=identf[:])
        return i_
    A("pe", f_a8, r=["affp", "identf"], w=[BK(0)])
    A("dve", lambda e: e.tensor_copy(out=A8[:], in_=bank(0)[:, 0:256]), r=[BK(0)], w=["A8"])
    A("dve", lambda e: e.memset(thr[:], 0.0), w=["thr"])
    A("dve", lambda e: e.memset(cand[:], 0.5), w=["cand"])
    for i in range(1, NBIS + 1):
        step = 2.0 ** (-i)
        pb_ = 1 + (i % 2)
        A("dve", lambda e: e.tensor_scalar(out=junk[:], in0=A8[:], scalar1=cand[:], scalar2=None, op0=ALU.is_ge, op1=ALU.add, accum_out=cnt[:]),
          r=["A8", "cand"], w=["junk", "cnt"])
        A("dve", lambda e: e.tensor_copy(out=cnt2[:], in_=cnt[:]), r=["cnt"], w=["cnt2"])
        A("pe", lambda e, pb_=pb_: e.matmul(bank(pb_)[:, 0:1], lhsT=gmat[:], rhs=cnt2[:], start=True, stop=True), r=["gmat", "cnt2"], w=[BK(pb_)])
        A("dve", lambda e, pb_=pb_, step=step: e.tensor_scalar(out=dlt[:], in0=bank(pb_)[:, 0:1], scalar1=float(CAP) - 0.5, scalar2=step, op0=ALU.is_ge, op1=ALU.mult),
          r=[BK(pb_)], w=["dlt"])
        A("dve", lambda e: e.tensor_tensor(out=thr[:], in0=thr[:], in1=dlt[:], op=ALU.add), r=["thr", "dlt"], w=["thr"])
        A("dve", lambda e, step=step: e.tensor_scalar(out=cand[:], in0=thr[:], scalar1=step * 0.5, scalar2=None, op0=ALU.add), r=["thr"], w=["cand"])
    A("dve", lambda e: e.tensor_scalar(out=mask8[:], in0=A8[:], scalar1=thr[:], scalar2=None, op0=ALU.is_ge), r=["A8", "thr"], w=["mask8"])

    def f_mtok(e):
        for h in range(2):
            i_ = e.transpose(out=bankb(4)[:, h * 128:(h + 1) * 128], in_=mask8[:, h * 128:(h + 1) * 128], identity=identb[:])
        return i_
    A("pe", f_mtok, r=["mask8", "identb"], w=[BK(4)])
    mtv = mask_tok[:].rearrange("p (t e) -> p t e", e=E)
    for h in range(2):
        A("dve", lambda e, h=h: e.tensor_copy(out=mtv[:, h::2, :], in_=bankb(4)[:, h * 128:(h + 1) * 128].rearrange("p (e g) -> p g e", g=8)), r=[BK(4)], w=["mask_tok"])

    def f_pos(e):
        for t in range(NT):
            for t2_ in range(t + 1):
                lhs = ustr[:] if t2_ == t else onesb[:]
                i_ = e.matmul(bank(5)[:, t * 16:(t + 1) * 16], lhsT=lhs, rhs=mask_tok[:, t2_ * 16:(t2_ + 1) * 16], start=(t2_ == 0), stop=(t2_ == t))
        return i_
    A("pe", f_pos, r=["mask_tok", "ustr", "onesb"], w=[BK(5)])
    A("dve", lambda e: e.scalar_tensor_tensor(out=posm[:], in0=bank(5)[:, 0:NT * E], scalar=1.0, in1=mask_tok[:], op0=ALU.add, op1=ALU.mult), r=[BK(5), "mask_tok"], w=["posm"])
    A("dve", lambda e: e.tensor_scalar(out=posm[:], in0=posm[:], scalar1=-1.0, scalar2=None, op0=ALU.add), r=["posm"], w=["posm"])
    affv = aff[:].rearrange("p t e -> p (t e)")
    A("dve", lambda e: e.tensor_copy(out=ahl[:, :, 0], in_=affv), r=["aff%d" % t for t in range(NT)], w=["ahl"])
    A("dve", lambda e: e.tensor_tensor(out=ahl[:, :, 1], in0=affv, in1=ahl[:, :, 0], op=ALU.subtract), r=["ahl"] + ["aff%d" % t for t in range(NT)], w=["ahl"])
    A("dve", lambda e: e.tensor_copy(out=ahl[:].rearrange("p (t e) c -> p t e c", e=E)[:, :, :, 2:4], in_=tp[:].unsqueeze(2).to_broadcast([128, NT, E, 2])), r=["tp", "ahl"], w=["ahl"])
    if debug and "posm" in debug:
        dump("posm", posm[:], [128, NT * E], F32)
    sch.barrier()
    ar.release(A8, junk, gmat, cand, thr, cnt, cnt2, dlt, mask8, mask_tok, tp, affp)
    ar.flush()

    Pm = [ar.alloc("Pm", [128, NT, CAP], BF16) for _ in range(2)]
    gate = [ar.alloc("gate", [128, 2], F32) for _ in range(2)]
    gi = [ar.alloc("gi", [128, 8], F32) for _ in range(2)]
    idxf = [ar.alloc("idxf", [128, 2], F32) for _ in range(2)]
    idxi = [ar.alloc("idxi", [128, 2], I32) for _ in range(2)]
    xgT = ar.alloc("xgT", [128, 8, CAP], BF16)
    yg = [ar.alloc("yg", [128, 2, D], F32) for _ in range(2)]
    sa = [ar.alloc("sa", [128, CAP], F32) for _ in range(2)]
    actT = [ar.alloc("actT", [128, CAP], BF16) for _ in range(4)]
    misc_ctr = [0]

    def misc_bank():
        b_ = 6 + (misc_ctr[0] % 2)
        misc_ctr[0] += 1
        return b_

    def prep_expert(e_):
        pe_ = e_ % 2
        eid = ORDER[e_]
        for t in range(NT):
            A("dve", lambda e, t=t, pe_=pe_, eid=eid: e.tensor_scalar(out=Pm[pe_][:, t, :], in0=iota_c[:], scalar1=posm[:, t * E + eid:t * E + eid + 1], scalar2=None, op0=ALU.is_equal),
              r=["iota_c", "posm"], w=["Pm%d_%d" % (pe_, t)])
        mb = misc_bank()

        def f_gate(e, mb=mb, pe_=pe_, eid=eid):
            for ct in range(2):
                for t in range(NT):
                    i_ = e.matmul(bank(mb)[:, ct * 4:ct * 4 + 4], lhsT=Pm[pe_][:, t, ct * 128:(ct + 1) * 128], rhs=ahl[:, t * E + eid, :], start=(t == 0), stop=(t == NT - 1))
            return i_
        A("pe", f_gate, r=["ahl"] + ["Pm%d_%d" % (pe_, t) for t in range(NT)], w=[BK(mb)])
        A("dve", lambda e, mb=mb, pe_=pe_: e.tensor_copy(out=gi[pe_][:], in_=bank(mb)[:, 0:8]), r=[BK(mb)], w=["gi%d" % pe_])
        giv = gi[pe_][:].rearrange("p (c f) -> p c f", f=4)
        A("dve", lambda e, pe_=pe_, giv=giv: e.tensor_tensor(out=gate[pe_][:], in0=giv[:, :, 0], in1=giv[:, :, 1], op=ALU.add), r=["gi%d" % pe_], w=["gate%d" % pe_])
        A("dve", lambda e, pe_=pe_, giv=giv: e.scalar_tensor_tensor(out=idxf[pe_][:], in0=giv[:, :, 2], scalar=128.0, in1=giv[:, :, 3], op0=ALU.mult, op1=ALU.add),
          r=["gi%d" % pe_], w=["idxf%d" % pe_])
        A("dve", lambda e, pe_=pe_: e.tensor_copy(out=idxi[pe_][:], in_=idxf[pe_][:]), r=["idxf%d" % pe_], w=["idxi%d" % pe_])

    def gather_expert(e_):
        pe_ = e_ % 2
        for dk in range(8):
            mb = misc_bank()

            def f_g(e, mb=mb, dk=dk, pe_=pe_):
                for t in range(NT):
                    i_ = e.matmul(bank(mb)[:, 0:CAP], lhsT=h1b[:, t, dk * 128:(dk + 1) * 128], rhs=Pm[pe_][:, t, :], start=(t == 0), stop=(t == NT - 1))
                return i_
            A("pe", f_g, r=["h1b%d" % t for t in range(NT)] + ["Pm%d_%d" % (pe_, t) for t in range(NT)], w=[BK(mb)])
            A("act", lambda e, mb=mb, dk=dk: e.activation(out=xgT[:, dk, :], in_=bank(mb)[:, 0:CAP], func=AF.Identity, bias=b1pk[:, dk:dk + 1], scale=g1pk[:, dk:dk + 1]),
              r=[BK(mb), "g1pk", "b1pk"], w=["xgT%d" % dk])

    def gu(e_, ft):
        c = e_ * NCH + ft // FPC
        s = c % NWB
        j = ft % FPC
        gb = 4 + (ft % 2)

        def f_gu(e, s=s, j=j, gb=gb):
            for k in range(8):
                e.matmul(bank(gb)[:, 0:CAP], lhsT=wgb[s][:, k, j * 128:(j + 1) * 128], rhs=xgT[:, k, :], start=(k == 0), stop=(k == 7))
            for k in range(8):
                i_ = e.matmul(bank(gb)[:, CAP:2 * CAP], lhsT=wub[s][:, k, j * 128:(j + 1) * 128], rhs=xgT[:, k, :], start=(k == 0), stop=(k == 7))
            return i_
        A("pe", f_gu, r=["wg%d" % s, "wu%d" % s] + ["xgT%d" % dk for dk in range(8)], w=[BK(gb)])
        ps_ = ft % 2
        pa_ = ft % 4
        A("act", lambda e, gb=gb, ps_=ps_: e.activation(out=sa[ps_][:], in_=bank(gb)[:, 0:CAP], func=AF.Silu), r=[BK(gb)], w=["sa%d" % ps_])
        A("dve", lambda e, gb=gb, ps_=ps_, pa_=pa_: e.tensor_tensor(out=actT[pa_][:], in0=bank(gb)[:, CAP:2 * CAP], in1=sa[ps_][:], op=ALU.mult), r=[BK(gb), "sa%d" % ps_], w=["actT%d" % pa_])

    def down(e_, ft):
        c = e_ * NCH + ft // FPC
        s = c % NWB
        j = ft % FPC
        pa_ = ft % 4

        def f_d(e, s=s, j=j, pa_=pa_, ft=ft):
            for ct in range(2):
                for dh in range(2):
                    i_ = e.matmul(pst[ct][:, dh * 512:(dh + 1) * 512], lhsT=actT[pa_][:, ct * 128:(ct + 1) * 128], rhs=wdb[s][:, j, dh * 512:(dh + 1) * 512], start=(ft == 0), stop=(ft == NT - 1))
            return i_
        A("pe", f_d, r=["actT%d" % pa_, "wd%d" % s], w=[BK(0), BK(1), BK(2), BK(3)])
        if j == FPC - 1 and c + NWB < E * NCH:
            load_chunk(c + NWB)

    def yevac_scatter(e_):
        pe_ = e_ % 2
        for ct in range(2):
            A("act", lambda e, ct=ct, pe_=pe_: e.activation(out=yg[pe_][:, ct, :], in_=pst[ct][:], func=AF.Copy, scale=gate[pe_][:, ct:ct + 1]),
              r=[BK(2 * ct), BK(2 * ct + 1), "gate%d" % pe_], w=["yg%d_%d" % (pe_, ct)])
        for ct in range(2):
            A("pool", lambda e, ct=ct, pe_=pe_: e.indirect_dma_start(out=macc, out_offset=bass.IndirectOffsetOnAxis(ap=idxi[pe_][:, ct:ct + 1], axis=0),
                                                                     in_=yg[pe_][:, ct, :], in_offset=None, bounds_check=S - 1, oob_is_err=True, compute_op=ALU.add),
              r=["yg%d_%d" % (pe_, ct), "idxi%d" % pe_, "macc"], w=["macc"], dma=True)

    SKEW = 2
    prep_expert(0)
    gather_expert(0)
    for e_ in range(E):
        for ft in range(NT):
            gu(e_, ft)
            if ft >= SKEW:
                down(e_, ft - SKEW)
        if e_ + 1 < E:
            prep_expert(e_ + 1)
        for ft in range(NT - SKEW, NT):
            down(e_, ft)
        yevac_scatter(e_)
        if e_ + 1 < E:
            gather_expert(e_ + 1)
    sch.barrier()
    ar.release(h1b, *Pm, *gate, *gi, *idxf, *idxi, xgT, *yg, *sa, *actT, *wgb, *wub, *wdb, posm, ahl, aff, g1pk, b1pk)
    ar.flush()

    g2 = ar.alloc("g2", [128, D], F32)
    b2 = ar.alloc("b2", [128, D], F32)
    DMA("sp", g2[:], g2_d, w=["g2"])
    DMA("sp", b2[:], b2_d, w=["b2"])
    NR7 = 6
    rt = [ar.alloc("rt", [128, D], F32) for _ in range(NR7)]
    ot = [ar.alloc("ot", [128, D], F32) for _ in range(NR7)]
    lnD = ln_small("lnD", NR7)

    def p7_s0(t):
        p = t % NR7
        DMA("sp", rt[p][:], macc[t * 128:(t + 1) * 128, :], r=["macc"], w=["rt%d" % p])

    def p7_s0b(t):
        pass

    def p7_s1(t):
        p = t % NR7
        ln_part1a(lnD[p], rt[p][:], ["rt%d" % p])

    def p7_s1b(t):
        p = t % NR7
        ln_part1b(lnD[p], rt[p][:], ["rt%d" % p], rt[p][:], ["rt%d" % p])

    def p7_s2(t):
        p = t % NR7
        ln_part2(rt[p][:], ["rt%d" % p], ot[p][:], ["ot%d" % p], g2, b2, "g2", "b2")
        DMA("sp", out_d[t * 128:(t + 1) * 128, :], ot[p][:], r=["ot%d" % p])
    swpipe(NT, [p7_s0, p7_s0b, p7_s1, p7_s1b, p7_s2])
    emit_program(sch, nc)
    return nc, dbg_outs


def _consts():
    bf = ml_dtypes.bfloat16
    ident = np.eye(128, dtype=np.float32)
    iota = np.broadcast_to(np.arange(256, dtype=np.float32)[None, :], (128, 256)).copy()
    iotap = np.stack([np.arange(128, dtype=np.float32), np.arange(128, dtype=np.float32) + 128.0], axis=1)
    ustr = np.triu(np.ones((128, 128), dtype=np.float32), k=1)
    half = 16
    invf = (np.float32(10000.0) ** (-np.arange(half, dtype=np.float32) / np.float32(half))).astype(np.float32)
    invf = np.concatenate([invf] * 8).reshape(128, 1)
    tp = np.zeros((128, 16, 2), dtype=np.float32)
    tp[:, :, 0] = np.arange(16, dtype=np.float32)[None, :]
    tp[:, :, 1] = np.arange(128, dtype=np.float32)[:, None]
    return {
        "c_identf": ident, "c_identb": ident.astype(bf), "c_iota": iota, "c_iotap": np.ascontiguousarray(iotap),
        "c_ustr": ustr.astype(bf), "c_invf": invf, "c_tp": tp.reshape(128, 32).astype(bf), "c_gmat": np.kron(np.eye(16, dtype=np.float32), np.ones((8, 8), dtype=np.float32)),
    }


def _bc(v):
    return np.ascontiguousarray(np.broadcast_to(np.asarray(v, dtype=np.float32).reshape(1, -1), (128, v.size)))


def _pk(v, k):
    return np.ascontiguousarray(np.asarray(v, dtype=np.float32).reshape(k, 128).T)


_CACHE = {}


def make_in_maps(inputs, cores):
    f = lambda a: np.ascontiguousarray(np.asarray(a))
    shared = {
        "emb_ln_g": _bc(f(inputs["emb_ln_g"])), "emb_ln_b": _bc(f(inputs["emb_ln_b"])),
        "w_in": f(inputs["w_in"])[0], "q_norm_g": _pk(f(inputs["q_norm_g"])[0], 3), "w_qb": f(inputs["w_qb"])[0],
        "kv_norm_g": _pk(f(inputs["kv_norm_g"])[0], 2), "w_kvb": f(inputs["w_kvb"])[0],
        "conv_w": np.ascontiguousarray(f(inputs["conv_w"])[0].reshape(31, 4, 128).transpose(2, 1, 0).reshape(128, 4 * 31)),
        "conv_b": _pk(f(inputs["conv_b"])[0], 4), "conv_ln_g": _pk(f(inputs["conv_ln_g"])[0], 4), "conv_ln_b": _pk(f(inputs["conv_ln_b"])[0], 4),
        "w_o": f(inputs["w_o"])[0], "ln1_g": _bc(f(inputs["ln1_g"])[0]), "ln1_b": _bc(f(inputs["ln1_b"])[0]),
        "ln1_g_pk": _pk(f(inputs["ln1_g"])[0], 8), "ln1_b_pk": _pk(f(inputs["ln1_b"])[0], 8),
        "w_router": f(inputs["w_router"])[0], "w_gate": f(inputs["w_gate"])[0], "w_up": f(inputs["w_up"])[0], "w_down": f(inputs["w_down"])[0],
        "ln2_g": _bc(f(inputs["ln2_g"])[0]), "ln2_b": _bc(f(inputs["ln2_b"])[0]),
    }
    shared.update(_consts())
    x = f(inputs["x"])
    pos = f(inputs["positions"]).astype(np.int32)
    maps = []
    for c in cores:
        m = dict(shared)
        m["x"] = np.ascontiguousarray(x[c])
        m["pos"] = np.ascontiguousarray(np.broadcast_to(pos[c].reshape(4, 1, 512), (4, 32, 512)).reshape(128, 512))
        maps.append(m)
    return maps


def kernel(**inputs):
    if "nc" not in _CACHE:
        _CACHE["nc"] = build()[0]
    nc = _CACHE["nc"]
    cores = list(range(8))
    in_maps = make_in_maps(inputs, cores)
    res = run_bass_kernel_spmd(nc, in_maps, core_ids=cores)
    out = np.stack([np.asarray(r["out"]) for r in res.results], axis=0)
    return out.astype(np.float32)
```

```python
import numpy as np
import ml_dtypes
import concourse.bass as bass
import concourse.mybir as mybir
from concourse.bass_utils import run_bass_kernel_spmd

F32 = mybir.dt.float32
BF16 = mybir.dt.bfloat16
I32 = mybir.dt.int32
AF = mybir.ActivationFunctionType
ALU = mybir.AluOpType
AX = mybir.AxisListType

S = 2048
D = 1024
NT = 16
H = 8
E = 16
CAP = 256
FF = 2048
ALPHA = float(2.0 ** 0.25)
LN_EPS = 1e-5
RMS_EPS = 1e-6
SCALE = float(96.0 ** -0.5)
PI = float(np.pi)
TWO_PI = float(2.0 * np.pi)
NBIS = 28
FG = 512
NWB = 5
NPC = 8
ORDER = [8, 0, 9, 1, 10, 2, 11, 3, 12, 4, 13, 5, 14, 6, 15, 7]


class _Op:
    __slots__ = ("eng", "fn", "deps", "sig", "sigval", "is_dma", "dsem", "dval")


class Sched:
    ENG = ("pe", "act", "dve", "pool", "sp")

    def __init__(self, nc, n_dma_sems=42):
        self.nc = nc
        self.ops = []
        self.last_w = {}
        self.readers = {}
        self.n_dma = n_dma_sems
        self.dma_rr = 0
        self.dma_rrq = {}
        self.dma_last = [None] * n_dma_sems
        self.dma_count = [0] * n_dma_sems
        self.eng_last = {e: None for e in self.ENG}
        self.out_dmas = []
        self.capture = None
        self.precast_mode = False

    def add(self, eng, fn, r=(), w=(), dma=False):
        if self.capture is not None:
            self.capture.append((eng, fn, tuple(r), tuple(w), dma))
            return None
        op = _Op()
        op.eng = eng
        op.fn = fn
        op.deps = {}
        op.sig = False
        op.sigval = 0
        op.is_dma = dma
        for x in r:
            wr = self.last_w.get(x)
            if wr is not None:
                op.deps[wr] = "raw"
        for x in w:
            wr = self.last_w.get(x)
            if wr is not None and wr not in op.deps:
                op.deps[wr] = "waw"
            for rd in self.readers.get(x, ()):
                if rd is not op and rd not in op.deps:
                    op.deps[rd] = "war"
        for x in r:
            self.readers.setdefault(x, []).append(op)
        for x in w:
            self.last_w[x] = op
            self.readers[x] = []
        if dma:
            third = self.n_dma // 3
            qk = "pc" if self.precast_mode else eng
            base = {"sp": 0, "pool": third, "pc": 2 * third}.get(qk, 0)
            k = base + self.dma_rrq.get(qk, 0)
            self.dma_rrq[qk] = (self.dma_rrq.get(qk, 0) + 1) % third
            prev = self.dma_last[k]
            if prev is not None:
                op.deps[prev] = "raw"
            self.dma_count[k] += 1
            op.dsem = k
            op.dval = 16 * self.dma_count[k]
            self.dma_last[k] = op
        else:
            self.eng_last[eng] = op
        self.ops.append(op)
        return op

    def barrier(self):
        lasts = [o for o in self.eng_last.values() if o is not None]
        third = self.n_dma // 3
        lasts += [o for k_, o in enumerate(self.dma_last) if o is not None and k_ < 2 * third]
        for e in self.ENG:
            op = _Op()
            op.eng = e
            op.fn = None
            op.deps = {o: "raw" for o in lasts}
            op.sig = False
            op.sigval = 0
            op.is_dma = False
            self.ops.append(op)


def _mk_sems(nc, names):
    import contextlib
    st = contextlib.ExitStack()
    sems = [st.enter_context(nc.semaphore(n)) for n in names]
    return st, sems


def emit_program(sch, nc):
    engobj = {"pe": nc.tensor, "act": nc.scalar, "dve": nc.vector, "pool": nc.gpsimd, "sp": nc.sync}

    def skip(op, d, kind):
        if d.is_dma or op.is_dma:
            return False
        if d.eng != op.eng:
            return False
        if op.fn is None:
            return True
        if op.eng == "pe":
            return True
        return False

    for op in sch.ops:
        for d, kind in op.deps.items():
            if d.is_dma or skip(op, d, kind):
                continue
            d.sig = True
    cnt = {e: 0 for e in Sched.ENG}
    for op in sch.ops:
        if op.fn is not None and (not op.is_dma) and op.sig:
            cnt[op.eng] += 1
            op.sigval = cnt[op.eng]
    st, sems = _mk_sems(nc, ["se_" + e for e in Sched.ENG] + ["sd_%d" % i for i in range(sch.n_dma)])
    esem = {e: sems[i] for i, e in enumerate(Sched.ENG)}
    dsem = sems[len(Sched.ENG):]
    waited = {e: {} for e in Sched.ENG}
    with st:
        for op in sch.ops:
            eo = engobj[op.eng]
            need = {}
            for d, kind in op.deps.items():
                if skip(op, d, kind):
                    continue
                if d.is_dma:
                    key = ("d", d.dsem)
                    val = d.dval
                else:
                    key = ("e", d.eng)
                    val = d.sigval
                if need.get(key, 0) < val:
                    need[key] = val
            for key, val in need.items():
                if waited[op.eng].get(key, 0) >= val:
                    continue
                waited[op.eng][key] = val
                sem = dsem[key[1]] if key[0] == "d" else esem[key[1]]
                eo.wait_ge(sem, val)
            if op.fn is None:
                continue
            inst = op.fn(eo)
            if op.is_dma:
                inst.then_inc(dsem[op.dsem], 16)
            elif op.sig:
                inst.then_inc(esem[op.eng], 1)
        for k in range(sch.n_dma):
            if sch.dma_count[k] > 0:
                nc.sync.wait_ge(dsem[k], 16 * sch.dma_count[k])


class Arena:
    LO = 16512
    HI = 229344

    def __init__(self, nc):
        self.nc = nc
        self.free = [(self.LO, self.HI)]
        self.pending = []
        self.n = 0
        self.live = {}

    def alloc(self, name, shape, dtype):
        esz = 2 if dtype == BF16 else 4
        nbytes = esz
        for d_ in shape[1:]:
            nbytes *= d_
        nbytes = (nbytes + 63) // 64 * 64
        for i, (lo, hi) in enumerate(self.free):
            if hi - lo >= nbytes:
                self.free[i] = (lo + nbytes, hi)
                self.n += 1
                t = self.nc.alloc_sbuf_tensor_at("%s_%d" % (name, self.n), list(shape), dtype, offset=lo)
                self.live[id(t)] = (lo, lo + nbytes)
                return t
        raise RuntimeError("SBUF arena out of memory for %s (%d bytes) free=%s" % (name, nbytes, self.free))

    def release(self, *tiles):
        for t in tiles:
            self.pending.append(self.live.pop(id(t)))

    def flush(self):
        segs = sorted(self.free + self.pending)
        self.pending = []
        out = []
        for lo, hi in segs:
            if lo == hi:
                continue
            if out and out[-1][1] == lo:
                out[-1] = (out[-1][0], hi)
            else:
                out.append((lo, hi))
        self.free = out


def build(debug=None):
    nc = bass.Bass("TRN2", target_bir_lowering=False)
    sch = Sched(nc)
    ar = Arena(nc)
    A = sch.add
    dbg_outs = []

    def din(name, shape, dtype=F32):
        return nc.dram_tensor(name, list(shape), dtype, kind="ExternalInput").ap()

    x_d = din("x", [S, D])
    pos_d = din("pos", [128, 512], I32)
    g0_d = din("emb_ln_g", [128, D])
    b0_d = din("emb_ln_b", [128, D])
    w_in_d = din("w_in", [D, 1696])
    qg_d = din("q_norm_g", [128, 3])
    wqb_d = din("w_qb", [384, 768])
    kvg_d = din("kv_norm_g", [128, 2])
    wkvb_d = din("w_kvb", [256, 1024])
    cw_d = din("conv_w", [128, 4 * 31])
    cb_d = din("conv_b", [128, 4])
    clg_d = din("conv_ln_g", [128, 4])
    clb_d = din("conv_ln_b", [128, 4])
    wo_d = din("w_o", [D, D])
    g1_d = din("ln1_g", [128, D])
    b1_d = din("ln1_b", [128, D])
    wr_d = din("w_router", [D, E])
    g1pk_d = din("ln1_g_pk", [128, 8])
    b1pk_d = din("ln1_b_pk", [128, 8])
    wg_d = din("w_gate", [E, D, FF])
    wu_d = din("w_up", [E, D, FF])
    wd_d = din("w_down", [E, FF, D])
    g2_d = din("ln2_g", [128, D])
    b2_d = din("ln2_b", [128, D])
    c_identf_d = din("c_identf", [128, 128])
    c_identb_d = din("c_identb", [128, 128], BF16)
    c_iota_d = din("c_iota", [128, 256])
    c_iotap_d = din("c_iotap", [128, 2])
    c_ustr_d = din("c_ustr", [128, 128], BF16)
    c_invf_d = din("c_invf", [128, 1])
    c_tp_d = din("c_tp", [128, NT * 2], BF16)
    c_gmat_d = din("c_gmat", [128, 128])
    out_d = nc.dram_tensor("out", [S, D], F32, kind="ExternalOutput").ap()

    def dump(name, tile_ap, shape, dtype=F32):
        if debug is None or name not in debug:
            return
        t = nc.dram_tensor("dbg_" + name, list(shape), dtype, kind="ExternalOutput").ap()
        dbg_outs.append("dbg_" + name)
        sch.barrier()
        A("sp", lambda e: e.dma_start(out=t, in_=tile_ap), r=[], w=[], dma=True)
        sch.barrier()

    def DMA(q, out, in_, r=(), w=()):
        return A(q, lambda e: e.dma_start(out=out, in_=in_), r=r, w=w, dma=True)

    pst = [nc.alloc_psum_tensor("ps%d" % i, [128, 1024], F32) for i in range(4)]

    def bank(i):
        return pst[i // 2][:, (i % 2) * 512:(i % 2 + 1) * 512]

    def bankb(i):
        return pst[i // 2].bitcast(BF16)[:, (i % 2) * 1024:(i % 2 + 1) * 1024]

    def BK(i):
        return "psum%d" % i

    wpc = {}
    for e_ in range(NPC):
        wpc[("g", e_)] = nc.dram_tensor("wpc_g%d" % e_, [D, FF], BF16).ap()
        wpc[("u", e_)] = nc.dram_tensor("wpc_u%d" % e_, [D, FF], BF16).ap()
        wpc[("d", e_)] = nc.dram_tensor("wpc_d%d" % e_, [FF, D], BF16).ap()
    pc_jobs = []
    for e_ in range(NPC):
        for kind, src in (("g", wg_d), ("u", wu_d), ("d", wd_d)):
            rows = D if kind != "d" else FF
            for hh in range(2):
                pc_jobs.append((kind, e_, src, hh * rows // 2, (hh + 1) * rows // 2))
    pc_pos = [0]

    def precast(n):
        sch.precast_mode = True
        for _ in range(n):
            if pc_pos[0] >= len(pc_jobs):
                break
            kind, e_, src, r0, r1_ = pc_jobs[pc_pos[0]]
            pc_pos[0] += 1
            DMA("pool", wpc[(kind, e_)][r0:r1_, :], src[e_][r0:r1_, :], w=["wpc_%s%d_%d" % (kind, e_, r0)])
        sch.precast_mode = False

    identf = ar.alloc("identf", [128, 128], F32)
    identb = ar.alloc("identb", [128, 128], BF16)
    onesf = ar.alloc("onesf", [128, 128], F32)
    onesb = ar.alloc("onesb", [128, 128], BF16)
    iota_c = ar.alloc("iota_c", [128, 256], F32)
    iota_p = ar.alloc("iota_p", [128, 2], F32)
    ustr = ar.alloc("ustr", [128, 128], BF16)
    DMA("sp", identf[:], c_identf_d, w=["identf"])
    DMA("sp", identb[:], c_identb_d, w=["identb"])
    DMA("sp", iota_c[:], c_iota_d, w=["iota_c"])
    DMA("sp", iota_p[:], c_iotap_d, w=["iota_p"])
    DMA("sp", ustr[:], c_ustr_d, w=["ustr"])
    A("pool", lambda e: e.memset(onesf[:], 1.0), w=["onesf"])
    A("pool", lambda e: e.memset(onesb[:], 1.0), w=["onesb"])

    def swpipe(n, stages):
        ns = len(stages)
        for it in range(n + ns - 1):
            lists = []
            for si in range(ns):
                t_ = it - si
                if 0 <= t_ < n:
                    sch.capture = []
                    stages[si](t_)
                    lists.append(sch.capture)
                    sch.capture = None
            pos_ = [0] * len(lists)
            left = sum(len(l_) for l_ in lists)
            while left:
                for li, l_ in enumerate(lists):
                    if pos_[li] < len(l_):
                        eng, fn, r_, w_, dma_ = l_[pos_[li]]
                        pos_[li] += 1
                        left -= 1
                        sch.add(eng, fn, r_, w_, dma_)

    def ln_small(nm, nrot):
        return [dict(stats=ar.alloc(nm + "st", [128, 2, 6], F32), mv=ar.alloc(nm + "mv", [128, 2], F32), std=ar.alloc(nm + "sd", [128, 1], F32),
                     rstd=ar.alloc(nm + "rs", [128, 1], F32), nmr=ar.alloc(nm + "nm", [128, 1], F32), name="%s%d_" % (nm, i_)) for i_ in range(nrot)]

    def ln_small_free(lst):
        for d_ in lst:
            ar.release(d_["stats"], d_["mv"], d_["std"], d_["rstd"], d_["nmr"])

    def ln_part1a(T, src, src_res):
        n = T["name"]
        stats, mv, std, rstd, nmr = T["stats"], T["mv"], T["std"], T["rstd"], T["nmr"]

        def f_stats(e):
            e.bn_stats(out=stats[:, 0, :], in_=src[:, 0:512])
            return e.bn_stats(out=stats[:, 1, :], in_=src[:, 512:1024])
        A("dve", f_stats, r=list(src_res), w=[n + "st"])
        A("dve", lambda e: e.bn_aggr(out=mv[:], in_=stats[:]), r=[n + "st"], w=[n + "mv"])
        A("act", lambda e: e.activation(out=std[:], in_=mv[:, 1:2], func=AF.Ln, bias=epsln[:], scale=1.0), r=[n + "mv", "epsln"], w=[n + "sd"])
        A("act", lambda e: e.activation(out=rstd[:], in_=std[:], func=AF.Exp, scale=-0.5), r=[n + "sd"], w=[n + "rs"])
        A("dve", lambda e: e.scalar_tensor_tensor(out=nmr[:], in0=mv[:, 0:1], scalar=-1.0, in1=rstd[:], op0=ALU.mult, op1=ALU.mult), r=[n + "mv", n + "rs"], w=[n + "nm"])

    def ln_part1b(T, src, src_res, xn, xn_res):
        n = T["name"]
        rstd, nmr = T["rstd"], T["nmr"]
        A("act", lambda e: e.activation(out=xn, in_=src, func=AF.Identity, bias=nmr[:], scale=rstd[:]), r=list(src_res) + [n + "nm", n + "rs"], w=list(xn_res))

    def ln_part2(xn, xn_res, dst, dst_res, g_bc, b_bc, gres, bres):
        A("pool", lambda e: e.tensor_tensor(out=xn, in0=xn, in1=g_bc[:], op=ALU.mult), r=list(xn_res) + [gres], w=list(xn_res))
        A("dve", lambda e: e.tensor_tensor(out=dst, in0=xn, in1=b_bc[:], op=ALU.add), r=list(xn_res) + [bres], w=list(dst_res))

    epsln = ar.alloc("epsln", [128, 1], F32)
    epsrms = ar.alloc("epsrms", [128, 1], F32)
    A("pool", lambda e: e.memset(epsln[:], LN_EPS), w=["epsln"])
    A("pool", lambda e: e.memset(epsrms[:], RMS_EPS), w=["epsrms"])

    wq_f = ar.alloc("wq_f", [128, 3, 768], F32)
    wkv_f = ar.alloc("wkv_f", [128, 2, 1024], F32)
    qg = ar.alloc("qg", [128, 3], F32)
    kvg = ar.alloc("kvg", [128, 2], F32)
    Wq2 = ar.alloc("Wq2", [128, 3, 8, 128], BF16)
    Wk2 = ar.alloc("Wk2", [128, 2, 8, 64], BF16)
    Wv = ar.alloc("Wv", [128, 2, 8, 64], BF16)
    DMA("sp", wq_f[:], wqb_d.rearrange("(k p) f -> p k f", p=128), w=["wq_f"])
    DMA("sp", wkv_f[:], wkvb_d.rearrange("(k p) f -> p k f", p=128), w=["wkv_f"])
    DMA("sp", qg[:], qg_d, w=["qg"])
    DMA("sp", kvg[:], kvg_d, w=["kvg"])
    for k in range(3):
        src = wq_f[:, k, :].rearrange("p (h c) -> p h c", c=96)
        sc = qg[:, k:k + 1]
        A("dve", lambda e, src=src, sc=sc, k=k: e.tensor_scalar(out=Wq2[:, k, :, 64:96], in0=src[:, :, 64:96], scalar1=sc, scalar2=None, op0=ALU.mult),
          r=["wq_f", "qg"], w=["Wq2"])
        A("dve", lambda e, src=src, sc=sc, k=k: e.tensor_scalar(out=Wq2[:, k, :, 0:64], in0=src[:, :, 0:64], scalar1=sc, scalar2=None, op0=ALU.mult),
          r=["wq_f", "qg"], w=["Wq2"])
        A("dve", lambda e, src=src, sc=sc, k=k: e.tensor_scalar(out=Wq2[:, k, :, 96:112], in0=src[:, :, 80:96], scalar1=sc, scalar2=-1.0, op0=ALU.mult, op1=ALU.mult),
          r=["wq_f", "qg"], w=["Wq2"])
        A("dve", lambda e, src=src, sc=sc, k=k: e.tensor_scalar(out=Wq2[:, k, :, 112:128], in0=src[:, :, 64:80], scalar1=sc, scalar2=None, op0=ALU.mult),
          r=["wq_f", "qg"], w=["Wq2"])
    for k in range(2):
        src = wkv_f[:, k, :].rearrange("p (h c) -> p h c", c=128)
        sc = kvg[:, k:k + 1]
        A("dve", lambda e, src=src, sc=sc, k=k: e.tensor_scalar(out=Wk2[:, k, :, :], in0=src[:, :, 0:64], scalar1=sc, scalar2=None, op0=ALU.mult),
          r=["wkv_f", "kvg"], w=["Wk2"])
        A("dve", lambda e, src=src, sc=sc, k=k: e.tensor_scalar(out=Wv[:, k, :, :], in0=src[:, :, 64:128], scalar1=sc, scalar2=None, op0=ALU.mult),
          r=["wkv_f", "kvg"], w=["Wv"])

    cosT = ar.alloc("cosT", [96, S], F32)
    sinT = ar.alloc("sinT", [96, S], F32)
    pos_i = ar.alloc("pos_i", [128, 512], I32)
    ang = ar.alloc("ang", [128, 512], F32)
    rt0 = ar.alloc("rt0", [128, 512], F32)
    rt1 = ar.alloc("rt1", [128, 512], F32)
    rti = ar.alloc("rti", [128, 512], I32)
    rsc = [ar.alloc("rsc", [128, 512], F32) for _ in range(2)]
    invf = ar.alloc("invf", [128, 1], F32)
    DMA("sp", pos_i[:], pos_d, w=["pos_i"])
    DMA("sp", invf[:], c_invf_d, w=["invf"])
    A("dve", lambda e: e.tensor_copy(out=ang[:], in_=pos_i[:]), r=["pos_i"], w=["ang"])
    A("dve", lambda e: e.tensor_scalar(out=ang[:], in0=ang[:], scalar1=invf[:], scalar2=None, op0=ALU.mult), r=["ang", "invf"], w=["ang"])

    def range_reduce_sin(dst, dres, shift, tmp, tres):
        A("dve", lambda e: e.tensor_scalar(out=rt0[:], in0=ang[:], scalar1=shift, scalar2=1.0 / TWO_PI, op0=ALU.add, op1=ALU.mult), r=["ang"], w=["rt0"])
        A("dve", lambda e: e.tensor_copy(out=rti[:], in_=rt0[:]), r=["rt0"], w=["rti"])
        A("dve", lambda e: e.tensor_copy(out=rt0[:], in_=rti[:]), r=["rti"], w=["rt0"])
        A("dve", lambda e: e.tensor_scalar(out=rt1[:], in0=ang[:], scalar1=shift, scalar2=None, op0=ALU.add), r=["ang"], w=["rt1"])
        A("dve", lambda e: e.scalar_tensor_tensor(out=rt1[:], in0=rt0[:], scalar=-TWO_PI, in1=rt1[:], op0=ALU.mult, op1=ALU.add), r=["rt0", "rt1"], w=["rt1"])
        A("dve", lambda e: e.tensor_scalar(out=rt0[:], in0=rt1[:], scalar1=PI, scalar2=None, op0=ALU.is_gt), r=["rt1"], w=["rt0"])
        A("dve", lambda e: e.scalar_tensor_tensor(out=rt1[:], in0=rt0[:], scalar=-TWO_PI, in1=rt1[:], op0=ALU.mult, op1=ALU.add), r=["rt0", "rt1"], w=["rt1"])
        A("dve", lambda e: e.tensor_scalar(out=rt0[:], in0=rt1[:], scalar1=-PI, scalar2=None, op0=ALU.is_lt), r=["rt1"], w=["rt0"])
        A("dve", lambda e: e.scalar_tensor_tensor(out=rt1[:], in0=rt0[:], scalar=TWO_PI, in1=rt1[:], op0=ALU.mult, op1=ALU.add), r=["rt0", "rt1"], w=["rt1"])
        A("dve", lambda e: e.tensor_scalar(out=rt1[:], in0=rt1[:], scalar1=-3.1415925, scalar2=3.1415925, op0=ALU.max, op1=ALU.min), r=["rt1"], w=["rt1"])
        A("act", lambda e: e.activation(out=tmp[:], in_=rt1[:], func=AF.Sin), r=["rt1"], w=[tres])
        for blk in range(4):
            DMA("sp", dst[64:96, blk * 512:(blk + 1) * 512], tmp[blk * 32:(blk + 1) * 32, :], r=[tres], w=[dres])

    range_reduce_sin(sinT, "sinT", 0.0, rsc[0], "rsc0")
    range_reduce_sin(cosT, "cosT", PI / 2.0, rsc[1], "rsc1")

    cw = ar.alloc("cw", [128, 4 * 31], F32)
    Dg = ar.alloc("Dg", [128, 4 * 31, 128], BF16)
    DMA("sp", cw[:], cw_d, w=["cw"])
    for m in range(4):
        A("dve", lambda e, m=m: e.tensor_tensor(out=Dg[:, m * 31:(m + 1) * 31, :], in0=identb[:].unsqueeze(1).to_broadcast([128, 31, 128]),
                                               in1=cw[:, m * 31:(m + 1) * 31].unsqueeze(2).to_broadcast([128, 31, 128]), op=ALU.mult),
          r=["identb", "cw"], w=["Dg%d" % m])

    hT = ar.alloc("hT", [128, 8, S], BF16)
    g0 = ar.alloc("g0", [128, D], F32)
    b0 = ar.alloc("b0", [128, D], F32)
    DMA("sp", g0[:], g0_d, w=["g0"])
    DMA("sp", b0[:], b0_d, w=["b0"])
    NR1 = 6
    xt = [ar.alloc("xt", [128, D], F32) for _ in range(NR1)]
    hb = [ar.alloc("hb", [128, D], BF16) for _ in range(NR1)]
    lnA = ln_small("lnA", NR1)

    def p1_s0(t):
        p = t % NR1
        DMA("sp", xt[p][:], x_d[t * 128:(t + 1) * 128, :], w=["xt%d" % p])

    def p1_s0b(t):
        pass

    def p1_s1(t):
        p = t % NR1
        ln_part1a(lnA[p], xt[p][:], ["xt%d" % p])

    def p1_s1b(t):
        p = t % NR1
        ln_part1b(lnA[p], xt[p][:], ["xt%d" % p], xt[p][:], ["xt%d" % p])

    def p1_s2(t):
        p = t % NR1
        ln_part2(xt[p][:], ["xt%d" % p], hb[p][:], ["hb%d" % p], g0, b0, "g0", "b0")

    def p1_s3(t):
        p = t % NR1
        pb_ = t % 2

        def f_tr(e, p=p, pb_=pb_):
            for k in range(8):
                i_ = e.transpose(out=bankb(pb_)[:, k * 128:(k + 1) * 128], in_=hb[p][:, k * 128:(k + 1) * 128], identity=identb[:])
            return i_
        A("pe", f_tr, r=["hb%d" % p, "identb"], w=[BK(pb_)])
        A("act", lambda e, pb_=pb_, t=t: e.activation(out=hT[:, :, t * 128:(t + 1) * 128], in_=bankb(pb_).rearrange("p (k c) -> p k c", c=128), func=AF.Copy),
          r=[BK(pb_)], w=["hT%d" % (t // 4)])
    swpipe(NT, [p1_s0, p1_s0b, p1_s1, p1_s1b, p1_s2, p1_s3])
    if debug and "hT" in debug:
        dump("hT", hT[:], [128, 8, S], BF16)
    sch.barrier()
    ar.release(wq_f, wkv_f, qg, kvg, pos_i, ang, rt0, rt1, rti, invf, *rsc, *xt, *hb, g0, b0)
    ln_small_free(lnA)
    ar.flush()

    convT = ar.alloc("convT", [128, 4, S], BF16)
    wc = ar.alloc("wc", [128, 8, 1024], BF16)
    hcp = ar.alloc("hcp", [128, 4, S + 30], BF16)
    cb = ar.alloc("cb", [128, 4], F32)
    clg = ar.alloc("clg", [128, 4], F32)
    clb = ar.alloc("clb", [128, 4], F32)
    sig = [ar.alloc("sig", [128, 512], F32) for _ in range(2)]
    DMA("pool", wc[:], w_in_d.rearrange("(k p) f -> p k f", p=128)[:, :, 672:1696], w=["wc"])
    DMA("sp", cb[:], cb_d, w=["cb"])
    DMA("sp", clg[:], clg_d, w=["clg"])
    DMA("sp", clb[:], clb_d, w=["clb"])
    A("pool", lambda e: e.memset(hcp[:, :, 0:15], 0.0), w=["hcp_lo"])
    A("pool", lambda e: e.memset(hcp[:, :, S + 15:S + 30], 0.0), w=["hcp_hi"])
    precast(16)
    it = 0
    for b in range(4):
        for m in range(4):
            pa = (it % 2) * 2
            pg = pa + 1
            sp_ = it % 2
            it += 1

            def f_glu(e, m=m, b=b, pa=pa, pg=pg):
                for k in range(8):
                    e.matmul(bank(pa), lhsT=wc[:, k, m * 128:(m + 1) * 128], rhs=hT[:, k, b * 512:(b + 1) * 512], start=(k == 0), stop=(k == 7))
                for k in range(8):
                    i_ = e.matmul(bank(pg), lhsT=wc[:, k, 512 + m * 128:512 + (m + 1) * 128], rhs=hT[:, k, b * 512:(b + 1) * 512], start=(k == 0), stop=(k == 7))
                return i_
            A("pe", f_glu, r=["wc", "hT%d" % b], w=[BK(pa), BK(pg)])
            A("act", lambda e, pg=pg, sp_=sp_: e.activation(out=sig[sp_][:], in_=bank(pg), func=AF.Sigmoid), r=[BK(pg)], w=["sig%d" % sp_])
            A("dve", lambda e, pa=pa, sp_=sp_, m=m, b=b: e.tensor_tensor(out=hcp[:, m, 15 + b * 512:15 + (b + 1) * 512], in0=bank(pa), in1=sig[sp_][:], op=ALU.mult),
              r=[BK(pa), "sig%d" % sp_], w=["hcp%d_%d" % (m, b)])
    yc = [ar.alloc("yc", [128, 4, 512], F32) for _ in range(2)]
    ysq = [ar.alloc("ysq", [128, 512], F32) for _ in range(2)]
    cmean = [ar.alloc("cmean", [128, 512], F32) for _ in range(2)]
    cm2 = [ar.alloc("cm2", [128, 512], F32) for _ in range(2)]
    crstd = [ar.alloc("crstd", [128, 512], F32) for _ in range(2)]
    ctmp = [ar.alloc("ctmp", [128, 512], F32) for _ in range(2)]
    seq = [(b, m) for b in range(4) for m in range(4)]

    def conv_step(i):
        b, m = seq[i]
        pb = b % 2
        pc = i % 2
        rds = ["Dg%d" % m, "hcp%d_%d" % (m, b)]
        rds.append("hcp%d_%d" % (m, b - 1) if b > 0 else "hcp_lo")
        rds.append("hcp%d_%d" % (m, b + 1) if b < 3 else "hcp_hi")

        def f_conv(e, m=m, b=b, pc=pc):
            for k in range(31):
                i_ = e.matmul(bank(pc), lhsT=Dg[:, m * 31 + k, :], rhs=hcp[:, m, b * 512 + k:b * 512 + k + 512], start=(k == 0), stop=(k == 30))
            return i_
        A("pe", f_conv, r=rds, w=[BK(pc)])
        A("act", lambda e, m=m, pb=pb, pc=pc: e.activation(out=yc[pb][:, m, :], in_=bank(pc), func=AF.Identity, bias=cb[:, m:m + 1], scale=1.0),
          r=[BK(pc), "cb"], w=["yc%d_%d" % (pb, m)])
        A("act", lambda e, m=m, pc=pc: e.activation(out=ysq[pc][:], in_=bank(pc), func=AF.Square, bias=cb[:, m:m + 1], scale=1.0),
          r=[BK(pc), "cb"], w=["ysq%d" % pc])

    def stat_step(i):
        b, m = seq[i]
        pb = b % 2
        pc = i % 2
        s1 = 4 + pb * 2
        s2 = 5 + pb * 2

        def f_st(e, m=m, pb=pb, pc=pc, s1=s1, s2=s2):
            e.matmul(bank(s1), lhsT=onesf[:], rhs=yc[pb][:, m, :], start=(m == 0), stop=(m == 3))
            return e.matmul(bank(s2), lhsT=onesf[:], rhs=ysq[pc][:], start=(m == 0), stop=(m == 3))
        A("pe", f_st, r=["onesf", "yc%d_%d" % (pb, m), "ysq%d" % pc], w=[BK(s1), BK(s2)])

    def norm_block(b):
        pb = b % 2
        s1 = 4 + pb * 2
        s2 = 5 + pb * 2
        A("dve", lambda e, pb=pb, s1=s1: e.tensor_scalar(out=cmean[pb][:], in0=bank(s1), scalar1=1.0 / 512.0, scalar2=None, op0=ALU.mult), r=[BK(s1)], w=["cmean%d" % pb])
        A("dve", lambda e, pb=pb: e.tensor_tensor(out=cm2[pb][:], in0=cmean[pb][:], in1=cmean[pb][:], op=ALU.mult), r=["cmean%d" % pb], w=["cm2%d" % pb])
        A("dve", lambda e, pb=pb, s2=s2: e.scalar_tensor_tensor(out=cm2[pb][:], in0=bank(s2), scalar=1.0 / 512.0, in1=cm2[pb][:], op0=ALU.mult, op1=ALU.subtract),
          r=[BK(s2), "cm2%d" % pb], w=["cm2%d" % pb])
        A("act", lambda e, pb=pb: e.activation(out=crstd[pb][:], in_=cm2[pb][:], func=AF.Ln, bias=epsln[:], scale=1.0), r=["cm2%d" % pb, "epsln"], w=["crstd%d" % pb])
        A("act", lambda e, pb=pb: e.activation(out=crstd[pb][:], in_=crstd[pb][:], func=AF.Exp, scale=-0.5), r=["crstd%d" % pb], w=["crstd%d" % pb])
        for m in range(4):
            pc = m % 2
            A("dve", lambda e, pb=pb, m=m, pc=pc: e.tensor_tensor(out=ctmp[pc][:], in0=yc[pb][:, m, :], in1=cmean[pb][:], op=ALU.subtract),
              r=["yc%d_%d" % (pb, m), "cmean%d" % pb], w=["ctmp%d" % pc])
            A("dve", lambda e, pb=pb, pc=pc: e.tensor_tensor(out=ctmp[pc][:], in0=ctmp[pc][:], in1=crstd[pb][:], op=ALU.mult),
              r=["ctmp%d" % pc, "crstd%d" % pb], w=["ctmp%d" % pc])
            A("act", lambda e, m=m, b=b, pc=pc: e.activation(out=convT[:, m, b * 512:(b + 1) * 512], in_=ctmp[pc][:], func=AF.Silu, bias=clb[:, m:m + 1], scale=clg[:, m:m + 1]),
              r=["ctmp%d" % pc, "clg", "clb"], w=["convT%d" % b])

    for i in range(17):
        if i < 16:
            conv_step(i)
        if i >= 1:
            stat_step(i - 1)
        if i >= 6 and (i - 6) % 4 == 0:
            norm_block((i - 6) // 4)
    norm_block(3)
    if debug and "convT" in debug:
        dump("convT", convT[:], [128, 4, S], BF16)
    sch.barrier()
    ar.release(wc, hcp, cw, cb, clg, clb, Dg, *sig, *yc, *ysq, *cmean, *cm2, *crstd, *ctmp)
    ar.flush()

    qT = ar.alloc("qT", [128, 8, S], BF16)
    kT = ar.alloc("kT", [128, 8, S], BF16)
    vaug = ar.alloc("vaug", [128, NT, 8, 65], BF16)
    wl = ar.alloc("wl", [128, 8, 704], BF16)
    DMA("pool", wl[:, :, 0:672], w_in_d.rearrange("(k p) f -> p k f", p=128)[:, :, 0:672], w=["wl"])
    A("dve", lambda e: e.tensor_scalar(out=wl[:, :, 672:688], in0=wl[:, :, 656:672], scalar1=-1.0, scalar2=None, op0=ALU.mult), r=["wl"], w=["wl"])
    A("dve", lambda e: e.tensor_copy(out=wl[:, :, 688:704], in_=wl[:, :, 640:656]), r=["wl"], w=["wl"])
    A("pool", lambda e: e.memset(vaug[:, :, :, 64:65], 1.0), w=["vaug1"])
    precast(16)
    cqb = ar.alloc("cqb", [128, 3, 512], BF16)
    ckvb = ar.alloc("ckvb", [128, 2, 512], BF16)
    cqn = [ar.alloc("cqn", [128, 3, 512], BF16) for _ in range(2)]
    ckvn = [ar.alloc("ckvn", [128, 2, 512], BF16) for _ in range(2)]
    sqq = [ar.alloc("sqq", [128, 512], F32) for _ in range(2)]
    rq = ar.alloc("rq", [128, 512], F32)
    rk = ar.alloc("rk", [128, 512], F32)
    t1 = ar.alloc("t1", [96, 512], F32)
    t2 = ar.alloc("t2", [96, 512], F32)
    sq_ctr = [0]

    def proj_chunk(b, col0, dst, dres, j, sbank, nj, jj):
        pj = sq_ctr[0] % 2
        sq_ctr[0] += 1

        def f_p(e, pj=pj, b=b, col0=col0):
            for k in range(8):
                i_ = e.matmul(bank(pj), lhsT=wl[:, k, col0:col0 + 128], rhs=hT[:, k, b * 512:(b + 1) * 512], start=(k == 0), stop=(k == 7))
            return i_
        A("pe", f_p, r=["wl", "hT%d" % b], w=[BK(pj)])
        A("act", lambda e, pj=pj, j=j: e.activation(out=dst[:, j, :], in_=bank(pj), func=AF.Copy), r=[BK(pj)], w=[dres + "%d" % j])
        A("act", lambda e, pj=pj: e.activation(out=sqq[pj][:], in_=bank(pj), func=AF.Square), r=[BK(pj)], w=["sqq%d" % pj])
        return lambda: A("pe", lambda e, pj=pj: e.matmul(bank(sbank), lhsT=onesf[:], rhs=sqq[pj][:], start=(jj == 0), stop=(jj == nj - 1)), r=["onesf", "sqq%d" % pj], w=[BK(sbank)])

    def rstd_ops(sbank, n, r_, rres):
        A("act", lambda e: e.activation(out=r_[:], in_=bank(sbank), func=AF.Ln, bias=epsrms[:], scale=1.0 / n), r=[BK(sbank), "epsrms"], w=[rres])
        A("act", lambda e: e.activation(out=r_[:], in_=r_[:], func=AF.Exp, scale=-0.5), r=[rres], w=[rres])

    def stage_P(b):
        pb = b % 2
        bs = slice(b * 512, (b + 1) * 512)
        pend = None
        for j in range(3):
            nxt = proj_chunk(b, j * 128, cqb, "cqb", j, 2, 3, j)
            if pend:
                pend()
            pend = nxt
        for j in range(2):
            nxt = proj_chunk(b, 384 + j * 128, ckvb, "ckvb", j, 3, 2, j)
            pend()
            pend = nxt

        def f_kr2(e, b=b):
            for k in range(8):
                e.matmul(bank(4)[64:96, :], lhsT=wl[:, k, 640:672], rhs=hT[:, k, b * 512:(b + 1) * 512], start=(k == 0), stop=(k == 7))
            for k in range(8):
                i_ = e.matmul(bank(5)[64:96, :], lhsT=wl[:, k, 672:704], rhs=hT[:, k, b * 512:(b + 1) * 512], start=(k == 0), stop=(k == 7))
            return i_
        A("pe", f_kr2, r=["wl", "hT%d" % b], w=[BK(4), BK(5)])
        pend()
        rstd_ops(2, 384.0, rq, "rq")
        rstd_ops(3, 256.0, rk, "rk")
        for j in range(3):
            A("dve", lambda e, j=j, pb=pb: e.tensor_tensor(out=cqn[pb][:, j, :], in0=cqb[:, j, :], in1=rq[:], op=ALU.mult), r=["cqb%d" % j, "rq"], w=["cqn%d_%d" % (pb, j)])
        for j in range(2):
            A("dve", lambda e, j=j, pb=pb: e.tensor_tensor(out=ckvn[pb][:, j, :], in0=ckvb[:, j, :], in1=rk[:], op=ALU.mult), r=["ckvb%d" % j, "rk"], w=["ckvn%d_%d" % (pb, j)])
        A("dve", lambda e, bs=bs: e.tensor_tensor(out=t1[64:96, :], in0=bank(4)[64:96, :], in1=cosT[64:96, bs], op=ALU.mult), r=[BK(4), "cosT"], w=["t1"])
        A("dve", lambda e, bs=bs: e.tensor_tensor(out=t2[64:96, :], in0=bank(5)[64:96, :], in1=sinT[64:96, bs], op=ALU.mult), r=[BK(5), "sinT"], w=["t2"])
        A("dve", lambda e, bs=bs: e.tensor_tensor(out=kT[64:96, 0, bs], in0=t1[64:96, :], in1=t2[64:96, :], op=ALU.add), r=["t1", "t2"], w=["kTr0"])
        for h in range(1, H):
            A("dve" if h % 2 else "act", (lambda e, bs=bs, h=h: e.tensor_copy(out=kT[64:96, h, bs], in_=kT[64:96, 0, bs])) if h % 2 else
              (lambda e, bs=bs, h=h: e.activation(out=kT[64:96, h, bs], in_=kT[64:96, 0, bs], func=AF.Copy)), r=["kTr0"], w=["kTr%d" % h])

    def stage_H(b):
        pb = b % 2
        bs = slice(b * 512, (b + 1) * 512)
        cq_r = ["cqn%d_%d" % (pb, j) for j in range(3)]
        ckv_r = ["ckvn%d_%d" % (pb, j) for j in range(2)]
        for h in range(H):
            pq = 6 + (h % 2)
            pr = 4 + (h % 2)

            def f_q(e, h=h, pq=pq, pr=pr, pb=pb):
                for k in range(3):
                    e.matmul(bank(pq)[0:96, :], lhsT=Wq2[:, k, h, 0:96], rhs=cqn[pb][:, k, :], start=(k == 0), stop=(k == 2))
                for k in range(3):
                    i_ = e.matmul(bank(pr)[64:96, :], lhsT=Wq2[:, k, h, 96:128], rhs=cqn[pb][:, k, :], start=(k == 0), stop=(k == 2))
                return i_
            A("pe", f_q, r=["Wq2"] + cq_r, w=[BK(pq), BK(pr)])
            A("act", lambda e, h=h, pq=pq, bs=bs: e.activation(out=qT[0:64, h, bs], in_=bank(pq)[0:64, :], func=AF.Copy), r=[BK(pq)], w=["qT%d_%d" % (h, b)])
            A("dve", lambda e, pq=pq, bs=bs: e.tensor_tensor(out=t1[64:96, :], in0=bank(pq)[64:96, :], in1=cosT[64:96, bs], op=ALU.mult), r=[BK(pq), "cosT"], w=["t1"])
            A("dve", lambda e, pr=pr, bs=bs: e.tensor_tensor(out=t2[64:96, :], in0=bank(pr)[64:96, :], in1=sinT[64:96, bs], op=ALU.mult), r=[BK(pr), "sinT"], w=["t2"])
            A("dve", lambda e, h=h, bs=bs: e.tensor_tensor(out=qT[64:96, h, bs], in0=t1[64:96, :], in1=t2[64:96, :], op=ALU.add), r=["t1", "t2"], w=["qTr%d_%d" % (h, b)])
        for h in range(H):
            pk = 6 + (h % 2)

            def f_k(e, h=h, pk=pk, pb=pb):
                for k in range(2):
                    i_ = e.matmul(bank(pk)[0:64, :], lhsT=Wk2[:, k, h, :], rhs=ckvn[pb][:, k, :], start=(k == 0), stop=(k == 1))
                return i_
            A("pe", f_k, r=["Wk2"] + ckv_r, w=[BK(pk)])
            A("act", lambda e, h=h, pk=pk, bs=bs: e.activation(out=kT[0:64, h, bs], in_=bank(pk)[0:64, :], func=AF.Copy), r=[BK(pk)], w=["kT%d_%d" % (h, b)])
        for tt in range(4):
            pv = 4 + (tt % 2)
            t = b * 4 + tt

            def f_v(e, tt=tt, pv=pv, pb=pb):
                for k in range(2):
                    i_ = e.matmul(bank(pv), lhsT=ckvn[pb][:, k, tt * 128:(tt + 1) * 128], rhs=Wv[:, k, :, :].rearrange("p h c -> p (h c)"), start=(k == 0), stop=(k == 1))
                return i_
            A("pe", f_v, r=["Wv"] + ckv_r, w=[BK(pv)])
            A("act", lambda e, pv=pv, t=t: e.activation(out=vaug[:, t, :, 0:64], in_=bank(pv).rearrange("p (h c) -> p h c", c=64), func=AF.Copy),
              r=[BK(pv)], w=["vaug%d" % t])

    stage_P(0)
    for b in range(4):
        if b + 1 < 4:
            stage_P(b + 1)
        stage_H(b)
    if debug and "qT" in debug:
        dump("qT", qT[:], [128, 8, S], BF16)
        dump("kT", kT[:], [128, 8, S], BF16)
        dump("vaug", vaug[:], [128, NT, 8, 65], BF16)
    sch.barrier()
    ar.release(hT, wl, cqb, ckvb, *cqn, *ckvn, *sqq, rq, rk, t1, t2, cosT, sinT, Wq2, Wk2, Wv)
    ar.flush()

    precast(16)
    attn_tok = ar.alloc("attn_tok", [128, NT, 512], BF16)
    ptb = [ar.alloc("ptb", [128, 1024], BF16) for _ in range(3)]
    rec = [ar.alloc("rec", [128, 4], F32) for _ in range(2)]
    steps = []
    for h in range(H):
        for qb in range(4):
            for kp in range(8):
                steps.append((h, qb, kp))

    def emit_scores(i):
        h, qb, kp = steps[i]
        sp_ = i % 2

        def f_s(e, h=h, qb=qb, kp=kp, sp_=sp_):
            for half in range(2):
                kt = kp * 2 + half
                i_ = e.matmul(pst[sp_][:, half * 512:(half + 1) * 512], lhsT=kT[0:96, h, kt * 128:(kt + 1) * 128], rhs=qT[0:96, h, qb * 512:(qb + 1) * 512], start=True, stop=True)
            return i_
        rds = ["kT%d_%d" % (h, (kp * 2) // 4), "kTr%d" % h, "qT%d_%d" % (h, qb), "qTr%d_%d" % (h, qb)]
        A("pe", f_s, r=rds, w=[BK(2 * sp_), BK(2 * sp_ + 1)])
        pp = i % 3
        A("act", lambda e, sp_=sp_, pp=pp: e.activation(out=ptb[pp][:], in_=pst[sp_][:], func=AF.Exp, scale=SCALE), r=[BK(2 * sp_), BK(2 * sp_ + 1)], w=["ptb%d" % pp])

    def emit_pv(i):
        h, qb, kp = steps[i]
        pp = i % 3
        g = i // 8
        ob = 4 + (g % 2)
        O = bank(ob).rearrange("p (q c) -> p q c", c=128)

        def f_pv(e, h=h, kp=kp, pp=pp, O=O):
            for half in range(2):
                kt = kp * 2 + half
                for qt in range(4):
                    i_ = e.matmul(O[:, qt, 0:65], lhsT=ptb[pp][:, half * 512 + qt * 128:half * 512 + (qt + 1) * 128], rhs=vaug[:, kt, h, :],
                                  start=(kt == 0 and qt == 0), stop=(kt == 15), skip_group_check=True)
            return i_
        rds = ["ptb%d" % pp, "vaug1"] + ["vaug%d" % (kp * 2), "vaug%d" % (kp * 2 + 1)]
        A("pe", f_pv, r=rds, w=[BK(ob)])
        if kp == 7:
            pr_ = g % 2
            A("dve", lambda e, O=O, pr_=pr_: e.reciprocal(out=rec[pr_][:], in_=O[:, :, 64]), r=[BK(ob)], w=["rec%d" % pr_])
            for qt in range(4):
                t = qb * 4 + qt
                A("dve", lambda e, O=O, pr_=pr_, qt=qt, t=t, h=h: e.tensor_scalar(out=attn_tok[:, t, h * 64:(h + 1) * 64], in0=O[:, qt, 0:64], scalar1=rec[pr_][:, qt:qt + 1], scalar2=None, op0=ALU.mult),
                  r=[BK(ob), "rec%d" % pr_], w=["attn_tok%d" % t])

    nst = len(steps)
    emit_scores(0)
    for i in range(nst):
        if i + 1 < nst:
            emit_scores(i + 1)
        emit_pv(i)
    if debug and "attn_tok" in debug:
        dump("attn_tok", attn_tok[:], [128, NT, 512], BF16)
    sch.barrier()
    ar.release(qT, kT, vaug, *ptb, *rec)
    ar.flush()

    attnT = ar.alloc("attnT", [128, 4, S], BF16)
    for t in range(NT):
        p = t % 2

        def f_tr2(e, t=t, p=p):
            for c in range(4):
                i_ = e.transpose(out=bankb(p)[:, c * 128:(c + 1) * 128], in_=attn_tok[:, t, c * 128:(c + 1) * 128], identity=identb[:])
            return i_
        A("pe", f_tr2, r=["attn_tok%d" % t, "identb"], w=[BK(p)])
        A("act", lambda e, t=t, p=p: e.activation(out=attnT[:, :, t * 128:(t + 1) * 128], in_=bankb(p)[:, 0:512].rearrange("p (k c) -> p k c", c=128), func=AF.Copy),
          r=[BK(p)], w=["attnT%d" % t])
    sch.barrier()
    ar.release(attn_tok)
    ar.flush()

    macc = nc.dram_tensor("moe_acc", [S, D], F32).ap()
    h1b = ar.alloc("h1b", [128, NT, D], BF16)
    wo = ar.alloc("wo", [128, 8, D], BF16)
    wr = ar.alloc("wr", [128, 8, E], F32)
    wrg = ar.alloc("wrg", [128, 8, E], F32)
    g0 = ar.alloc("g0", [128, D], F32)
    b0 = ar.alloc("b0", [128, D], F32)
    g1 = ar.alloc("g1", [128, D], F32)
    b1 = ar.alloc("b1", [128, D], F32)
    g1pk = ar.alloc("g1pk", [128, 8], F32)
    b1pk = ar.alloc("b1pk", [128, 8], F32)
    brow = ar.alloc("brow", [1, E], F32)
    aff = ar.alloc("aff", [128, NT, E], F32)
    DMA("pool", wo[:], wo_d.rearrange("(k p) f -> p k f", p=128), w=["wo"])
    DMA("sp", wr[:], wr_d.rearrange("(k p) f -> p k f", p=128), w=["wr"])
    DMA("sp", g0[:], g0_d, w=["g0"])
    DMA("sp", b0[:], b0_d, w=["b0"])
    DMA("sp", g1[:], g1_d, w=["g1"])
    DMA("sp", b1[:], b1_d, w=["b1"])
    DMA("sp", g1pk[:], g1pk_d, w=["g1pk"])
    DMA("sp", b1pk[:], b1pk_d, w=["b1pk"])
    A("dve", lambda e: e.tensor_scalar(out=g0[:], in0=g0[:], scalar1=ALPHA, scalar2=None, op0=ALU.mult), r=["g0"], w=["g0"])
    A("dve", lambda e: e.tensor_scalar(out=b0[:], in0=b0[:], scalar1=ALPHA, scalar2=None, op0=ALU.mult), r=["b0"], w=["b0"])
    bh = ar.alloc("bh", [1, D], BF16)
    bl = ar.alloc("bl", [1, D], BF16)
    blf = ar.alloc("blf", [1, D], F32)
    A("dve", lambda e: e.tensor_copy(out=bh[:], in_=b0[0:1, :]), r=["b0"], w=["bh"])
    A("dve", lambda e: e.tensor_tensor(out=blf[:], in0=b0[0:1, :], in1=bh[:], op=ALU.subtract), r=["b0", "bh"], w=["blf"])
    A("dve", lambda e: e.tensor_copy(out=bl[:], in_=blf[:]), r=["blf"], w=["bl"])
    A("dve", lambda e: e.tensor_scalar(out=g1[:], in0=g1[:], scalar1=ALPHA, scalar2=None, op0=ALU.mult), r=["g1"], w=["g1"])
    A("dve", lambda e: e.tensor_scalar(out=b1[:], in0=b1[:], scalar1=ALPHA, scalar2=None, op0=ALU.mult), r=["b1"], w=["b1"])
    for k in range(8):
        A("dve", lambda e, k=k: e.tensor_scalar(out=wrg[:, k, :], in0=wr[:, k, :], scalar1=g1pk[:, k:k + 1], scalar2=None, op0=ALU.mult), r=["wr", "g1pk"], w=["wrg"])

    def f_brow(e):
        for k in range(8):
            i_ = e.matmul(bank(7)[0:1, 0:E], lhsT=b1pk[:, k:k + 1], rhs=wr[:, k, :], start=(k == 0), stop=(k == 7))
        return i_
    A("pe", f_brow, r=["b1pk", "wr"], w=[BK(7)])
    A("dve", lambda e: e.tensor_copy(out=brow[:], in_=bank(7)[0:1, 0:E]), r=[BK(7)], w=["brow"])
    NR4 = 6
    NRX = 7
    NRZ = 5
    lgb = [bank(6)[:, 0:E], bank(7)[:, 0:E]]
    xt = [ar.alloc("xt", [128, D], F32) for _ in range(NRX)]
    zt = [ar.alloc("zt", [128, D], F32) for _ in range(NRZ)]
    r1 = [ar.alloc("r1", [128, D], F32) for _ in range(2)]
    h1T = [ar.alloc("h1T", [128, D], F32) for _ in range(2)]
    smx = [ar.alloc("smx", [128, 1], F32) for _ in range(2)]
    ssum = [ar.alloc("ssum", [128, 1], F32) for _ in range(2)]
    sex = [ar.alloc("sex", [128, E], F32) for _ in range(2)]
    lnB = ln_small("lnB", NR4)
    lnC = ln_small("lnC", NR4)

    def p4_s0(t):
        p = t % NRX
        DMA("sp", xt[p][:], x_d[t * 128:(t + 1) * 128, :], w=["xt%d" % p])

    def p4_s0b(t):
        pass

    def p4_s1(t):
        p = t % NRX
        ln_part1a(lnB[t % NR4], xt[p][:], ["xt%d" % p])

    def p4_s1b(t):
        p = t % NRX
        ln_part1b(lnB[t % NR4], xt[p][:], ["xt%d" % p], xt[p][:], ["xt%d" % p])

    def p4_s2a(t):
        p = t % NRX
        A("pool", lambda e, p=p: e.tensor_tensor(out=xt[p][:], in0=xt[p][:], in1=g0[:], op=ALU.mult), r=["xt%d" % p, "g0"], w=["xt%d" % p])
        for half in range(2):
            pw = 2 * (t % 2) + half

            def f_wo(e, t=t, half=half, pw=pw):
                for k in range(8):
                    src = attnT[:, k, t * 128:(t + 1) * 128] if k < 4 else convT[:, k - 4, t * 128:(t + 1) * 128]
                    e.matmul(bank(pw), lhsT=src, rhs=wo[:, k, half * 512:(half + 1) * 512], start=(k == 0), stop=False)
                e.matmul(bank(pw), lhsT=onesb[0:1, :], rhs=bh[0:1, half * 512:(half + 1) * 512], start=False, stop=False)
                return e.matmul(bank(pw), lhsT=onesb[0:1, :], rhs=bl[0:1, half * 512:(half + 1) * 512], start=False, stop=True)
            A("pe", f_wo, r=["attnT%d" % t, "convT%d" % (t // 4), "wo", "onesb", "bh", "bl"], w=[BK(pw)])

    def p4_s2b(t):
        p = t % NRX
        pz = t % NRZ
        for half in range(2):
            pw = 2 * (t % 2) + half
            A("dve", lambda e, p=p, pz=pz, half=half, pw=pw: e.tensor_tensor(out=zt[pz][:, half * 512:(half + 1) * 512], in0=xt[p][:, half * 512:(half + 1) * 512], in1=bank(pw), op=ALU.add),
              r=["xt%d" % p, BK(pw)], w=["zt%d_%d" % (pz, half)])

    def p4_s3(t):
        p = t % NRZ
        ln_part1a(lnC[t % NR4], zt[p][:], ["zt%d_0" % p, "zt%d_1" % p])

    def p4_s3b(t):
        p = t % NRZ
        ln_part1b(lnC[t % NR4], zt[p][:], ["zt%d_0" % p, "zt%d_1" % p], zt[p][:], ["zt%d_0" % p, "zt%d_1" % p])

    def p4_s4(t):
        p = t % NRZ
        zr = ["zt%d_0" % p, "zt%d_1" % p]
        A("act", lambda e, t=t, p=p: e.activation(out=h1b[:, t, :], in_=zt[p][:], func=AF.Copy), r=zr, w=["h1b%d" % t])
        pt_ = 2

        def f_trf(e, p=p, pt_=pt_):
            for k in range(8):
                i_ = e.transpose(out=pst[pt_][:, k * 128:(k + 1) * 128], in_=zt[p][:, k * 128:(k + 1) * 128], identity=identf[:])
            return i_
        A("pe", f_trf, r=zr + ["identf"], w=[BK(2 * pt_), BK(2 * pt_ + 1)])
        pr_ = t % 2
        A("pool", lambda e, p=p, pr_=pr_: e.tensor_tensor(out=r1[pr_][:], in0=zt[p][:], in1=g1[:], op=ALU.mult), r=zr + ["g1"], w=["r1%d" % pr_])
        A("pool", lambda e, pr_=pr_: e.tensor_tensor(out=r1[pr_][:], in0=r1[pr_][:], in1=b1[:], op=ALU.add), r=["r1%d" % pr_, "b1"], w=["r1%d" % pr_])
        DMA("sp", macc[t * 128:(t + 1) * 128, :], r1[pr_][:], r=["r1%d" % pr_], w=["macc"])
        ph_ = t % 2
        A("act", lambda e, ph_=ph_, pt_=pt_: e.activation(out=h1T[ph_][:], in_=pst[pt_][:], func=AF.Copy), r=[BK(2 * pt_), BK(2 * pt_ + 1)], w=["h1T%d" % ph_])

    def p4_s5b(t):
        p = t % 2
        lg = lgb[p]

        def f_lg(e, p=p, lg=lg):
            for k in range(8):
                e.matmul(lg, lhsT=h1T[p][:, k * 128:(k + 1) * 128], rhs=wrg[:, k, :], start=(k == 0), stop=False)
            return e.matmul(lg, lhsT=onesf[0:1, :], rhs=brow[:], start=False, stop=True)
        A("pe", f_lg, r=["h1T%d" % p, "wrg", "brow", "onesf"], w=[BK(6 + p)])
        A("dve", lambda e, p=p, lg=lg: e.tensor_reduce(out=smx[p][:], in_=lg, axis=AX.X, op=ALU.max), r=[BK(6 + p)], w=["smx%d" % p])
        A("dve", lambda e, p=p: e.tensor_scalar(out=smx[p][:], in0=smx[p][:], scalar1=-1.0, scalar2=None, op0=ALU.mult), r=["smx%d" % p], w=["smx%d" % p])

    def p4_s5c(t):
        p = t % 2
        lg = lgb[p]
        A("act", lambda e, p=p, lg=lg: e.activation(out=sex[p][:], in_=lg, func=AF.Exp, bias=smx[p][:], scale=1.0),
          r=[BK(6 + p), "smx%d" % p], w=["sex%d" % p])
        A("dve", lambda e, p=p: e.tensor_reduce(out=ssum[p][:], in_=sex[p][:], axis=AX.X, op=ALU.add), r=["sex%d" % p], w=["ssum%d" % p])
        A("dve", lambda e, p=p: e.reciprocal(out=ssum[p][:], in_=ssum[p][:]), r=["ssum%d" % p], w=["ssum%d" % p])
        A("dve", lambda e, p=p, t=t: e.tensor_scalar(out=aff[:, t, :], in0=sex[p][:], scalar1=ssum[p][:], scalar2=None, op0=ALU.mult), r=["sex%d" % p, "ssum%d" % p], w=["aff%d" % t])
    swpipe(NT, [p4_s0, p4_s0b, p4_s1, p4_s1b, p4_s2a, p4_s2b, p4_s3, p4_s3b, p4_s4, p4_s5b, p4_s5c])
    if debug and "h1" in debug:
        dump("h1n", h1b[:], [128, NT, D], BF16)
        dump("aff", aff[:], [128, NT, E], F32)
    sch.barrier()
    ar.release(attnT, convT, wo, wr, wrg, g0, b0, g1, b1, brow, bh, bl, blf, *xt, *zt, *r1, *h1T, *smx, *ssum, *sex)
    ln_small_free(lnB)
    ln_small_free(lnC)
    ar.flush()

    wgb = [ar.alloc("wgb", [128, 8, FG], BF16) for _ in range(NWB)]
    wub = [ar.alloc("wub", [128, 8, FG], BF16) for _ in range(NWB)]
    wdb = [ar.alloc("wdb", [128, FG // 128, D], BF16) for _ in range(NWB)]
    NCH = FF // FG
    FPC = FG // 128

    def load_chunk(c):
        pos_e, fg = divmod(c, NCH)
        eid = ORDER[pos_e]
        s = c % NWB
        if eid < NPC:
            f0, f1 = fg * FG, (fg + 1) * FG
            rg = ["wpc_g%d_%d" % (eid, 0), "wpc_g%d_%d" % (eid, D // 2)]
            ru = ["wpc_u%d_%d" % (eid, 0), "wpc_u%d_%d" % (eid, D // 2)]
            rd = ["wpc_d%d_%d" % (eid, 0), "wpc_d%d_%d" % (eid, FF // 2)]
            DMA("sp", wgb[s][:], wpc[("g", eid)].rearrange("(k p) f -> p k f", p=128)[:, :, f0:f1], r=rg, w=["wg%d" % s])
            DMA("sp", wub[s][:], wpc[("u", eid)].rearrange("(k p) f -> p k f", p=128)[:, :, f0:f1], r=ru, w=["wu%d" % s])
            DMA("sp", wdb[s][:], wpc[("d", eid)][f0:f1, :].rearrange("(j p) d -> p j d", p=128), r=rd, w=["wd%d" % s])
        else:
            DMA("pool", wgb[s][:], wg_d[eid].rearrange("(k p) f -> p k f", p=128)[:, :, fg * FG:(fg + 1) * FG], w=["wg%d" % s])
            DMA("pool", wub[s][:], wu_d[eid].rearrange("(k p) f -> p k f", p=128)[:, :, fg * FG:(fg + 1) * FG], w=["wu%d" % s])
            DMA("pool", wdb[s][:], wd_d[eid][fg * FG:(fg + 1) * FG, :].rearrange("(j p) d -> p j d", p=128), w=["wd%d" % s])

    for c in range(NWB):
        load_chunk(c)

    tp = ar.alloc("tp", [128, NT, 2], BF16)
    DMA("sp", tp[:].rearrange("p t c -> p (t c)"), c_tp_d, w=["tp"])
    A8 = ar.alloc("A8", [128, 256], F32)
    junk = ar.alloc("junk", [128, 256], F32)
    gmat = ar.alloc("gmat", [128, 128], F32)
    cand = ar.alloc("cand", [128, 1], F32)
    cnt = ar.alloc("cnt", [128, 1], F32)
    dlt = ar.alloc("dlt", [128, 1], F32)
    cnt2 = ar.alloc("cnt2", [128, 1], F32)
    thr = ar.alloc("thr", [128, 1], F32)
    mask8 = ar.alloc("mask8", [128, 256], BF16)
    mask_tok = ar.alloc("mask_tok", [128, NT * E], BF16)
    posm = ar.alloc("posm", [128, NT * E], F32)
    ahl = ar.alloc("ahl", [128, NT * E, 4], BF16)
    DMA("sp", gmat[:], c_gmat_d, w=["gmat"])

    affp = ar.alloc("affp", [128, 2, 128], F32)
    for h in range(2):
        A("dve", lambda e, h=h: e.tensor_copy(out=affp[:, h, :].rearrange("p (e g) -> p e g", g=8), in_=aff[:, h::2, :].rearrange("p g e -> p e g")),
          r=["aff%d" % t for t in range(NT)], w=["affp"])

    def f_a8(e):
        for h in range(2):
            i_ = e.transpose(out=bank(0)[:, h * 128:(h + 1) * 128], in_=affp[:, h, :], identity=identf[:])
        return i_
    A("pe", f_a8, r=["affp", "identf"], w=[BK(0)])
    A("dve", lambda e: e.tensor_copy(out=A8[:], in_=bank(0)[:, 0:256]), r=[BK(0)], w=["A8"])
    A("dve", lambda e: e.memset(thr[:], 0.0), w=["thr"])
    A("dve", lambda e: e.memset(cand[:], 0.5), w=["cand"])
    for i in range(1, NBIS + 1):
        step = 2.0 ** (-i)
        pb_ = 1 + (i % 2)
        A("dve", lambda e: e.tensor_scalar(out=junk[:], in0=A8[:], scalar1=cand[:], scalar2=None, op0=ALU.is_ge, op1=ALU.add, accum_out=cnt[:]),
          r=["A8", "cand"], w=["junk", "cnt"])
        A("dve", lambda e: e.tensor_copy(out=cnt2[:], in_=cnt[:]), r=["cnt"], w=["cnt2"])
        A("pe", lambda e, pb_=pb_: e.matmul(bank(pb_)[:, 0:1], lhsT=gmat[:], rhs=cnt2[:], start=True, stop=True), r=["gmat", "cnt2"], w=[BK(pb_)])
        A("dve", lambda e, pb_=pb_, step=step: e.tensor_scalar(out=dlt[:], in0=bank(pb_)[:, 0:1], scalar1=float(CAP) - 0.5, scalar2=step, op0=ALU.is_ge, op1=ALU.mult),
          r=[BK(pb_)], w=["dlt"])
        A("dve", lambda e: e.tensor_tensor(out=thr[:], in0=thr[:], in1=dlt[:], op=ALU.add), r=["thr", "dlt"], w=["thr"])
        A("dve", lambda e, step=step: e.tensor_scalar(out=cand[:], in0=thr[:], scalar1=step * 0.5, scalar2=None, op0=ALU.add), r=["thr"], w=["cand"])
    A("dve", lambda e: e.tensor_scalar(out=mask8[:], in0=A8[:], scalar1=thr[:], scalar2=None, op0=ALU.is_ge), r=["A8", "thr"], w=["mask8"])

    def f_mtok(e):
        for h in range(2):
            i_ = e.transpose(out=bankb(4)[:, h * 128:(h + 1) * 128], in_=mask8[:, h * 128:(h + 1) * 128], identity=identb[:])
        return i_
    A("pe", f_mtok, r=["mask8", "identb"], w=[BK(4)])
    mtv = mask_tok[:].rearrange("p (t e) -> p t e", e=E)
    for h in range(2):
        A("dve", lambda e, h=h: e.tensor_copy(out=mtv[:, h::2, :], in_=bankb(4)[:, h * 128:(h + 1) * 128].rearrange("p (e g) -> p g e", g=8)), r=[BK(4)], w=["mask_tok"])

    def f_pos(e):
        for t in range(NT):
            for t2_ in range(t + 1):
                lhs = ustr[:] if t2_ == t else onesb[:]
                i_ = e.matmul(bank(5)[:, t * 16:(t + 1) * 16], lhsT=lhs, rhs=mask_tok[:, t2_ * 16:(t2_ + 1) * 16], start=(t2_ == 0), stop=(t2_ == t))
        return i_
    A("pe", f_pos, r=["mask_tok", "ustr", "onesb"], w=[BK(5)])
    A("dve", lambda e: e.scalar_tensor_tensor(out=posm[:], in0=bank(5)[:, 0:NT * E], scalar=1.0, in1=mask_tok[:], op0=ALU.add, op1=ALU.mult), r=[BK(5), "mask_tok"], w=["posm"])
    A("dve", lambda e: e.tensor_scalar(out=posm[:], in0=posm[:], scalar1=-1.0, scalar2=None, op0=ALU.add), r=["posm"], w=["posm"])
    affv = aff[:].rearrange("p t e -> p (t e)")
    A("dve", lambda e: e.tensor_copy(out=ahl[:, :, 0], in_=affv), r=["aff%d" % t for t in range(NT)], w=["ahl"])
    A("dve", lambda e: e.tensor_tensor(out=ahl[:, :, 1], in0=affv, in1=ahl[:, :, 0], op=ALU.subtract), r=["ahl"] + ["aff%d" % t for t in range(NT)], w=["ahl"])
    A("dve", lambda e: e.tensor_copy(out=ahl[:].rearrange("p (t e) c -> p t e c", e=E)[:, :, :, 2:4], in_=tp[:].unsqueeze(2).to_broadcast([128, NT, E, 2])), r=["tp", "ahl"], w=["ahl"])
    if debug and "posm" in debug:
        dump("posm", posm[:], [128, NT * E], F32)
    sch.barrier()
    ar.release(A8, junk, gmat, cand, thr, cnt, cnt2, dlt, mask8, mask_tok, tp, affp)
    ar.flush()

    Pm = [ar.alloc("Pm", [128, NT, CAP], BF16) for _ in range(2)]
    gate = [ar.alloc("gate", [128, 2], F32) for _ in range(2)]
    gi = [ar.alloc("gi", [128, 8], F32) for _ in range(2)]
    idxf = [ar.alloc("idxf", [128, 2], F32) for _ in range(2)]
    idxi = [ar.alloc("idxi", [128, 2], I32) for _ in range(2)]
    xgT = ar.alloc("xgT", [128, 8, CAP], BF16)
    yg = [ar.alloc("yg", [128, 2, D], F32) for _ in range(2)]
    sa = [ar.alloc("sa", [128, CAP], F32) for _ in range(2)]
    actT = [ar.alloc("actT", [128, CAP], BF16) for _ in range(4)]
    misc_ctr = [0]

    def misc_bank():
        b_ = 6 + (misc_ctr[0] % 2)
        misc_ctr[0] += 1
        return b_

    def prep_expert(e_):
        pe_ = e_ % 2
        eid = ORDER[e_]
        for t in range(NT):
            A("dve", lambda e, t=t, pe_=pe_, eid=eid: e.tensor_scalar(out=Pm[pe_][:, t, :], in0=iota_c[:], scalar1=posm[:, t * E + eid:t * E + eid + 1], scalar2=None, op0=ALU.is_equal),
              r=["iota_c", "posm"], w=["Pm%d_%d" % (pe_, t)])
        mb = misc_bank()

        def f_gate(e, mb=mb, pe_=pe_, eid=eid):
            for ct in range(2):
                for t in range(NT):
                    i_ = e.matmul(bank(mb)[:, ct * 4:ct * 4 + 4], lhsT=Pm[pe_][:, t, ct * 128:(ct + 1) * 128], rhs=ahl[:, t * E + eid, :], start=(t == 0), stop=(t == NT - 1))
            return i_
        A("pe", f_gate, r=["ahl"] + ["Pm%d_%d" % (pe_, t) for t in range(NT)], w=[BK(mb)])
        A("dve", lambda e, mb=mb, pe_=pe_: e.tensor_copy(out=gi[pe_][:], in_=bank(mb)[:, 0:8]), r=[BK(mb)], w=["gi%d" % pe_])
        giv = gi[pe_][:].rearrange("p (c f) -> p c f", f=4)
        A("dve", lambda e, pe_=pe_, giv=giv: e.tensor_tensor(out=gate[pe_][:], in0=giv[:, :, 0], in1=giv[:, :, 1], op=ALU.add), r=["gi%d" % pe_], w=["gate%d" % pe_])
        A("dve", lambda e, pe_=pe_, giv=giv: e.scalar_tensor_tensor(out=idxf[pe_][:], in0=giv[:, :, 2], scalar=128.0, in1=giv[:, :, 3], op0=ALU.mult, op1=ALU.add),
          r=["gi%d" % pe_], w=["idxf%d" % pe_])
        A("dve", lambda e, pe_=pe_: e.tensor_copy(out=idxi[pe_][:], in_=idxf[pe_][:]), r=["idxf%d" % pe_], w=["idxi%d" % pe_])

    def gather_expert(e_):
        pe_ = e_ % 2
        for dk in range(8):
            mb = misc_bank()

            def f_g(e, mb=mb, dk=dk, pe_=pe_):
                for t in range(NT):
                    i_ = e.matmul(bank(mb)[:, 0:CAP], lhsT=h1b[:, t, dk * 128:(dk + 1) * 128], rhs=Pm[pe_][:, t, :], start=(t == 0), stop=(t == NT - 1))
                return i_
            A("pe", f_g, r=["h1b%d" % t for t in range(NT)] + ["Pm%d_%d" % (pe_, t) for t in range(NT)], w=[BK(mb)])
            A("act", lambda e, mb=mb, dk=dk: e.activation(out=xgT[:, dk, :], in_=bank(mb)[:, 0:CAP], func=AF.Identity, bias=b1pk[:, dk:dk + 1], scale=g1pk[:, dk:dk + 1]),
              r=[BK(mb), "g1pk", "b1pk"], w=["xgT%d" % dk])

    def gu(e_, ft):
        c = e_ * NCH + ft // FPC
        s = c % NWB
        j = ft % FPC
        gb = 4 + (ft % 2)

        def f_gu(e, s=s, j=j, gb=gb):
            for k in range(8):
                e.matmul(bank(gb)[:, 0:CAP], lhsT=wgb[s][:, k, j * 128:(j + 1) * 128], rhs=xgT[:, k, :], start=(k == 0), stop=(k == 7))
            for k in range(8):
                i_ = e.matmul(bank(gb)[:, CAP:2 * CAP], lhsT=wub[s][:, k, j * 128:(j + 1) * 128], rhs=xgT[:, k, :], start=(k == 0), stop=(k == 7))
            return i_
        A("pe", f_gu, r=["wg%d" % s, "wu%d" % s] + ["xgT%d" % dk for dk in range(8)], w=[BK(gb)])
        ps_ = ft % 2
        pa_ = ft % 4
        A("act", lambda e, gb=gb, ps_=ps_: e.activation(out=sa[ps_][:], in_=bank(gb)[:, 0:CAP], func=AF.Silu), r=[BK(gb)], w=["sa%d" % ps_])
        A("dve", lambda e, gb=gb, ps_=ps_, pa_=pa_: e.tensor_tensor(out=actT[pa_][:], in0=bank(gb)[:, CAP:2 * CAP], in1=sa[ps_][:], op=ALU.mult), r=[BK(gb), "sa%d" % ps_], w=["actT%d" % pa_])

    def down(e_, ft):
        c = e_ * NCH + ft // FPC
        s = c % NWB
        j = ft % FPC
        pa_ = ft % 4

        def f_d(e, s=s, j=j, pa_=pa_, ft=ft):
            for ct in range(2):
                for dh in range(2):
                    i_ = e.matmul(pst[ct][:, dh * 512:(dh + 1) * 512], lhsT=actT[pa_][:, ct * 128:(ct + 1) * 128], rhs=wdb[s][:, j, dh * 512:(dh + 1) * 512], start=(ft == 0), stop=(ft == NT - 1))
            return i_
        A("pe", f_d, r=["actT%d" % pa_, "wd%d" % s], w=[BK(0), BK(1), BK(2), BK(3)])
        if j == FPC - 1 and c + NWB < E * NCH:
            load_chunk(c + NWB)

    def yevac_scatter(e_):
        pe_ = e_ % 2
        for ct in range(2):
            A("act", lambda e, ct=ct, pe_=pe_: e.activation(out=yg[pe_][:, ct, :], in_=pst[ct][:], func=AF.Copy, scale=gate[pe_][:, ct:ct + 1]),
              r=[BK(2 * ct), BK(2 * ct + 1), "gate%d" % pe_], w=["yg%d_%d" % (pe_, ct)])
        for ct in range(2):
            A("pool", lambda e, ct=ct, pe_=pe_: e.indirect_dma_start(out=macc, out_offset=bass.IndirectOffsetOnAxis(ap=idxi[pe_][:, ct:ct + 1], axis=0),
                                                                     in_=yg[pe_][:, ct, :], in_offset=None, bounds_check=S - 1, oob_is_err=True, compute_op=ALU.add),
              r=["yg%d_%d" % (pe_, ct), "idxi%d" % pe_, "macc"], w=["macc"], dma=True)

    SKEW = 2
    prep_expert(0)
    gather_expert(0)
    for e_ in range(E):
        for ft in range(NT):
            gu(e_, ft)
            if ft >= SKEW:
                down(e_, ft - SKEW)
        if e_ + 1 < E:
            prep_expert(e_ + 1)
        for ft in range(NT - SKEW, NT):
            down(e_, ft)
        yevac_scatter(e_)
        if e_ + 1 < E:
            gather_expert(e_ + 1)
    sch.barrier()
    ar.release(h1b, *Pm, *gate, *gi, *idxf, *idxi, xgT, *yg, *sa, *actT, *wgb, *wub, *wdb, posm, ahl, aff, g1pk, b1pk)
    ar.flush()

    g2 = ar.alloc("g2", [128, D], F32)
    b2 = ar.alloc("b2", [128, D], F32)
    DMA("sp", g2[:], g2_d, w=["g2"])
    DMA("sp", b2[:], b2_d, w=["b2"])
    NR7 = 6
    rt = [ar.alloc("rt", [128, D], F32) for _ in range(NR7)]
    ot = [ar.alloc("ot", [128, D], F32) for _ in range(NR7)]
    lnD = ln_small("lnD", NR7)

    def p7_s0(t):
        p = t % NR7
        DMA("sp", rt[p][:], macc[t * 128:(t + 1) * 128, :], r=["macc"], w=["rt%d" % p])

    def p7_s0b(t):
        pass

    def p7_s1(t):
        p = t % NR7
        ln_part1a(lnD[p], rt[p][:], ["rt%d" % p])

    def p7_s1b(t):
        p = t % NR7
        ln_part1b(lnD[p], rt[p][:], ["rt%d" % p], rt[p][:], ["rt%d" % p])

    def p7_s2(t):
        p = t % NR7
        ln_part2(rt[p][:], ["rt%d" % p], ot[p][:], ["ot%d" % p], g2, b2, "g2", "b2")
        DMA("sp", out_d[t * 128:(t + 1) * 128, :], ot[p][:], r=["ot%d" % p])
    swpipe(NT, [p7_s0, p7_s0b, p7_s1, p7_s1b, p7_s2])
    emit_program(sch, nc)
    return nc, dbg_outs


def _consts():
    bf = ml_dtypes.bfloat16
    ident = np.eye(128, dtype=np.float32)
    iota = np.broadcast_to(np.arange(256, dtype=np.float32)[None, :], (128, 256)).copy()
    iotap = np.stack([np.arange(128, dtype=np.float32), np.arange(128, dtype=np.float32) + 128.0], axis=1)
    ustr = np.triu(np.ones((128, 128), dtype=np.float32), k=1)
    half = 16
    invf = (np.float32(10000.0) ** (-np.arange(half, dtype=np.float32) / np.float32(half))).astype(np.float32)
    invf = np.concatenate([invf] * 8).reshape(128, 1)
    tp = np.zeros((128, 16, 2), dtype=np.float32)
    tp[:, :, 0] = np.arange(16, dtype=np.float32)[None, :]
    tp[:, :, 1] = np.arange(128, dtype=np.float32)[:, None]
    return {
        "c_identf": ident, "c_identb": ident.astype(bf), "c_iota": iota, "c_iotap": np.ascontiguousarray(iotap),
        "c_ustr": ustr.astype(bf), "c_invf": invf, "c_tp": tp.reshape(128, 32).astype(bf), "c_gmat": np.kron(np.eye(16, dtype=np.float32), np.ones((8, 8), dtype=np.float32)),
    }


def _bc(v):
    return np.ascontiguousarray(np.broadcast_to(np.asarray(v, dtype=np.float32).reshape(1, -1), (128, v.size)))


def _pk(v, k):
    return np.ascontiguousarray(np.asarray(v, dtype=np.float32).reshape(k, 128).T)


_CACHE = {}


def make_in_maps(inputs, cores):
    f = lambda a: np.ascontiguousarray(np.asarray(a))
    shared = {
        "emb_ln_g": _bc(f(inputs["emb_ln_g"])), "emb_ln_b": _bc(f(inputs["emb_ln_b"])),
        "w_in": f(inputs["w_in"])[0], "q_norm_g": _pk(f(inputs["q_norm_g"])[0], 3), "w_qb": f(inputs["w_qb"])[0],
        "kv_norm_g": _pk(f(inputs["kv_norm_g"])[0], 2), "w_kvb": f(inputs["w_kvb"])[0],
        "conv_w": np.ascontiguousarray(f(inputs["conv_w"])[0].reshape(31, 4, 128).transpose(2, 1, 0).reshape(128, 4 * 31)),
        "conv_b": _pk(f(inputs["conv_b"])[0], 4), "conv_ln_g": _pk(f(inputs["conv_ln_g"])[0], 4), "conv_ln_b": _pk(f(inputs["conv_ln_b"])[0], 4),
        "w_o": f(inputs["w_o"])[0], "ln1_g": _bc(f(inputs["ln1_g"])[0]), "ln1_b": _bc(f(inputs["ln1_b"])[0]),
        "ln1_g_pk": _pk(f(inputs["ln1_g"])[0], 8), "ln1_b_pk": _pk(f(inputs["ln1_b"])[0], 8),
        "w_router": f(inputs["w_router"])[0], "w_gate": f(inputs["w_gate"])[0], "w_up": f(inputs["w_up"])[0], "w_down": f(inputs["w_down"])[0],
        "ln2_g": _bc(f(inputs["ln2_g"])[0]), "ln2_b": _bc(f(inputs["ln2_b"])[0]),
    }
    shared.update(_consts())
    x = f(inputs["x"])
    pos = f(inputs["positions"]).astype(np.int32)
    maps = []
    for c in cores:
        m = dict(shared)
        m["x"] = np.ascontiguousarray(x[c])
        m["pos"] = np.ascontiguousarray(np.broadcast_to(pos[c].reshape(4, 1, 512), (4, 32, 512)).reshape(128, 512))
        maps.append(m)
    return maps


def kernel(**inputs):
    if "nc" not in _CACHE:
        _CACHE["nc"] = build()[0]
    nc = _CACHE["nc"]
    cores = list(range(8))
    in_maps = make_in_maps(inputs, cores)
    res = run_bass_kernel_spmd(nc, in_maps, core_ids=cores)
    out = np.stack([np.asarray(r["out"]) for r in res.results], axis=0)
    return out.astype(np.float32)
```
